# Optimizing a Trainium2 kernel written in Bass

```python
import math
import jax, jax.numpy as jnp
from jax import lax
import numpy as np

D_MODEL = 1024
BATCH = 8
SEQ = 2048
DEPTH = 1

N_META = 16
N_DIFF_HEADS = 8
HEAD_DIM = 64
V_DIM = 2 * HEAD_DIM
QK_WIDTH = N_DIFF_HEADS * 2 * HEAD_DIM
ATTN_WIDTH = N_DIFF_HEADS * V_DIM
POOL_WINDOWS = (2, 4, 8, 16)
N_POOL_GROUPS = len(POOL_WINDOWS)
POOL_GROUP_DIM = 128
POOL_WIDTH = N_POOL_GROUPS * POOL_GROUP_DIM
N_BRANCHES = 2
GATE_WIDTH = N_BRANCHES * D_MODEL
IN_WIDTH = 2 * QK_WIDTH + ATTN_WIDTH + POOL_WIDTH + GATE_WIDTH
ROPE_THETA = 10000.0
Q_BLOCK = 128
N_GROUPS = 4
EXPERTS_PER_GROUP = 4
N_EXPERTS = N_GROUPS * EXPERTS_PER_GROUP
TOP_K = 2
D_EXPERT = 512
EPS = 1e-6

kernel_name = "hybrid_diffattn_pool_hmoe_block"


def rmsnorm(x, g):
    xf = x.astype(jnp.float32)
    y = xf * lax.rsqrt(jnp.mean(xf * xf, axis=-1, keepdims=True) + EPS)
    return (y * g.astype(jnp.float32)).astype(x.dtype)


def rope_tables(T, dtype):
    inv = 1.0 / (ROPE_THETA ** (jnp.arange(0, HEAD_DIM, 2, dtype=jnp.float32) / HEAD_DIM))
    ang = jnp.arange(T, dtype=jnp.float32)[:, None] * inv[None, :]
    ang = jnp.concatenate([ang, ang], axis=-1)
    return jnp.cos(ang).astype(dtype), jnp.sin(ang).astype(dtype)


def apply_rope(x, cos, sin):
    half = HEAD_DIM // 2
    rot = jnp.concatenate([-x[..., half:], x[..., :half]], axis=-1)
    c = cos[None, :, None, None, :]
    s = sin[None, :, None, None, :]
    return x * c + rot * s


def diff_attention(q, k, v, lam):
    B, T, H = q.shape[0], q.shape[1], q.shape[2]
    n_blk = -(-T // Q_BLOCK)
    Tp = n_blk * Q_BLOCK
    pad5 = ((0, 0), (0, Tp - T), (0, 0), (0, 0), (0, 0))
    q = jnp.pad(q, pad5)
    k = jnp.pad(k, pad5)
    v = jnp.pad(v, pad5[:4])
    scale = 1.0 / math.sqrt(HEAD_DIM)
    key_pos = jnp.arange(Tp)

    def block(i):
        start = i * Q_BLOCK
        qb = lax.dynamic_slice_in_dim(q, start, Q_BLOCK, axis=1)
        s = jnp.einsum('bqhcd,bkhcd->bhcqk', qb, k).astype(jnp.float32) * scale
        q_pos = start + jnp.arange(Q_BLOCK)
        mask = key_pos[None, :] <= q_pos[:, None]
        s = jnp.where(mask[None, None, None], s, -jnp.inf)
        p = jax.nn.softmax(s, axis=-1)
        a = p[:, :, 0] - lam * p[:, :, 1]
        return jnp.einsum('bhqk,bkhe->bqhe', a.astype(v.dtype), v)

    out = lax.map(block, jnp.arange(n_blk))
    out = jnp.moveaxis(out, 0, 1).reshape(B, Tp, H, V_DIM)
    return out[:, :T]


def multiscale_pool(u, pool_w, pool_scale):
    B, T, G, C = u.shape
    uf = u.astype(jnp.float32)
    cs = jnp.pad(lax.cumsum(uf, axis=1), ((0, 0), (1, 0), (0, 0), (0, 0)))
    t = jnp.arange(T)
    means = []
    for g, w in enumerate(POOL_WINDOWS):
        c = cs[:, :, g]
        hi = c[:, 1:]
        lo = jnp.pad(c[:, :T + 1 - w], ((0, 0), (w - 1, 0), (0, 0)))
        cnt = jnp.minimum(t + 1, w).astype(jnp.float32)
        means.append((hi - lo) / cnt[None, :, None])
    mean = jnp.stack(means, axis=2)
    y = (mean - uf).astype(u.dtype)
    y = jnp.einsum('btgc,gcd->btgd', y, pool_w).reshape(B, T, G * C)
    return y * pool_scale


def hier_moe(h, w_rg, b_rg, w_re, b_re, w_gate, w_up, w_down):
    B, T, D = h.shape
    hf = h.reshape(-1, D)
    glog = (hf @ w_rg + b_rg).astype(jnp.float32)
    gprob = jax.nn.softmax(glog, axis=-1)
    gsel = jnp.argmax(glog, axis=-1)
    p_group = jnp.take_along_axis(gprob, gsel[:, None], axis=1)
    elog = (hf @ w_re + b_re).astype(jnp.float32).reshape(-1, N_GROUPS, EXPERTS_PER_GROUP)
    elog_g = jnp.take_along_axis(elog, gsel[:, None, None], axis=1)[:, 0]
    top_v, top_i = lax.top_k(elog_g, TOP_K)
    w2 = jax.nn.softmax(top_v, axis=-1) * p_group
    eidx = gsel[:, None] * EXPERTS_PER_GROUP + top_i
    combine = jnp.sum(jax.nn.one_hot(eidx, N_EXPERTS, dtype=jnp.float32) * w2[..., None], axis=1)
    combine = combine.astype(h.dtype)
    y = jnp.zeros_like(hf)
    for e in range(N_EXPERTS):
        a = jax.nn.silu(hf @ w_gate[e]) * (hf @ w_up[e])
        y = y + combine[:, e:e + 1] * (a @ w_down[e])
    return y.reshape(B, T, D)


def setup_inputs(seed: int = 0) -> dict:
    key = jax.random.key(seed)
    ks = jax.random.split(key, 24)
    f32 = jnp.float32
    nrm = lambda k, shape, s: jax.random.normal(k, shape, f32) * s
    L = DEPTH
    return {
        "x": nrm(ks[0], (BATCH, SEQ, D_MODEL), 1.0),
        "meta": nrm(ks[1], (N_META, D_MODEL), 1.0),
        "norm1_g": 1.0 + nrm(ks[2], (L, D_MODEL), 0.02),
        "w_in": nrm(ks[3], (L, D_MODEL, IN_WIDTH), D_MODEL ** -0.5),
        "b_gate": nrm(ks[4], (L, GATE_WIDTH), 0.02),
        "lambda_q1": nrm(ks[5], (L, HEAD_DIM), 0.1),
        "lambda_k1": nrm(ks[6], (L, HEAD_DIM), 0.1),
        "lambda_q2": nrm(ks[7], (L, HEAD_DIM), 0.1),
        "lambda_k2": nrm(ks[8], (L, HEAD_DIM), 0.1),
        "subln_g": 1.0 + nrm(ks[9], (L, V_DIM), 0.02),
        "pool_w": nrm(ks[10], (L, N_POOL_GROUPS, POOL_GROUP_DIM, POOL_GROUP_DIM), POOL_GROUP_DIM ** -0.5),
        "pool_scale": 1.0 + nrm(ks[11], (L, POOL_WIDTH), 0.1),
        "w_attn_br": nrm(ks[12], (L, ATTN_WIDTH, D_MODEL), ATTN_WIDTH ** -0.5),
        "w_pool_br": nrm(ks[13], (L, POOL_WIDTH, D_MODEL), POOL_WIDTH ** -0.5),
        "w_out": nrm(ks[14], (L, D_MODEL, D_MODEL), D_MODEL ** -0.5),
        "norm2_g": 1.0 + nrm(ks[15], (L, D_MODEL), 0.02),
        "w_router_group": nrm(ks[16], (L, D_MODEL, N_GROUPS), D_MODEL ** -0.5),
        "b_router_group": nrm(ks[17], (L, N_GROUPS), 0.01),
        "w_router_expert": nrm(ks[18], (L, D_MODEL, N_EXPERTS), D_MODEL ** -0.5),
        "b_router_expert": nrm(ks[19], (L, N_EXPERTS), 0.01),
        "w_e_gate": nrm(ks[20], (L, N_EXPERTS, D_MODEL, D_EXPERT), D_MODEL ** -0.5),
        "w_e_up": nrm(ks[21], (L, N_EXPERTS, D_MODEL, D_EXPERT), D_MODEL ** -0.5),
        "w_e_down": nrm(ks[22], (L, N_EXPERTS, D_EXPERT, D_MODEL), D_EXPERT ** -0.5),
        "final_g": 1.0 + nrm(ks[23], (D_MODEL,), 0.02),
    }


def reference(x, meta, norm1_g, w_in, b_gate, lambda_q1, lambda_k1, lambda_q2, lambda_k2,
              subln_g, pool_w, pool_scale, w_attn_br, w_pool_br, w_out, norm2_g,
              w_router_group, b_router_group, w_router_expert, b_router_expert,
              w_e_gate, w_e_up, w_e_down, final_g):
    B = x.shape[0]
    h = jnp.concatenate([jnp.broadcast_to(meta[None].astype(x.dtype), (B, N_META, D_MODEL)), x], axis=1)
    T = h.shape[1]
    cos, sin = rope_tables(T, h.dtype)
    o_q, o_k, o_v = 0, QK_WIDTH, 2 * QK_WIDTH
    o_p = o_v + ATTN_WIDTH
    o_g = o_p + POOL_WIDTH

    for l in range(DEPTH):
        lambda_init = 0.8 - 0.6 * math.exp(-0.3 * l)
        hn = rmsnorm(h, norm1_g[l])
        z = hn @ w_in[l]
        q = z[..., o_q:o_k].reshape(B, T, N_DIFF_HEADS, 2, HEAD_DIM)
        k = z[..., o_k:o_v].reshape(B, T, N_DIFF_HEADS, 2, HEAD_DIM)
        v = z[..., o_v:o_p].reshape(B, T, N_DIFF_HEADS, V_DIM)
        u = z[..., o_p:o_g].reshape(B, T, N_POOL_GROUPS, POOL_GROUP_DIM)
        gates = jax.nn.sigmoid(z[..., o_g:] + b_gate[l]).reshape(B, T, N_BRANCHES, D_MODEL)

        q = apply_rope(q, cos, sin)
        k = apply_rope(k, cos, sin)
        lam = (jnp.exp(jnp.sum(lambda_q1[l].astype(jnp.float32) * lambda_k1[l].astype(jnp.float32)))
               - jnp.exp(jnp.sum(lambda_q2[l].astype(jnp.float32) * lambda_k2[l].astype(jnp.float32)))
               + lambda_init)
        a = diff_attention(q, k, v, lam)
        a = rmsnorm(a, subln_g[l]) * (1.0 - lambda_init)
        y_attn = a.reshape(B, T, ATTN_WIDTH) @ w_attn_br[l]

        y_pool = multiscale_pool(u, pool_w[l], pool_scale[l]) @ w_pool_br[l]

        mixed = gates[:, :, 0] * y_attn + gates[:, :, 1] * y_pool
        h = h + mixed @ w_out[l]

        hn2 = rmsnorm(h, norm2_g[l])
        h = h + hier_moe(hn2, w_router_group[l], b_router_group[l], w_router_expert[l],
                         b_router_expert[l], w_e_gate[l], w_e_up[l], w_e_down[l])

    out = rmsnorm(h, final_g)
    return out[:, N_META:]
```

```python
import contextlib
import math

import numpy as np

import concourse.bass as bass
import concourse.mybir as mybir
from concourse.bass_utils import run_bass_kernel_spmd

F32 = mybir.dt.float32
BF16 = mybir.dt.bfloat16
AF = mybir.ActivationFunctionType
ALU = mybir.AluOpType
AX = mybir.AxisListType

D = 1024
SEQ = 2048
NMETA = 16
NH = 8
HD = 64
VD = 128
NPG = 4
POOL_W = (2, 4, 8, 16)
IN_W = 5632
NE = 16
DE = 512
EPS = 1e-6
TC = 2176
MOFF = 112
ROFF = 128
O_Q, O_K, O_V, O_P, O_G = 0, 1024, 2048, 3072, 3584
LAMBDA_INIT = 0.8 - 0.6 * math.exp(-0.3 * 0)
EPOCH = 12000


class _Op:
    __slots__ = ("eng", "fn", "waits", "signal", "idx", "dma_stream", "sigcount")

    def __init__(self, eng, fn):
        self.eng = eng
        self.fn = fn
        self.waits = []
        self.signal = False
        self.idx = -1
        self.dma_stream = None
        self.sigcount = 0


class Sched:
    ENGS = ("pe", "act", "dve", "pool", "sp")

    def __init__(self):
        self.ops = {e: [] for e in self.ENGS}
        self.last_w = {}
        self.readers = {}
        self.known = {e: {} for e in self.ENGS}
        self.streams = {}
        self.pending = {e: [] for e in self.ENGS}

    def _deps(self, eng, reads, writes, is_dma=False):
        deps = {}

        def add(kind, src, n, raw):
            if kind == "e" and src == eng and not is_dma:
                if eng == "pe" or eng == "sp":
                    return
                if not raw and eng != "pool":
                    return
            if kind == "d":
                n = self.streams[src]
            key = (kind, src)
            if deps.get(key, -1) < n:
                deps[key] = n

        for r in reads:
            ev = self.last_w.get(r)
            if ev is not None:
                add(ev[0], ev[1], ev[2], True)
        for w in writes:
            ev = self.last_w.get(w)
            if ev is not None:
                add(ev[0], ev[1], ev[2], False)
            rd = self.readers.get(w)
            if rd:
                for key, n in rd.items():
                    add(key[0], key[1], n, False)
        for (kind, src, n) in self.pending[eng]:
            if kind == "e" and src == eng and not is_dma:
                continue
            key = (kind, src)
            if deps.get(key, -1) < n:
                deps[key] = n
        self.pending[eng] = []
        out = []
        kn = self.known[eng]
        for key, n in deps.items():
            if kn.get(key, -1) >= n:
                continue
            kn[key] = n
            out.append((key[0], key[1], n))
        return out

    def _commit(self, ev, reads, writes):
        for w in writes:
            self.last_w[w] = ev
            self.readers[w] = {}
        key = (ev[0], ev[1])
        for r in reads:
            d = self.readers.setdefault(r, {})
            if d.get(key, -1) < ev[2]:
                d[key] = ev[2]

    def _add(self, eng, fn, reads, writes, stream):
        ps_reads = [r for r in reads if isinstance(r, tuple) and r[0] == "ps"]
        if ps_reads:
            writes = list(writes) + ps_reads
        o = _Op(eng, fn)
        o.waits = self._deps(eng, reads, writes, is_dma=stream is not None)
        lst = self.ops[eng]
        o.idx = len(lst)
        lst.append(o)
        for (kind, src, n) in o.waits:
            if kind == "e":
                self.ops[src][n].signal = True
        if stream is None:
            ev = ("e", eng, o.idx)
        else:
            c = self.streams.get(stream, 0) + 1
            self.streams[stream] = c
            o.dma_stream = stream
            ev = ("d", stream, c)
        self._commit(ev, reads, writes)
        return o

    def op(self, eng, fn, reads=(), writes=()):
        return self._add(eng, fn, reads, writes, None)

    def dma(self, queue, stream, fn, reads=(), writes=()):
        return self._add(queue, fn, reads, writes, stream)

    def barrier(self):
        evs = []
        for e in self.ENGS:
            if self.ops[e]:
                evs.append(("e", e, len(self.ops[e]) - 1))
        for s, c in self.streams.items():
            evs.append(("d", s, c))
        for e in self.ENGS:
            self.pending[e] = list(evs)
        fixed = []
        for (kind, src, n) in evs:
            if kind == "e":
                i = n
                while i >= 0 and self.ops[src][i].dma_stream is not None:
                    i -= 1
                if i < 0:
                    continue
                fixed.append(("e", src, i))
            else:
                fixed.append((kind, src, n))
        for e in self.ENGS:
            self.pending[e] = list(fixed)

    def finish(self, eng="sp"):
        self.barrier()
        o = _Op(eng, None)
        o.waits = self._deps(eng, (), ())
        for (kind, src, n) in o.waits:
            if kind == "e":
                self.ops[src][n].signal = True
        o.idx = len(self.ops[eng])
        self.ops[eng].append(o)

    def emit(self, nc):
        for e, lst in self.ops.items():
            c = 0
            for o in lst:
                if o.signal:
                    c += 1
                o.sigcount = c
        nsig = {e: (lst[-1].sigcount if lst else 0) for e, lst in self.ops.items()}
        with contextlib.ExitStack() as st:
            esems = {}
            for e in self.ENGS:
                n_ep = nsig[e] // EPOCH + 1
                esems[e] = [st.enter_context(nc.semaphore(f"s_{e}_{k}")) for k in range(n_ep)]
            dsems = {s: st.enter_context(nc.semaphore("d_" + "_".join(str(x) for x in (s if isinstance(s, tuple) else (s,))))) for s in self.streams}
            block = st.enter_context(nc.Block())

            def run(engname):
                def body(eng):
                    for o in self.ops[engname]:
                        for (kind, src, n) in o.waits:
                            if kind == "e":
                                sc = self.ops[src][n].sigcount
                                ep, v = (sc - 1) // EPOCH, (sc - 1) % EPOCH + 1
                                eng.wait_ge(esems[src][ep], v)
                            else:
                                eng.wait_ge(dsems[src], 16 * n)
                        if o.fn is None:
                            continue
                        ins = o.fn(eng)
                        if o.dma_stream is not None:
                            ins.then_inc(dsems[o.dma_stream], 16)
                        elif o.signal:
                            ep = (o.sigcount - 1) // EPOCH
                            ins.then_inc(esems[engname][ep], 1)
                return body

            block.tensor(run("pe"))
            block.scalar(run("act"))
            block.vector(run("dve"))
            block.gpsimd(run("pool"))
            block.sync(run("sp"))
        return nsig


def build_program(dbg=(), stop_after=None):
    nc = bass.Bass("TRN2", target_bir_lowering=False)
    dbg = set(dbg)

    def din(name, shape, dt=F32):
        return nc.dram_tensor(name, list(shape), dt, kind="ExternalInput").ap()

    x_d = din("x", [SEQ, D])
    meta_d = din("meta", [NMETA, D])
    g1_d = din("norm1_g", [1, D])
    w_in_d = din("w_in", [D, IN_W])
    bgate_d = din("b_gate_t", [128, 16])
    lq1_d = din("lambda_q1", [1, HD])
    lk1_d = din("lambda_k1", [1, HD])
    lq2_d = din("lambda_q2", [1, HD])
    lk2_d = din("lambda_k2", [1, HD])
    subg_d = din("subln_g_t", [128, 1])
    poolw_d = din("pool_w", [NPG, 128, 128])
    pscale_d = din("pool_scale_t", [128, NPG])
    wattn_d = din("w_attn_br", [D, D])
    wpool_d = din("w_pool_br", [512, D])
    wout_d = din("w_out", [D, D])
    g2_d = din("norm2_g", [1, D])
    wr_d = din("w_router", [D, 20])
    br_d = din("b_router", [1, 20])
    weg_d = din("w_e_gate", [NE, D, DE])
    weu_d = din("w_e_up", [NE, D, DE])
    wed_d = din("w_e_down", [NE, DE, D])
    gf_d = din("final_g", [1, D])
    cos_d = din("cos_t", [128, TC])
    sin_d = din("sin_t", [128, TC])
    ident_d = din("c_ident", [128, 128])
    perm_d = din("c_perm", [128, 128])
    tri_d = din("c_tri", [128, 256])
    ones_d = din("c_ones", [128, 128])
    ones16_d = din("c_ones16", [128, 128])
    out_d = nc.dram_tensor("out", [SEQ, D], F32, kind="ExternalOutput").ap()
    dbg_d = {}

    def dbg_out(name, shape):
        dbg_d[name] = nc.dram_tensor("dbg_" + name, list(shape), F32, kind="ExternalOutput").ap()
        return dbg_d[name]

    S = Sched()
    phases = ["A", "B0", "B1", "D", "E1", "E2", "F", "G"]
    last_phase = stop_after or "G"

    def active(ph):
        return phases.index(ph) <= phases.index(last_phase)

    with contextlib.ExitStack() as st:
        def sb(name, shape, dt):
            return st.enter_context(nc.sbuf_tensor(name, list(shape), dt))

        R1 = sb("R1", [128, 34816 // 4], F32)
        R42 = sb("R42", [128, 67584 // 4], F32)
        R35 = sb("R35", [128, 51200 // 4], F32)
        RT_BYTES = 50 * 1024
        RT = sb("RT", [128, RT_BYTES // 4], F32)

        def view(region, off_bytes, shape, dt):
            n = 1
            for s_ in shape:
                n *= s_
            if dt == F32:
                assert off_bytes % 4 == 0
                a = region[:, off_bytes // 4: off_bytes // 4 + n]
            else:
                assert off_bytes % 4 == 0 and (n * 2) % 4 == 0
                a = region[:, off_bytes // 4: off_bytes // 4 + (n * 2) // 4].bitcast(BF16)
            if len(shape) == 2:
                a = a.rearrange("p (a b) -> p a b", a=shape[0])
            elif len(shape) == 3:
                a = a.rearrange("p (a b c) -> p a b c", a=shape[0], b=shape[1])
            return a

        class RTAlloc:
            def __init__(self):
                self.off = 0

            def reset(self):
                self.off = 0

            def get(self, shape, dt):
                n = 1
                for s_ in shape:
                    n *= s_
                nb = n * (4 if dt == F32 else 2)
                nb = (nb + 31) // 32 * 32
                v = view(RT, self.off, shape, dt)
                self.off += nb
                assert self.off <= RT_BYTES, ("RT overflow", self.off)
                return v

        rt = RTAlloc()
        _tail = RT_BYTES - (1280 + 4096 + 1024)
        rl = view(RT, _tail, [16, 20], F32)
        rw = view(RT, _tail + 1280, [16, 64], F32)
        comb = view(RT, _tail + 1280 + 4096, [16, 16], F32)

        hnT = view(R1, 0, [8, TC], BF16)
        hn2T = view(R1, 0, [8, SEQ], BF16)
        Vt = view(R42, 0, [17, D], BF16)
        QT = view(R42, 34816, [8, SEQ], BF16)
        h1 = view(R42, 0, [16, D], F32)
        KT = view(R35, 0, [8, TC], BF16)
        mixT = view(R35, 0, [8, SEQ], BF16)
        PT = view(R35, 34816, [NPG, SEQ], BF16)

        ident = sb("ident", [128, 128], BF16)
        permm = sb("permm", [128, 128], BF16)
        tri = sb("tri", [128, 2, 128], BF16)
        ones = sb("ones", [128, 128], BF16)
        ones16 = sb("ones16", [128, 128], BF16)
        poolw = sb("poolw", [128, NPG, 128], BF16)
        bgate = sb("bgate", [128, 16], F32)
        pscale = sb("pscale", [128, NPG], F32)
        subg = sb("subg", [128, 1], F32)
        lamt = sb("lamt", [128, 4 * HD], F32)
        lams = sb("lams", [128, 8], F32)
        stats = sb("stats", [128, 17, 2], F32)
        stats2 = sb("stats2", [128, 16, 2], F32)
        stats3 = sb("stats3", [128, 16, 2], F32)
        wr_sb = sb("wr_sb", [128, 8, 20], BF16)
        br_sb = sb("br_sb", [128, 20], F32)

        _pb = [st.enter_context(nc.psum_tensor(f"pb{i}", [128, 512], F32)) for i in range(4)]
        pS = [st.enter_context(nc.psum_tensor(f"pS{i}", [128, 1024], F32)) for i in range(2)]
        pbanks = [t[:, :] for t in _pb] + [pS[0][:, 0:512], pS[0][:, 512:1024], pS[1][:, 0:512], pS[1][:, 512:1024]]

        def pbf(i):
            return pbanks[i].bitcast(BF16)

        cres = lambda t: ("c", t.name)
        for t, d_ in ((ident, ident_d), (permm, perm_d), (ones, ones_d), (ones16, ones16_d)):
            S.dma("pool", "constP", lambda e, t=t, d_=d_: e.dma_start(out=t[:, :], in_=d_), writes=[cres(t)])
        S.dma("pool", "constP", lambda e: e.dma_start(out=tri[:, :, :], in_=tri_d.rearrange("p (a b) -> p a b", a=2)), writes=[cres(tri)])
        S.dma("pool", "constP", lambda e: e.dma_start(out=poolw[:, :, :], in_=poolw_d.rearrange("g c d -> c g d")), writes=[cres(poolw)])
        S.dma("pool", "constP", lambda e: e.dma_start(out=wr_sb[:, :, :], in_=wr_d.rearrange("(c p) n -> p c n", p=128)), writes=[cres(wr_sb)])
        for t, d_ in ((bgate, bgate_d), (pscale, pscale_d), (subg, subg_d)):
            S.dma("sp", "constS", lambda e, t=t, d_=d_: e.dma_start(out=t[:, :], in_=d_), writes=[cres(t)])
        S.dma("sp", "constS", lambda e: e.dma_start(out=br_sb[:, :], in_=br_d.to_broadcast([128, 20])), writes=[cres(br_sb)])
        for i, d_ in enumerate((lq1_d, lk1_d, lq2_d, lk2_d)):
            S.dma("sp", "constS", lambda e, i=i, d_=d_: e.dma_start(out=lamt[:, i * HD:(i + 1) * HD], in_=d_.to_broadcast([128, HD])), writes=[("lamt", i)])

        S.op("dve", lambda e: e.tensor_tensor(out=lamt[:, 0:HD], in0=lamt[:, 0:HD], in1=lamt[:, HD:2 * HD], op=ALU.mult),
             reads=[("lamt", 0), ("lamt", 1)], writes=[("lamt", 0)])
        S.op("dve", lambda e: e.tensor_tensor(out=lamt[:, 2 * HD:3 * HD], in0=lamt[:, 2 * HD:3 * HD], in1=lamt[:, 3 * HD:4 * HD], op=ALU.mult),
             reads=[("lamt", 2), ("lamt", 3)], writes=[("lamt", 2)])
        S.op("dve", lambda e: e.reduce_sum(out=lams[:, 0:1], in_=lamt[:, 0:HD], axis=AX.X), reads=[("lamt", 0)], writes=[("lams", 0)])
        S.op("dve", lambda e: e.reduce_sum(out=lams[:, 1:2], in_=lamt[:, 2 * HD:3 * HD], axis=AX.X), reads=[("lamt", 2)], writes=[("lams", 1)])
        S.op("act", lambda e: e.activation(out=lams[:, 2:4], in_=lams[:, 0:2], func=AF.Exp), reads=[("lams", 0), ("lams", 1)], writes=[("lams", 2)])
        S.op("dve", lambda e: e.scalar_tensor_tensor(out=lams[:, 4:5], in0=lams[:, 3:4], scalar=-LAMBDA_INIT, in1=lams[:, 2:3],
                                                     op0=ALU.add, op1=ALU.subtract), reads=[("lams", 2)], writes=[("lams", 4)])
        S.op("dve", lambda e: e.tensor_scalar(out=lams[:, 5:6], in0=subg[:, 0:1], scalar1=1.0 - LAMBDA_INIT, scalar2=None, op0=ALU.mult),
             reads=[cres(subg)], writes=[("lams", 5)])
        neglam = lams[:, 4:5]
        gsub = lams[:, 5:6]

        def PS(i):
            return ("ps", i)

        rt.reset()
        wblk = [rt.get([8, 512], BF16) for _ in range(2)]
        S.dma("pool", ("wld", 0), lambda e: e.dma_start(out=wblk[0], in_=w_in_d[:, O_P:O_P + 512].rearrange("(c p) n -> p c n", p=128)), writes=[("wblk", 0)])
        S.dma("pool", ("wld", 1), lambda e: e.dma_start(out=wblk[1], in_=w_in_d[:, O_Q:O_Q + 512].rearrange("(c p) n -> p c n", p=128)), writes=[("wblk", 1)])
        g1bc = rt.get([D], F32)
        NBA = 4
        xt = [rt.get([D], F32) for _ in range(NBA)]
        junk = rt.get([D], BF16)
        xn = [rt.get([D], BF16) for _ in range(NBA)]
        S.dma("sp", "miscS", lambda e: e.dma_start(out=g1bc, in_=g1_d.to_broadcast([128, D])), writes=["g1bc"])
        S.op("pool", lambda e: e.memset(xt[0], 0.0), writes=[("xt", 0)])
        def a_s1(i):
            b = i % NBA
            if i == 0:
                S.dma("sp", ("xt", 0), lambda e: e.dma_start(out=xt[0][MOFF:128, :], in_=meta_d), writes=[("xt", 0)], reads=[])
            else:
                S.dma("sp", ("xt", b), lambda e: e.dma_start(out=xt[b], in_=x_d[(i - 1) * 128:i * 128, :]), writes=[("xt", b)])
            S.op("act", lambda e: e.activation(out=junk, in_=xt[b], func=AF.Square, accum_out=stats[:, i, 0:1]),
                 reads=[("xt", b)], writes=["junk", ("st", i)])
            S.op("act", lambda e: e.activation(out=stats[:, i, 1:2], in_=stats[:, i, 0:1], func=AF.Sqrt, scale=1.0 / D, bias=EPS),
                 reads=[("st", i)], writes=[("st1", i)])
            S.op("dve", lambda e: e.reciprocal(out=stats[:, i, 1:2], in_=stats[:, i, 1:2]), reads=[("st1", i)], writes=[("st1", i)])
            S.op("dve", lambda e: e.scalar_tensor_tensor(out=xn[b], in0=xt[b], scalar=stats[:, i, 1:2], in1=g1bc, op0=ALU.mult, op1=ALU.mult),
                 reads=[("xt", b), ("st1", i), "g1bc"], writes=[("xn", b)])

        def a_s2(i):
            b = i % NBA
            pbk = i % 4
            for c in range(8):
                S.op("pe", lambda e, c=c: e.transpose(out=pbf(pbk)[:, c * 128:(c + 1) * 128], in_=xn[b][:, c * 128:(c + 1) * 128], identity=ident[:, :]),
                     reads=[("xn", b), cres(ident)], writes=[PS(pbk)])
            S.op("act", lambda e: e.copy(out=hnT[:, :, i * 128:(i + 1) * 128], in_=pbf(pbk).rearrange("p (c n) -> p c n", c=8)),
                 reads=[PS(pbk)], writes=[("hnT", i)])

        for it_ in range(17 + 2):
            if it_ < 17:
                a_s1(it_)
            if 0 <= it_ - 2 < 17:
                a_s2(it_ - 2)
        HNT_ALL = [("hnT", i) for i in range(17)]

        if "hnT" in dbg:
            o = dbg_out("hnT", [128, 8 * TC])
            S.dma("pool", "dbgP", lambda e, o=o: e.dma_start(out=o.rearrange("p (c t) -> p c t", c=8), in_=hnT), reads=HNT_ALL, writes=["dbg_hnT"])

        def load_wblock(dst, src2d, c0, ncols, res, kch=8):
            S.dma("pool", ("wld", res[1]), lambda e: e.dma_start(out=dst, in_=src2d[:, c0:c0 + ncols].rearrange("(c p) n -> p c n", p=128)),
                  writes=[res])

        if active("B0"):
            S.barrier()
            rt.reset()
            wblk = [rt.get([8, 512], BF16) for _ in range(2)]
            u32 = [rt.get([TC], F32) for _ in range(2)]
            tmpA = rt.get([TC], F32)
            tmpB = rt.get([TC], F32)
            psrot = [0]

            def nextbank(lo, hi):
                b_ = lo + psrot[0] % (hi - lo)
                psrot[0] += 1
                return b_

            for g in range(NPG):
                ub = u32[g % 2]
                ures = ("u32", g % 2)
                for jt in range(5):
                    c0, n = (0, 128) if jt == 0 else (ROFF + (jt - 1) * 512, 512)
                    bk = nextbank(0, 4)
                    for c in range(8):
                        S.op("pe", lambda e, c=c, g=g, c0=c0, n=n, bk=bk: e.matmul(pbanks[bk][:, 0:n], lhsT=wblk[0][:, c, g * 128:(g + 1) * 128],
                                                                                 rhs=hnT[:, c, c0:c0 + n], start=(c == 0), stop=(c == 7)),
                             reads=[("wblk", 0)] + HNT_ALL, writes=[PS(bk)])
                    S.op("act", lambda e, ub=ub, c0=c0, n=n, bk=bk: e.copy(out=ub[:, c0:c0 + n], in_=pbanks[bk][:, 0:n]),
                         reads=[PS(bk)], writes=[ures])
                w = POOL_W[g]
                L = TC
                cur, cur_res = ub, ures
                step = 1
                bufs = [(tmpA, "tmpA"), (tmpB, "tmpB")]
                k = 0
                while step < w:
                    dst, dres = bufs[k % 2]
                    lo = 2 * step - 1
                    S.op("dve", lambda e, dst=dst, cur=cur, lo=lo, step=step: e.tensor_tensor(out=dst[:, lo:L], in0=cur[:, lo:L], in1=cur[:, lo - step:L - step], op=ALU.add),
                         reads=[cur_res], writes=[dres])
                    cur, cur_res = dst, dres
                    step *= 2
                    k += 1
                ydst, yres = bufs[k % 2]
                ybf = ydst[:, 0:SEQ // 2].bitcast(BF16)
                S.op("dve", lambda e, ybf=ybf, cur=cur, ub=ub, w=w: e.scalar_tensor_tensor(out=ybf, in0=cur[:, ROFF:TC], scalar=1.0 / w, in1=ub[:, ROFF:TC],
                                                                                            op0=ALU.mult, op1=ALU.subtract),
                     reads=[cur_res, ures], writes=[yres])
                for jt in range(4):
                    bk = nextbank(0, 4)
                    S.op("pe", lambda e, g=g, jt=jt, bk=bk, ybf=ybf: e.matmul(pbanks[bk][:, :], lhsT=poolw[:, g, :], rhs=ybf[:, jt * 512:(jt + 1) * 512], start=True, stop=True),
                         reads=[yres, cres(poolw)], writes=[PS(bk)])
                    S.op("act", lambda e, g=g, jt=jt, bk=bk: e.activation(out=PT[:, g, jt * 512:(jt + 1) * 512], in_=pbanks[bk][:, :], func=AF.Identity, scale=pscale[:, g:g + 1]),
                         reads=[PS(bk), cres(pscale)], writes=[("PT", g)])
            if "PT" in dbg:
                o = dbg_out("PT", [128, NPG * SEQ])
                S.dma("pool", "dbgP", lambda e, o=o: e.dma_start(out=o.rearrange("p (c t) -> p c t", c=NPG), in_=PT), reads=[("PT", g) for g in range(NPG)], writes=["dbg_PT"])

        if active("B1"):
            S.barrier()
            rt.reset()
            wblk = [rt.get([8, 512], BF16) for _ in range(2)]
            cosT = rt.get([TC], F32)
            sinT = rt.get([TC], F32)
            zb = [rt.get([512], BF16) for _ in range(2)]
            t1 = [rt.get([512], F32) for _ in range(2)]
            t2 = [rt.get([512], F32) for _ in range(2)]
            S.dma("sp", "miscS", lambda e: e.dma_start(out=cosT, in_=cos_d), writes=["cosT"])
            S.dma("sp", "miscS", lambda e: e.dma_start(out=sinT, in_=sin_d), writes=["sinT"])
            blocks = [("q", 0), ("q", 1), ("k", 0), ("k", 1), ("v", 0), ("v", 1)]
            col_of = {"q": O_Q, "k": O_K, "v": O_V}
            items = []

            def stage1(it):
                kind, h, c0, n, wb, nchunk, slot = it
                bk = slot % 2
                for c in range(8):
                    S.op("pe", lambda e, c=c: e.matmul(pbanks[bk][:, 0:n], lhsT=wblk[wb][:, c, nchunk * 128:(nchunk + 1) * 128], rhs=hnT[:, c, c0:c0 + n],
                                                       start=(c == 0), stop=(c == 7)),
                         reads=[("wblk", wb)] + HNT_ALL, writes=[PS(bk)])
                S.op("act", lambda e: e.copy(out=zb[bk][:, 0:n], in_=pbanks[bk][:, 0:n]), reads=[PS(bk)], writes=[("zb", bk)])

            def stage2(it):
                kind, h, c0, n, wb, nchunk, slot = it
                bk = slot % 2
                rb = 2 + slot % 2
                S.op("pe", lambda e: e.matmul(pbanks[rb][:, 0:n], lhsT=permm[:, :], rhs=zb[bk][:, 0:n], start=True, stop=True),
                     reads=[("zb", bk), cres(permm)], writes=[PS(rb)])
                S.op("dve", lambda e: e.tensor_tensor(out=t1[bk][:, 0:n], in0=pbanks[bk][:, 0:n], in1=cosT[:, c0:c0 + n], op=ALU.mult),
                     reads=[PS(bk), "cosT"], writes=[("t1", bk)])
                S.op("dve", lambda e: e.tensor_tensor(out=t2[bk][:, 0:n], in0=pbanks[rb][:, 0:n], in1=sinT[:, c0:c0 + n], op=ALU.mult),
                     reads=[PS(rb), "sinT"], writes=[("t2", bk)])
                if kind == "q":
                    dst = QT[:, h, c0 - ROFF:c0 - ROFF + n]
                    dres = ("QT", h, (c0 - ROFF) // 512)
                else:
                    dst = KT[:, h, c0:c0 + n]
                    dres = ("KT", h)
                S.op("pool", lambda e: e.tensor_tensor(out=dst, in0=t1[bk][:, 0:n], in1=t2[bk][:, 0:n], op=ALU.add),
                     reads=[("t1", bk), ("t2", bk)], writes=[dres])

            slot = 0
            prev = None
            for bi, (kind, half) in enumerate(blocks):
                wb = (bi + 1) % 2
                if bi > 0:
                    load_wblock(wblk[wb], w_in_d, col_of[kind] + half * 512, 512, ("wblk", wb))
                if kind in ("q", "k"):
                    for nchunk in range(4):
                        h = half * 4 + nchunk
                        tiles = ([(0, 128)] if kind == "k" else []) + [(ROFF + jt * 512, 512) for jt in range(4)]
                        for (c0, n) in tiles:
                            it = (kind, h, c0, n, wb, nchunk, slot)
                            slot += 1
                            stage1(it)
                            if prev is not None:
                                stage2(prev)
                            prev = it
                else:
                    if prev is not None:
                        stage2(prev)
                        prev = None
                    for i in range(17):
                        for sub in range(1):
                            bk = 4 + (i % 2)
                            for c in range(8):
                                S.op("pe", lambda e, c=c, i=i, bk=bk, wb=wb: e.matmul(pbanks[bk][:, :], lhsT=hnT[:, c, i * 128:(i + 1) * 128], rhs=wblk[wb][:, c, :],
                                                                                     start=(c == 0), stop=(c == 7)),
                                     reads=[("wblk", wb), ("hnT", i)], writes=[PS(bk)])
                            S.op("act", lambda e, i=i, bk=bk, half=half: e.copy(out=Vt[:, i, half * 512:(half + 1) * 512], in_=pbanks[bk][:, :]),
                                 reads=[PS(bk)], writes=[("V", i)])
            if prev is not None:
                stage2(prev)
                prev = None
            for nm, tns, shp, res in (("QT", QT, [128, 8 * SEQ], [("QT", h, j) for h in range(8) for j in range(4)]),
                                      ("KT", KT, [128, 8 * TC], [("KT", h) for h in range(8)]),
                                      ("V", Vt, [128, 17 * D], [("V", i) for i in range(17)])):
                if nm in dbg:
                    o = dbg_out(nm, shp)
                    S.dma("pool", "dbgP", lambda e, o=o, tns=tns: e.dma_start(out=o.rearrange("p (c t) -> p c t", c=tns.shape[1]), in_=tns), reads=res, writes=["dbg_" + nm])

        if active("D"):
            S.barrier()
            rt.reset()
            QZ = [[rt.get([SEQ], BF16) for c in range(2)] for _ in range(2)]
            pTb = [rt.get([2, 512], BF16) for _ in range(3)]
            r2 = rt.get([2, 512], F32)
            o_t = [rt.get([512], F32) for _ in range(2)]
            a_t = rt.get([512], F32)
            sq_t = rt.get([512], BF16)
            ln_t = rt.get([512], F32)
            for par in range(2):
                S.op("pool", lambda e, par=par: e.memset(QZ[par][0], 0.0), writes=[("QZ", par, 0)])
                S.op("pool", lambda e, par=par: e.memset(QZ[par][1], 0.0), writes=[("QZ", par, 1)])
            ACC_O = (0, 2)
            ACC_S = (1, 3)
            SCALE = 1.0 / math.sqrt(HD)
            cnts = {"s": 0, "pt": 0}

            def build_qz(h):
                par = h % 2
                S.op("dve", lambda e: e.tensor_copy(out=QZ[par][0][0:64, :], in_=QT[0:64, h, :]),
                     reads=[("QT", h, j) for j in range(4)], writes=[("QZ", par, 0)])
                S.op("dve", lambda e: e.tensor_copy(out=QZ[par][1][64:128, :], in_=QT[64:128, h, :]),
                     reads=[("QT", h, j) for j in range(4)], writes=[("QZ", par, 1)])

            items = []
            prev_hj = None
            for h in range(NH):
                for j in range(4):
                    kts = [(0, 0, 0, False)]
                    for i in range(4 * j):
                        kts.append((1 + i, ROFF + i * 128, 0, False))
                    for r in range(4):
                        i = 4 * j + r
                        kts.append((1 + i, ROFF + i * 128, 128 * r, True))
                    for idx, kt in enumerate(kts):
                        items.append(("kt", h, j, kt, idx == 0, idx == len(kts) - 1))
                        if idx == min(5, len(kts) - 2) and prev_hj is not None:
                            items.append(("norm",) + prev_hj)
                    prev_hj = (h, j)
            final_norm = ("norm",) + prev_hj

            def stage_s(it):
                p = cnts["s"] % 2
                cnts["s"] += 1
                if it[0] == "norm":
                    _, h, j = it
                    S.op("pe", lambda e: e.matmul(pS[p][:, 0:512], lhsT=ones[:, :], rhs=sq_t, start=True, stop=True),
                         reads=["sq_t", cres(ones)], writes=[PS(4 + 2 * p)])
                    S.op("act", lambda e: e.activation(out=ln_t, in_=pS[p][:, 0:512], func=AF.Ln, scale=1.0 / VD, bias=EPS),
                         reads=[PS(4 + 2 * p)], writes=["ln_t"])
                    S.op("act", lambda e: e.activation(out=ln_t, in_=ln_t, func=AF.Exp, scale=-0.5), reads=["ln_t"], writes=["ln_t"])
                    return None
                _, h, j, (vt_i, kc0, q0, diag), first, last = it
                par = h % 2
                if first and j == 0 and h == 0:
                    build_qz(0)
                if first and j == 2 and h + 1 < NH:
                    build_qz(h + 1)
                pb_i = cnts["pt"] % 3
                cnts["pt"] += 1
                for c in range(2):
                    S.op("pe", lambda e, c=c: e.matmul(pS[p][:, c * 512 + q0:(c + 1) * 512], lhsT=KT[:, h, kc0:kc0 + 128],
                                                       rhs=QZ[par][c][:, j * 512 + q0:(j + 1) * 512], start=True, stop=True),
                         reads=[("KT", h), ("QZ", par, c)], writes=[PS(4 + 2 * p + c)])
                S.op("act", lambda e: e.activation(out=pTb[pb_i][:, :, q0:512], in_=pS[p][:, :].rearrange("p (c n) -> p c n", c=2)[:, :, q0:512],
                                                   func=AF.Exp, scale=SCALE),
                     reads=[PS(4 + 2 * p), PS(4 + 2 * p + 1)], writes=[("pT", pb_i)])
                if diag:
                    S.op("pool", lambda e: e.tensor_tensor(out=pTb[pb_i][:, :, q0:q0 + 128], in0=pTb[pb_i][:, :, q0:q0 + 128], in1=tri[:, :, :], op=ALU.mult),
                         reads=[("pT", pb_i), cres(tri)], writes=[("pT", pb_i)])
                return pb_i

            def stage_av(it, pb_i):
                if it[0] == "norm":
                    _, h, j = it
                    S.op("dve", lambda e: e.scalar_tensor_tensor(out=QT[:, h, j * 512:(j + 1) * 512], in0=a_t, scalar=gsub, in1=ln_t, op0=ALU.mult, op1=ALU.mult),
                         reads=["a_t", "ln_t", ("lams", 5)], writes=[("QT", h, j)])
                    return
                _, h, j, (vt_i, kc0, q0, diag), first, last = it
                on = ones16 if vt_i == 0 else ones
                for c in range(2):
                    S.op("pe", lambda e, c=c: e.matmul(pbanks[ACC_O[c]][:, q0:512], lhsT=Vt[:, vt_i, h * 128:(h + 1) * 128], rhs=pTb[pb_i][:, c, q0:512],
                                                       start=first, stop=last),
                         reads=[("V", vt_i), ("pT", pb_i)], writes=[PS(ACC_O[c])])
                    S.op("pe", lambda e, c=c: e.matmul(pbanks[ACC_S[c]][:, q0:512], lhsT=on[:, :], rhs=pTb[pb_i][:, c, q0:512],
                                                       start=first, stop=last),
                         reads=[cres(on), ("pT", pb_i)], writes=[PS(ACC_S[c])])
                if last:
                    for c in range(2):
                        S.op("act", lambda e, c=c: e.activation(out=r2[:, c, :], in_=pbanks[ACC_S[c]], func=AF.Ln), reads=[PS(ACC_S[c])], writes=["r2"])
                        S.op("dve", lambda e, c=c: e.tensor_copy(out=o_t[c], in_=pbanks[ACC_O[c]]), reads=[PS(ACC_O[c])], writes=[("o_t", c)])
                    S.op("act", lambda e: e.activation(out=r2[:, :, :], in_=r2[:, :, :], func=AF.Exp, scale=-1.0), reads=["r2"], writes=["r2"])
                    for c in range(2):
                        S.op("dve", lambda e, c=c: e.tensor_tensor(out=o_t[c], in0=o_t[c], in1=r2[:, c, :], op=ALU.mult),
                             reads=[("o_t", c), "r2"], writes=[("o_t", c)])
                    S.op("dve", lambda e: e.scalar_tensor_tensor(out=a_t, in0=o_t[1], scalar=neglam, in1=o_t[0], op0=ALU.mult, op1=ALU.add),
                         reads=[("o_t", 0), ("o_t", 1), ("lams", 4)], writes=["a_t"])
                    S.op("pool", lambda e: e.tensor_tensor(out=sq_t, in0=a_t, in1=a_t, op=ALU.mult), reads=["a_t"], writes=["sq_t"])

            prev_it, prev_pb = None, None
            for it in items + [None]:
                pb_new = stage_s(it) if it is not None else None
                if prev_it is not None:
                    stage_av(prev_it, prev_pb)
                prev_it, prev_pb = it, pb_new
            stage_s(final_norm)
            stage_av(final_norm, None)
            if "AT" in dbg:
                o = dbg_out("AT", [128, 8 * SEQ])
                S.dma("pool", "dbgP", lambda e, o=o: e.dma_start(out=o.rearrange("p (c t) -> p c t", c=8), in_=QT), reads=[("QT", h, j) for h in range(8) for j in range(4)], writes=["dbg_AT"])

        if active("E1"):
            S.barrier()
            rt.reset()
            wout_sb = rt.get([8, D], BF16)
            wa = [rt.get([8, 128], BF16) for _ in range(2)]
            wp = [rt.get([4, 128], BF16) for _ in range(2)]
            wg0 = [rt.get([8, 128], BF16) for _ in range(2)]
            wg1 = [rt.get([8, 128], BF16) for _ in range(2)]
            gt = [[rt.get([512], F32) for _ in range(2)] for _ in range(2)]
            mt = [[rt.get([512], F32) for _ in range(2)] for _ in range(2)]
            ATR = [("QT", h, j) for h in range(8) for j in range(4)]

            def load_e1(dc):
                b = dc % 2
                S.dma("pool", ("e1w", b), lambda e: e.dma_start(out=wa[b], in_=wattn_d[:, dc * 128:(dc + 1) * 128].rearrange("(c p) n -> p c n", p=128)), writes=[("wa", b)])
                S.dma("pool", ("e1w", b), lambda e: e.dma_start(out=wp[b], in_=wpool_d[:, dc * 128:(dc + 1) * 128].rearrange("(c p) n -> p c n", p=128)), writes=[("wp", b)])
                S.dma("pool", ("e1w", b), lambda e: e.dma_start(out=wg0[b], in_=w_in_d[:, O_G + dc * 128:O_G + (dc + 1) * 128].rearrange("(c p) n -> p c n", p=128)), writes=[("wg0", b)])
                S.dma("pool", ("e1w", b), lambda e: e.dma_start(out=wg1[b], in_=w_in_d[:, O_G + D + dc * 128:O_G + D + (dc + 1) * 128].rearrange("(c p) n -> p c n", p=128)), writes=[("wg1", b)])

            def e1_tile(dc, j, b, pr):
                cs = slice(ROFF + j * 512, ROFF + (j + 1) * 512)
                qs = slice(j * 512, (j + 1) * 512)
                bk_g0, bk_a, bk_g1, bk_p = (0 + pr, 2 + pr, 4 + pr, 6 + pr)
                for c in range(8):
                    S.op("pe", lambda e, c=c: e.matmul(pbanks[bk_g0][:, :], lhsT=wg0[b][:, c, :], rhs=hnT[:, c, cs], start=(c == 0), stop=(c == 7)),
                         reads=[("wg0", b)] + HNT_ALL, writes=[PS(bk_g0)])
                S.op("act", lambda e: e.activation(out=gt[0][pr], in_=pbanks[bk_g0][:, :], func=AF.Sigmoid, bias=bgate[:, dc:dc + 1]),
                     reads=[PS(bk_g0), cres(bgate)], writes=[("gt", 0, pr)])
                for c in range(8):
                    S.op("pe", lambda e, c=c: e.matmul(pbanks[bk_a][:, :], lhsT=wa[b][:, c, :], rhs=QT[:, c, qs], start=(c == 0), stop=(c == 7)),
                         reads=[("wa", b)] + ATR, writes=[PS(bk_a)])
                S.op("dve", lambda e: e.tensor_tensor(out=mt[0][pr], in0=pbanks[bk_a][:, :], in1=gt[0][pr], op=ALU.mult),
                     reads=[PS(bk_a), ("gt", 0, pr)], writes=[("mt", 0, pr)])
                for c in range(8):
                    S.op("pe", lambda e, c=c: e.matmul(pbanks[bk_g1][:, :], lhsT=wg1[b][:, c, :], rhs=hnT[:, c, cs], start=(c == 0), stop=(c == 7)),
                         reads=[("wg1", b)] + HNT_ALL, writes=[PS(bk_g1)])
                S.op("act", lambda e: e.activation(out=gt[1][pr], in_=pbanks[bk_g1][:, :], func=AF.Sigmoid, bias=bgate[:, 8 + dc:9 + dc]),
                     reads=[PS(bk_g1), cres(bgate)], writes=[("gt", 1, pr)])
                for c in range(4):
                    S.op("pe", lambda e, c=c: e.matmul(pbanks[bk_p][:, :], lhsT=wp[b][:, c, :], rhs=PT[:, c, qs], start=(c == 0), stop=(c == 3)),
                         reads=[("wp", b)] + [("PT", g) for g in range(NPG)], writes=[PS(bk_p)])
                S.op("dve", lambda e: e.tensor_tensor(out=mt[1][pr], in0=pbanks[bk_p][:, :], in1=gt[1][pr], op=ALU.mult),
                     reads=[PS(bk_p), ("gt", 1, pr)], writes=[("mt", 1, pr)])
                S.op("pool", lambda e: e.tensor_tensor(out=mixT[:, dc, qs], in0=mt[0][pr], in1=mt[1][pr], op=ALU.add),
                     reads=[("mt", 0, pr), ("mt", 1, pr)], writes=[("mixT", j)])

            load_e1(0)
            cnt = 0
            for dc in range(8):
                b = dc % 2
                if dc + 1 < 8:
                    load_e1(dc + 1)
                if dc == 1:
                    S.dma("pool", "miscP", lambda e: e.dma_start(out=wout_sb, in_=wout_d.rearrange("(c p) n -> p c n", p=128)), writes=["wout"])
                for j in range(4):
                    e1_tile(dc, j, b, cnt % 2)
                    cnt += 1
            if "mixT" in dbg:
                o = dbg_out("mixT", [128, 8 * SEQ])
                S.dma("pool", "dbgP", lambda e, o=o: e.dma_start(out=o.rearrange("p (c t) -> p c t", c=8), in_=mixT), reads=[("mixT", j) for j in range(4)], writes=["dbg_mixT"])

        if active("E2"):
            S.barrier()
            rt.reset()
            wout_sb = rt.get([8, D], BF16)
            x2 = [rt.get([D], F32) for _ in range(2)]
            g2bc = rt.get([D], F32)
            junk2 = rt.get([D], BF16)
            hn2 = [rt.get([D], BF16) for _ in range(2)]
            S.dma("sp", "miscS", lambda e: e.dma_start(out=g2bc, in_=g2_d.to_broadcast([128, D])), writes=["g2bc"])
            MIXR = [("mixT", j) for j in range(4)]
            def e2_s1(i):
                b = i % 2
                S.dma("sp", ("x2", b), lambda e: e.dma_start(out=x2[b], in_=x_d[i * 128:(i + 1) * 128, :]), writes=[("x2", b)])
                for half in range(2):
                    bk = (2 * i + half) % 4
                    for c in range(8):
                        S.op("pe", lambda e, c=c, half=half, bk=bk: e.matmul(pbanks[bk][:, :], lhsT=mixT[:, c, i * 128:(i + 1) * 128],
                                                                             rhs=wout_sb[:, c, half * 512:(half + 1) * 512], start=(c == 0), stop=(c == 7)),
                             reads=["wout"] + MIXR, writes=[PS(bk)])
                    S.op("dve", lambda e, half=half, bk=bk: e.tensor_tensor(out=h1[:, i, half * 512:(half + 1) * 512], in0=pbanks[bk][:, :],
                                                                            in1=x2[b][:, half * 512:(half + 1) * 512], op=ALU.add),
                         reads=[PS(bk), ("x2", b)], writes=[("h1", i)])
                S.op("act", lambda e: e.activation(out=junk2, in_=h1[:, i, :], func=AF.Square, accum_out=stats2[:, i, 0:1]),
                     reads=[("h1", i)], writes=["junk2", ("s2", i)])
                S.op("act", lambda e: e.activation(out=stats2[:, i, 1:2], in_=stats2[:, i, 0:1], func=AF.Sqrt, scale=1.0 / D, bias=EPS),
                     reads=[("s2", i)], writes=[("s21", i)])
                S.op("dve", lambda e: e.reciprocal(out=stats2[:, i, 1:2], in_=stats2[:, i, 1:2]), reads=[("s21", i)], writes=[("s21", i)])
                S.op("dve", lambda e: e.scalar_tensor_tensor(out=hn2[b], in0=h1[:, i, :], scalar=stats2[:, i, 1:2], in1=g2bc, op0=ALU.mult, op1=ALU.mult),
                     reads=[("h1", i), ("s21", i), "g2bc"], writes=[("hn2", b)])

            def e2_s2(i):
                b = i % 2
                pbk = 4 + i % 2
                for c in range(8):
                    S.op("pe", lambda e, c=c: e.transpose(out=pbf(pbk)[:, c * 128:(c + 1) * 128], in_=hn2[b][:, c * 128:(c + 1) * 128], identity=ident[:, :]),
                         reads=[("hn2", b), cres(ident)], writes=[PS(pbk)])
                S.op("act", lambda e: e.copy(out=hn2T[:, :, i * 128:(i + 1) * 128], in_=pbf(pbk).rearrange("p (c n) -> p c n", c=8)),
                     reads=[PS(pbk)], writes=[("hn2T", i)])

            def e2_s3(i):
                rbk = 6 + i % 2
                for c in range(8):
                    S.op("pe", lambda e, c=c: e.matmul(pbanks[rbk][:, 0:20], lhsT=hn2T[:, c, i * 128:(i + 1) * 128], rhs=wr_sb[:, c, :],
                                                       start=(c == 0), stop=(c == 7)),
                         reads=[("hn2T", i), cres(wr_sb)], writes=[PS(rbk)])
                S.op("dve", lambda e: e.tensor_tensor(out=rl[:, i, :], in0=pbanks[rbk][:, 0:20], in1=br_sb[:, :], op=ALU.add),
                     reads=[PS(rbk), cres(br_sb)], writes=[("rl", i)])

            for it_ in range(16 + 2):
                if it_ < 16:
                    e2_s1(it_)
                if 0 <= it_ - 1 < 16:
                    e2_s2(it_ - 1)
                if 0 <= it_ - 2 < 16:
                    e2_s3(it_ - 2)
            if "h1" in dbg:
                o = dbg_out("h1", [128, 16 * D])
                S.dma("sp", "dbgS", lambda e, o=o: e.dma_start(out=o.rearrange("p (c t) -> p c t", c=16), in_=h1), reads=[("h1", i) for i in range(16)], writes=["dbg_h1"])
            if "rl" in dbg:
                o = dbg_out("rl", [128, 16 * 20])
                S.dma("sp", "dbgS", lambda e, o=o: e.dma_start(out=o.rearrange("p (c t) -> p c t", c=16), in_=rl), reads=[("rl", i) for i in range(16)], writes=["dbg_rl"])

        if active("F"):
            S.barrier()
            rt.reset()
            RL = [("rl", i) for i in range(16)]
            glog = rl[:, :, 0:4]
            gmx = rw[:, :, 0:1]
            gex = rw[:, :, 4:8]
            gsum = rw[:, :, 8:9]
            goh = rw[:, :, 12:16]
            esel = rw[:, :, 16:20]
            etmp = rw[:, :, 20:24]
            m1 = rw[:, :, 24:25]
            oh1 = rw[:, :, 28:32]
            e2v = rw[:, :, 32:36]
            m2 = rw[:, :, 36:37]
            oh2 = rw[:, :, 40:44]
            dd = rw[:, :, 44:45]
            w1 = rw[:, :, 45:46]
            w2_ = rw[:, :, 46:47]
            c4 = rw[:, :, 48:52]
            RW = "rw"
            V_ = S.op
            V_("dve", lambda e: e.tensor_reduce(out=gmx, in_=glog, axis=AX.X, op=ALU.max), reads=RL, writes=[RW])
            V_("dve", lambda e: e.tensor_tensor(out=gex, in0=glog, in1=gmx.to_broadcast([128, 16, 4]), op=ALU.subtract), reads=RL + [RW], writes=[RW])
            V_("dve", lambda e: e.tensor_tensor(out=goh, in0=glog, in1=gmx.to_broadcast([128, 16, 4]), op=ALU.is_ge), reads=RL + [RW], writes=[RW])
            V_("act", lambda e: e.activation(out=gex, in_=gex, func=AF.Exp), reads=[RW], writes=[RW])
            V_("dve", lambda e: e.tensor_reduce(out=gsum, in_=gex, axis=AX.X, op=ALU.add), reads=[RW], writes=[RW])
            V_("dve", lambda e: e.reciprocal(out=gsum, in_=gsum), reads=[RW], writes=[RW])
            for g in range(4):
                if g == 0:
                    V_("dve", lambda e: e.tensor_tensor(out=esel, in0=rl[:, :, 4:8], in1=goh[:, :, 0:1].to_broadcast([128, 16, 4]), op=ALU.mult), reads=RL + [RW], writes=[RW])
                else:
                    V_("dve", lambda e, g=g: e.tensor_tensor(out=etmp, in0=rl[:, :, 4 + 4 * g:8 + 4 * g], in1=goh[:, :, g:g + 1].to_broadcast([128, 16, 4]), op=ALU.mult),
                       reads=RL + [RW], writes=[RW])
                    V_("dve", lambda e: e.tensor_tensor(out=esel, in0=esel, in1=etmp, op=ALU.add), reads=[RW], writes=[RW])
            V_("dve", lambda e: e.tensor_reduce(out=m1, in_=esel, axis=AX.X, op=ALU.max), reads=[RW], writes=[RW])
            V_("dve", lambda e: e.tensor_tensor(out=oh1, in0=esel, in1=m1.to_broadcast([128, 16, 4]), op=ALU.is_ge), reads=[RW], writes=[RW])
            V_("dve", lambda e: e.scalar_tensor_tensor(out=e2v, in0=oh1, scalar=-1e30, in1=esel, op0=ALU.mult, op1=ALU.add), reads=[RW], writes=[RW])
            V_("dve", lambda e: e.tensor_reduce(out=m2, in_=e2v, axis=AX.X, op=ALU.max), reads=[RW], writes=[RW])
            V_("dve", lambda e: e.tensor_tensor(out=oh2, in0=e2v, in1=m2.to_broadcast([128, 16, 4]), op=ALU.is_ge), reads=[RW], writes=[RW])
            V_("dve", lambda e: e.tensor_tensor(out=dd, in0=m2, in1=m1, op=ALU.subtract), reads=[RW], writes=[RW])
            V_("act", lambda e: e.activation(out=dd, in_=dd, func=AF.Exp), reads=[RW], writes=[RW])
            V_("dve", lambda e: e.tensor_scalar(out=w1, in0=dd, scalar1=1.0, scalar2=None, op0=ALU.add), reads=[RW], writes=[RW])
            V_("dve", lambda e: e.reciprocal(out=w1, in_=w1), reads=[RW], writes=[RW])
            V_("dve", lambda e: e.tensor_tensor(out=w1, in0=w1, in1=gsum, op=ALU.mult), reads=[RW], writes=[RW])
            V_("dve", lambda e: e.tensor_tensor(out=w2_, in0=w1, in1=dd, op=ALU.mult), reads=[RW], writes=[RW])
            V_("dve", lambda e: e.tensor_tensor(out=c4, in0=oh1, in1=w1.to_broadcast([128, 16, 4]), op=ALU.mult), reads=[RW], writes=[RW])
            V_("dve", lambda e: e.tensor_tensor(out=etmp, in0=oh2, in1=w2_.to_broadcast([128, 16, 4]), op=ALU.mult), reads=[RW], writes=[RW])
            V_("dve", lambda e: e.tensor_tensor(out=c4, in0=c4, in1=etmp, op=ALU.add), reads=[RW], writes=[RW])
            for g in range(4):
                V_("dve", lambda e, g=g: e.tensor_tensor(out=comb[:, :, 4 * g:4 * g + 4], in0=c4, in1=goh[:, :, g:g + 1].to_broadcast([128, 16, 4]), op=ALU.mult),
                   reads=[RW], writes=["comb"])
            if "comb" in dbg:
                o = dbg_out("comb", [128, 16 * 16])
                S.dma("sp", "dbgS", lambda e, o=o: e.dma_start(out=o.rearrange("p (c t) -> p c t", c=16), in_=comb), reads=["comb"], writes=["dbg_comb"])

            WB = 24576
            wgt = [view(R35, k * WB, [8, DE], BF16) for k in range(2)]
            wup = [view(R35, k * WB + 8192, [8, DE], BF16) for k in range(2)]
            wdn = [view(R35, k * WB + 16384, [4, D], BF16) for k in range(2)]
            sg = [rt.get([512], F32) for _ in range(2)]
            At = [rt.get([4, 512], BF16) for _ in range(2)]
            HN2R = [("hn2T", i) for i in range(16)]

            def load_expert(ex):
                b = ex % 2
                S.dma("pool", ("exw", b), lambda e: e.dma_start(out=wgt[b], in_=weg_d[ex].rearrange("(c p) n -> p c n", p=128)), writes=[("wgt", b)])
                S.dma("pool", ("exw", b), lambda e: e.dma_start(out=wup[b], in_=weu_d[ex].rearrange("(c p) n -> p c n", p=128)), writes=[("wup", b)])
                S.dma("pool", ("exw", b), lambda e: e.dma_start(out=wdn[b], in_=wed_d[ex].rearrange("(c p) n -> p c n", p=128)), writes=[("wdn", b)])

            do_g = active("G")
            if do_g:
                gfbc = rt.get([D], F32)
                junk3 = rt.get([D], BF16)
                ot = [rt.get([D], F32) for _ in range(2)]
                S.dma("sp", "miscS", lambda e: e.dma_start(out=gfbc, in_=gf_d.to_broadcast([128, D])), writes=["gfbc"])

            def g_tile(i):
                b = i % 2
                S.op("act", lambda e: e.activation(out=junk3, in_=h1[:, i, :], func=AF.Square, accum_out=stats3[:, i, 0:1]),
                     reads=[("h1", i)], writes=["junk3", ("s3", i)])
                S.op("act", lambda e: e.activation(out=stats3[:, i, 1:2], in_=stats3[:, i, 0:1], func=AF.Sqrt, scale=1.0 / D, bias=EPS),
                     reads=[("s3", i)], writes=[("s31", i)])
                S.op("dve", lambda e: e.reciprocal(out=stats3[:, i, 1:2], in_=stats3[:, i, 1:2]), reads=[("s31", i)], writes=[("s31", i)])
                S.op("dve", lambda e: e.scalar_tensor_tensor(out=ot[b], in0=h1[:, i, :], scalar=stats3[:, i, 1:2], in1=gfbc, op0=ALU.mult, op1=ALU.mult),
                     reads=[("h1", i), ("s31", i), "gfbc"], writes=[("ot", b)])
                S.dma("sp", ("st", b), lambda e: e.dma_start(out=out_d[i * 128:(i + 1) * 128, :], in_=ot[b]), reads=[("ot", b)], writes=[("out", i)])

            cnt_f = [0, 0]
            def f_tile(ex, j, b, ab):
                ts_ = slice(j * 512, (j + 1) * 512)
                for f in range(4):
                    pr = cnt_f[0] % 2
                    cnt_f[0] += 1
                    bg, bu = 0 + pr, 2 + pr
                    for c in range(8):
                        S.op("pe", lambda e, c=c, f=f, bg=bg: e.matmul(pbanks[bg][:, :], lhsT=wgt[b][:, c, f * 128:(f + 1) * 128], rhs=hn2T[:, c, ts_], start=(c == 0), stop=(c == 7)),
                             reads=[("wgt", b)] + HN2R, writes=[PS(bg)])
                    S.op("act", lambda e, pr=pr, bg=bg: e.activation(out=sg[pr], in_=pbanks[bg][:, :], func=AF.Silu), reads=[PS(bg)], writes=[("sg", pr)])
                    for c in range(8):
                        S.op("pe", lambda e, c=c, f=f, bu=bu: e.matmul(pbanks[bu][:, :], lhsT=wup[b][:, c, f * 128:(f + 1) * 128], rhs=hn2T[:, c, ts_], start=(c == 0), stop=(c == 7)),
                             reads=[("wup", b)] + HN2R, writes=[PS(bu)])
                    S.op("dve", lambda e, pr=pr, f=f, bu=bu: e.tensor_tensor(out=At[ab][:, f, :], in0=pbanks[bu][:, :], in1=sg[pr], op=ALU.mult),
                         reads=[PS(bu), ("sg", pr)], writes=[("At", ab)])
                for sub in range(4):
                    i = 4 * j + sub
                    for half in range(2):
                        bd = 4 + cnt_f[1] % 4
                        cnt_f[1] += 1
                        for f in range(4):
                            S.op("pe", lambda e, f=f, sub=sub, half=half, bd=bd: e.matmul(pbanks[bd][:, :], lhsT=At[ab][:, f, sub * 128:(sub + 1) * 128],
                                                                                          rhs=wdn[b][:, f, half * 512:(half + 1) * 512], start=(f == 0), stop=(f == 3)),
                                 reads=[("At", ab), ("wdn", b)], writes=[PS(bd)])
                        S.op("dve", lambda e, i=i, half=half, bd=bd, ex=ex: e.scalar_tensor_tensor(out=h1[:, i, half * 512:(half + 1) * 512], in0=pbanks[bd][:, :],
                                                                                                  scalar=comb[:, i, ex:ex + 1], in1=h1[:, i, half * 512:(half + 1) * 512],
                                                                                                  op0=ALU.mult, op1=ALU.add),
                             reads=[PS(bd), "comb", ("h1", i)], writes=[("h1", i)])
                    if do_g and ex == NE - 1:
                        g_tile(i)

            load_expert(0)
            cnt = 0
            acnt = 0
            dcnt = 0
            for ex in range(NE):
                b = ex % 2
                if ex + 1 < NE:
                    load_expert(ex + 1)
                for j in range(4):
                    f_tile(ex, j, b, acnt % 2)
                    acnt += 1
            if "h2" in dbg:
                o = dbg_out("h2", [128, 16 * D])
                S.dma("sp", "dbgS", lambda e, o=o: e.dma_start(out=o.rearrange("p (c t) -> p c t", c=16), in_=h1), reads=[("h1", i) for i in range(16)], writes=["dbg_h2"])

        if active("G") and not active("F"):
            raise RuntimeError("phase G is fused into phase F")

        S.finish("sp")
        nsig = S.emit(nc)
    return nc, dbg_d, nsig


def _consts():
    f32 = np.float32
    inv = (1.0 / (10000.0 ** (np.arange(0, HD, 2, dtype=f32) / f32(HD)))).astype(f32)
    cos_t = np.zeros((128, TC), f32)
    sin_t = np.zeros((128, TC), f32)
    pos = np.arange(SEQ + NMETA, dtype=f32)
    ang = (pos[:, None] * inv[None, :]).astype(f32)
    ang = np.concatenate([ang, ang], axis=-1)
    c = np.cos(ang).astype(f32).T
    s = np.sin(ang).astype(f32).T
    s_signed = s.copy()
    s_signed[:HD // 2] *= -1.0
    for half in range(2):
        cos_t[half * 64:(half + 1) * 64, MOFF:] = c
        sin_t[half * 64:(half + 1) * 64, MOFF:] = s_signed
    ident = np.eye(128, dtype=f32)
    perm = np.zeros((128, 128), f32)
    for n2 in range(128):
        blk, d = divmod(n2, 64)
        perm[blk * 64 + (d + 32) % 64, n2] = 1.0
    k_ = np.arange(128)[:, None]
    q_ = np.arange(128)[None, :]
    tri = (q_ >= k_).astype(f32)
    tri = np.concatenate([tri, tri], axis=1)
    ones = np.ones((128, 128), f32)
    ones16 = np.zeros((128, 128), f32)
    ones16[MOFF:, :] = 1.0
    return dict(cos_t=cos_t, sin_t=sin_t, c_ident=ident, c_perm=perm, c_tri=tri, c_ones=ones, c_ones16=ones16)


def make_in_maps(inputs, n_cores=8):
    f = lambda a: np.ascontiguousarray(np.asarray(a, dtype=np.float32))
    x = f(inputs["x"])
    shared = dict(
        meta=f(inputs["meta"]),
        norm1_g=f(inputs["norm1_g"]).reshape(1, D),
        w_in=f(inputs["w_in"]).reshape(D, IN_W),
        b_gate_t=np.ascontiguousarray(f(inputs["b_gate"]).reshape(16, 128).T),
        lambda_q1=f(inputs["lambda_q1"]).reshape(1, HD),
        lambda_k1=f(inputs["lambda_k1"]).reshape(1, HD),
        lambda_q2=f(inputs["lambda_q2"]).reshape(1, HD),
        lambda_k2=f(inputs["lambda_k2"]).reshape(1, HD),
        subln_g_t=f(inputs["subln_g"]).reshape(128, 1),
        pool_w=f(inputs["pool_w"]).reshape(NPG, 128, 128),
        pool_scale_t=np.ascontiguousarray(f(inputs["pool_scale"]).reshape(NPG, 128).T),
        w_attn_br=f(inputs["w_attn_br"]).reshape(D, D),
        w_pool_br=f(inputs["w_pool_br"]).reshape(512, D),
        w_out=f(inputs["w_out"]).reshape(D, D),
        norm2_g=f(inputs["norm2_g"]).reshape(1, D),
        w_router=np.ascontiguousarray(np.concatenate([f(inputs["w_router_group"]).reshape(D, 4), f(inputs["w_router_expert"]).reshape(D, 16)], axis=1)),
        b_router=np.ascontiguousarray(np.concatenate([f(inputs["b_router_group"]).reshape(1, 4), f(inputs["b_router_expert"]).reshape(1, 16)], axis=1)),
        w_e_gate=f(inputs["w_e_gate"]).reshape(NE, D, DE),
        w_e_up=f(inputs["w_e_up"]).reshape(NE, D, DE),
        w_e_down=f(inputs["w_e_down"]).reshape(NE, DE, D),
        final_g=f(inputs["final_g"]).reshape(1, D),
    )
    shared.update(_consts())
    maps = []
    for b in range(n_cores):
        m = dict(shared)
        m["x"] = np.ascontiguousarray(x[b])
        maps.append(m)
    return maps


_CACHE = {}


def kernel(**inputs):
    if "nc" not in _CACHE:
        _CACHE["nc"] = build_program()[0]
    nc = _CACHE["nc"]
    maps = make_in_maps(inputs, 8)
    res = run_bass_kernel_spmd(nc, maps, core_ids=list(range(8)))
    out = np.stack([np.asarray(r["out"], dtype=np.float32) for r in res.results], axis=0)
    return out
```

```python
import contextlib
import math

import numpy as np

import concourse.bass as bass
import concourse.mybir as mybir
from concourse.bass_utils import run_bass_kernel_spmd

F32 = mybir.dt.float32
BF16 = mybir.dt.bfloat16
AF = mybir.ActivationFunctionType
ALU = mybir.AluOpType
AX = mybir.AxisListType

D = 1024
SEQ = 2048
NMETA = 16
NH = 8
HD = 64
VD = 128
NPG = 4
POOL_W = (2, 4, 8, 16)
IN_W = 5632
NE = 16
DE = 512
EPS = 1e-6
TC = 2176
MOFF = 112
ROFF = 128
O_Q, O_K, O_V, O_P, O_G = 0, 1024, 2048, 3072, 3584
LAMBDA_INIT = 0.8 - 0.6 * math.exp(-0.3 * 0)
EPOCH = 12000


class _Op:
    __slots__ = ("eng", "fn", "waits", "signal", "idx", "dma_stream", "sigcount")

    def __init__(self, eng, fn):
        self.eng = eng
        self.fn = fn
        self.waits = []
        self.signal = False
        self.idx = -1
        self.dma_stream = None
        self.sigcount = 0


class Sched:
    ENGS = ("pe", "act", "dve", "pool", "sp")

    def __init__(self):
        self.ops = {e: [] for e in self.ENGS}
        self.last_w = {}
        self.readers = {}
        self.known = {e: {} for e in self.ENGS}
        self.streams = {}
        self.pending = {e: [] for e in self.ENGS}

    def _deps(self, eng, reads, writes, is_dma=False):
        deps = {}

        def add(kind, src, n, raw):
            if kind == "e" and src == eng and not is_dma:
                if eng == "pe" or eng == "sp":
                    return
            if kind == "d":
                n = self.streams[src]
            key = (kind, src)
            if deps.get(key, -1) < n:
                deps[key] = n

        for r in reads:
            ev = self.last_w.get(r)
            if ev is not None:
                add(ev[0], ev[1], ev[2], True)
        for w in writes:
            ev = self.last_w.get(w)
            if ev is not None:
                add(ev[0], ev[1], ev[2], False)
            rd = self.readers.get(w)
            if rd:
                for key, n in rd.items():
                    add(key[0], key[1], n, False)
        for (kind, src, n) in self.pending[eng]:
            if kind == "e" and src == eng and not is_dma:
                continue
            key = (kind, src)
            if deps.get(key, -1) < n:
                deps[key] = n
        self.pending[eng] = []
        out = []
        kn = self.known[eng]
        for key, n in deps.items():
            if kn.get(key, -1) >= n:
                continue
            kn[key] = n
            out.append((key[0], key[1], n))
        return out

    def _commit(self, ev, reads, writes):
        for w in writes:
            self.last_w[w] = ev
            self.readers[w] = {}
        key = (ev[0], ev[1])
        for r in reads:
            d = self.readers.setdefault(r, {})
            if d.get(key, -1) < ev[2]:
                d[key] = ev[2]

    def _add(self, eng, fn, reads, writes, stream):
        ps_reads = [r for r in reads if isinstance(r, tuple) and r[0] == "ps"]
        if ps_reads:
            writes = list(writes) + ps_reads
        o = _Op(eng, fn)
        o.waits = self._deps(eng, reads, writes, is_dma=stream is not None)
        lst = self.ops[eng]
        o.idx = len(lst)
        lst.append(o)
        for (kind, src, n) in o.waits:
            if kind == "e":
                self.ops[src][n].signal = True
        if stream is None:
            ev = ("e", eng, o.idx)
        else:
            c = self.streams.get(stream, 0) + 1
            self.streams[stream] = c
            o.dma_stream = stream
            ev = ("d", stream, c)
        self._commit(ev, reads, writes)
        return o

    def op(self, eng, fn, reads=(), writes=()):
        return self._add(eng, fn, reads, writes, None)

    def dma(self, queue, stream, fn, reads=(), writes=()):
        return self._add(queue, fn, reads, writes, stream)

    def barrier(self):
        evs = []
        for e in self.ENGS:
            if self.ops[e]:
                evs.append(("e", e, len(self.ops[e]) - 1))
        for s, c in self.streams.items():
            evs.append(("d", s, c))
        for e in self.ENGS:
            self.pending[e] = list(evs)
        fixed = []
        for (kind, src, n) in evs:
            if kind == "e":
                i = n
                while i >= 0 and self.ops[src][i].dma_stream is not None:
                    i -= 1
                if i < 0:
                    continue
                fixed.append(("e", src, i))
            else:
                fixed.append((kind, src, n))
        for e in self.ENGS:
            self.pending[e] = list(fixed)

    def finish(self, eng="sp"):
        self.barrier()
        o = _Op(eng, None)
        o.waits = self._deps(eng, (), ())
        for (kind, src, n) in o.waits:
            if kind == "e":
                self.ops[src][n].signal = True
        o.idx = len(self.ops[eng])
        self.ops[eng].append(o)

    def emit(self, nc):
        for e, lst in self.ops.items():
            c = 0
            for o in lst:
                if o.signal:
                    c += 1
                o.sigcount = c
        nsig = {e: (lst[-1].sigcount if lst else 0) for e, lst in self.ops.items()}
        with contextlib.ExitStack() as st:
            esems = {}
            for e in self.ENGS:
                n_ep = nsig[e] // EPOCH + 1
                esems[e] = [st.enter_context(nc.semaphore(f"s_{e}_{k}")) for k in range(n_ep)]
            dsems = {s: st.enter_context(nc.semaphore("d_" + "_".join(str(x) for x in (s if isinstance(s, tuple) else (s,))))) for s in self.streams}
            block = st.enter_context(nc.Block())

            def run(engname):
                def body(eng):
                    for o in self.ops[engname]:
                        for (kind, src, n) in o.waits:
                            if kind == "e":
                                sc = self.ops[src][n].sigcount
                                ep, v = (sc - 1) // EPOCH, (sc - 1) % EPOCH + 1
                                eng.wait_ge(esems[src][ep], v)
                            else:
                                eng.wait_ge(dsems[src], 16 * n)
                        if o.fn is None:
                            continue
                        ins = o.fn(eng)
                        if o.dma_stream is not None:
                            ins.then_inc(dsems[o.dma_stream], 16)
                        elif o.signal:
                            ep = (o.sigcount - 1) // EPOCH
                            ins.then_inc(esems[engname][ep], 1)
                return body

            block.tensor(run("pe"))
            block.scalar(run("act"))
            block.vector(run("dve"))
            block.gpsimd(run("pool"))
            block.sync(run("sp"))
        return nsig


def build_program(dbg=(), stop_after=None):
    nc = bass.Bass("TRN2", target_bir_lowering=False)
    dbg = set(dbg)

    def din(name, shape, dt=F32):
        return nc.dram_tensor(name, list(shape), dt, kind="ExternalInput").ap()

    x_d = din("x", [SEQ, D])
    meta_d = din("meta", [NMETA, D])
    g1_d = din("norm1_g", [1, D])
    w_in_d = din("w_in", [D, IN_W])
    bgate_d = din("b_gate_t", [128, 16])
    lq1_d = din("lambda_q1", [1, HD])
    lk1_d = din("lambda_k1", [1, HD])
    lq2_d = din("lambda_q2", [1, HD])
    lk2_d = din("lambda_k2", [1, HD])
    subg_d = din("subln_g_t", [128, 1])
    poolw_d = din("pool_w", [NPG, 128, 128])
    pscale_d = din("pool_scale_t", [128, NPG])
    wattn_d = din("w_attn_br", [D, D])
    wpool_d = din("w_pool_br", [512, D])
    wout_d = din("w_out", [D, D])
    g2_d = din("norm2_g", [1, D])
    wr_d = din("w_router", [D, 20])
    br_d = din("b_router", [1, 20])
    weg_d = din("w_e_gate", [NE, D, DE])
    weu_d = din("w_e_up", [NE, D, DE])
    wed_d = din("w_e_down", [NE, DE, D])
    gf_d = din("final_g", [1, D])
    cos_d = din("cos_t", [128, TC])
    sin_d = din("sin_t", [128, TC])
    ident_d = din("c_ident", [128, 128])
    perm_d = din("c_perm", [128, 128])
    tri_d = din("c_tri", [128, 256])
    ones_d = din("c_ones", [128, 128])
    ones16_d = din("c_ones16", [128, 128])
    negm_d = din("c_negm", [128, 128])
    out_d = nc.dram_tensor("out", [SEQ, D], F32, kind="ExternalOutput").ap()
    dbg_d = {}

    def dbg_out(name, shape):
        dbg_d[name] = nc.dram_tensor("dbg_" + name, list(shape), F32, kind="ExternalOutput").ap()
        return dbg_d[name]

    S = Sched()
    phases = ["A", "B0", "B1", "D", "E1", "E2", "F", "G"]
    last_phase = stop_after or "G"

    def active(ph):
        return phases.index(ph) <= phases.index(last_phase)

    with contextlib.ExitStack() as st:
        def sb(name, shape, dt):
            return st.enter_context(nc.sbuf_tensor(name, list(shape), dt))

        R1 = sb("R1", [128, 34816 // 4], F32)
        R42 = sb("R42", [128, 67584 // 4], F32)
        R35 = sb("R35", [128, 51200 // 4], F32)
        RT_BYTES = 50 * 1024
        RT = sb("RT", [128, RT_BYTES // 4], F32)

        def view(region, off_bytes, shape, dt):
            n = 1
            for s_ in shape:
                n *= s_
            if dt == F32:
                assert off_bytes % 4 == 0
                a = region[:, off_bytes // 4: off_bytes // 4 + n]
            else:
                assert off_bytes % 4 == 0 and (n * 2) % 4 == 0
                a = region[:, off_bytes // 4: off_bytes // 4 + (n * 2) // 4].bitcast(BF16)
            if len(shape) == 2:
                a = a.rearrange("p (a b) -> p a b", a=shape[0])
            elif len(shape) == 3:
                a = a.rearrange("p (a b c) -> p a b c", a=shape[0], b=shape[1])
            return a

        class RTAlloc:
            def __init__(self):
                self.off = 0

            def reset(self):
                self.off = 0

            def get(self, shape, dt):
                n = 1
                for s_ in shape:
                    n *= s_
                nb = n * (4 if dt == F32 else 2)
                nb = (nb + 31) // 32 * 32
                v = view(RT, self.off, shape, dt)
                self.off += nb
                assert self.off <= RT_BYTES, ("RT overflow", self.off)
                return v

        rt = RTAlloc()
        _tail = RT_BYTES - (1280 + 4096 + 1024)
        rl = view(RT, _tail, [16, 20], F32)
        rw = view(RT, _tail + 1280, [16, 64], F32)
        comb = view(RT, _tail + 1280 + 4096, [16, 16], F32)

        hnT = view(R1, 0, [8, TC], BF16)
        hn2T = view(R1, 0, [8, SEQ], BF16)
        Vt = view(R42, 0, [17, D], BF16)
        QT = view(R42, 34816, [8, SEQ], BF16)
        h1 = view(R42, 0, [16, D], F32)
        KT = view(R35, 0, [8, TC], BF16)
        mixT = view(R35, 0, [8, SEQ], BF16)
        PT = view(R35, 34816, [NPG, SEQ], BF16)
        WB = 24576
        wgt = [view(R35, k * WB, [8, DE], BF16) for k in range(2)]
        wup = [view(R35, k * WB + 8192, [8, DE], BF16) for k in range(2)]
        wdn = [view(R35, k * WB + 16384, [4, D], BF16) for k in range(2)]

        ident = sb("ident", [128, 128], BF16)
        permm = sb("permm", [128, 128], BF16)
        tri = sb("tri", [128, 2, 128], BF16)
        ones = sb("ones", [128, 128], BF16)
        ones16 = sb("ones16", [128, 128], BF16)
        negm = sb("negm", [128, 128], BF16)
        poolw = sb("poolw", [128, NPG, 128], BF16)
        bgate = sb("bgate", [128, 16], F32)
        pscale = sb("pscale", [128, NPG], F32)
        subg = sb("subg", [128, 1], F32)
        lamt = sb("lamt", [128, 4 * HD], F32)
        lams = sb("lams", [128, 8], F32)
        stats = sb("stats", [128, 17, 2], F32)
        stats2 = sb("stats2", [128, 16, 2], F32)
        stats3 = sb("stats3", [128, 16, 2], F32)
        wr_sb = sb("wr_sb", [128, 8, 20], BF16)
        br_sb = sb("br_sb", [128, 20], F32)

        _pb = [st.enter_context(nc.psum_tensor(f"pb{i}", [128, 512], F32)) for i in range(4)]
        pS = [st.enter_context(nc.psum_tensor(f"pS{i}", [128, 1024], F32)) for i in range(2)]
        pbanks = [t[:, :] for t in _pb] + [pS[0][:, 0:512], pS[0][:, 512:1024], pS[1][:, 0:512], pS[1][:, 512:1024]]

        def pbf(i):
            return pbanks[i].bitcast(BF16)

        cres = lambda t: ("c", t.name)
        for t, d_ in ((ident, ident_d), (permm, perm_d), (ones, ones_d), (ones16, ones16_d), (negm, negm_d)):
            S.dma("pool", "constP", lambda e, t=t, d_=d_: e.dma_start(out=t[:, :], in_=d_), writes=[cres(t)])
        S.dma("pool", "constP", lambda e: e.dma_start(out=tri[:, :, :], in_=tri_d.rearrange("p (a b) -> p a b", a=2)), writes=[cres(tri)])
        S.dma("pool", "constP", lambda e: e.dma_start(out=poolw[:, :, :], in_=poolw_d.rearrange("g c d -> c g d")), writes=[cres(poolw)])
        S.dma("pool", "constP", lambda e: e.dma_start(out=wr_sb[:, :, :], in_=wr_d.rearrange("(c p) n -> p c n", p=128)), writes=[cres(wr_sb)])
        for t, d_ in ((bgate, bgate_d), (pscale, pscale_d), (subg, subg_d)):
            S.dma("sp", "constS", lambda e, t=t, d_=d_: e.dma_start(out=t[:, :], in_=d_), writes=[cres(t)])
        S.dma("sp", "constS", lambda e: e.dma_start(out=br_sb[:, :], in_=br_d.to_broadcast([128, 20])), writes=[cres(br_sb)])
        for i, d_ in enumerate((lq1_d, lk1_d, lq2_d, lk2_d)):
            S.dma("sp", "constS", lambda e, i=i, d_=d_: e.dma_start(out=lamt[:, i * HD:(i + 1) * HD], in_=d_.to_broadcast([128, HD])), writes=[("lamt", i)])

        S.op("dve", lambda e: e.tensor_tensor(out=lamt[:, 0:HD], in0=lamt[:, 0:HD], in1=lamt[:, HD:2 * HD], op=ALU.mult),
             reads=[("lamt", 0), ("lamt", 1)], writes=[("lamt", 0)])
        S.op("dve", lambda e: e.tensor_tensor(out=lamt[:, 2 * HD:3 * HD], in0=lamt[:, 2 * HD:3 * HD], in1=lamt[:, 3 * HD:4 * HD], op=ALU.mult),
             reads=[("lamt", 2), ("lamt", 3)], writes=[("lamt", 2)])
        S.op("dve", lambda e: e.reduce_sum(out=lams[:, 0:1], in_=lamt[:, 0:HD], axis=AX.X), reads=[("lamt", 0)], writes=[("lams", 0)])
        S.op("dve", lambda e: e.reduce_sum(out=lams[:, 1:2], in_=lamt[:, 2 * HD:3 * HD], axis=AX.X), reads=[("lamt", 2)], writes=[("lams", 1)])
        S.op("act", lambda e: e.activation(out=lams[:, 2:4], in_=lams[:, 0:2], func=AF.Exp), reads=[("lams", 0), ("lams", 1)], writes=[("lams", 2)])
        S.op("dve", lambda e: e.scalar_tensor_tensor(out=lams[:, 4:5], in0=lams[:, 3:4], scalar=-LAMBDA_INIT, in1=lams[:, 2:3],
                                                     op0=ALU.add, op1=ALU.subtract), reads=[("lams", 2)], writes=[("lams", 4)])
        S.op("dve", lambda e: e.tensor_scalar(out=lams[:, 5:6], in0=subg[:, 0:1], scalar1=1.0 - LAMBDA_INIT, scalar2=None, op0=ALU.mult),
             reads=[cres(subg)], writes=[("lams", 5)])
        neglam = lams[:, 4:5]
        gsub = lams[:, 5:6]

        def PS(i):
            return ("ps", i)

        rt.reset()
        wblk = [rt.get([8, 512], BF16) for _ in range(2)]
        S.dma("pool", ("wld", 0), lambda e: e.dma_start(out=wblk[0], in_=w_in_d[:, O_P:O_P + 512].rearrange("(c p) n -> p c n", p=128)), writes=[("wblk", 0)])
        S.dma("pool", ("wld", 1), lambda e: e.dma_start(out=wblk[1], in_=w_in_d[:, O_Q:O_Q + 512].rearrange("(c p) n -> p c n", p=128)), writes=[("wblk", 1)])
        g1bc = rt.get([D], F32)
        NBA = 4
        xt = [rt.get([D], F32) for _ in range(NBA)]
        junk = rt.get([D], BF16)
        xn = [rt.get([D], BF16) for _ in range(NBA)]
        S.dma("sp", "miscS", lambda e: e.dma_start(out=g1bc, in_=g1_d.to_broadcast([128, D])), writes=["g1bc"])
        S.op("pool", lambda e: e.memset(xt[0], 0.0), writes=[("xt", 0)])
        def a_s1(i):
            b = i % NBA
            if i == 0:
                S.dma("sp", ("xt", 0), lambda e: e.dma_start(out=xt[0][MOFF:128, :], in_=meta_d), writes=[("xt", 0)], reads=[])
            else:
                S.dma("sp", ("xt", b), lambda e: e.dma_start(out=xt[b], in_=x_d[(i - 1) * 128:i * 128, :]), writes=[("xt", b)])
            S.op("act", lambda e: e.activation(out=junk, in_=xt[b], func=AF.Square, accum_out=stats[:, i, 0:1]),
                 reads=[("xt", b)], writes=["junk", ("st", i)])
            S.op("act", lambda e: e.activation(out=stats[:, i, 1:2], in_=stats[:, i, 0:1], func=AF.Sqrt, scale=1.0 / D, bias=EPS),
                 reads=[("st", i)], writes=[("st1", i)])
            S.op("dve", lambda e: e.reciprocal(out=stats[:, i, 1:2], in_=stats[:, i, 1:2]), reads=[("st1", i)], writes=[("st1", i)])
            S.op("dve", lambda e: e.scalar_tensor_tensor(out=xn[b], in0=xt[b], scalar=stats[:, i, 1:2], in1=g1bc, op0=ALU.mult, op1=ALU.mult),
                 reads=[("xt", b), ("st1", i), "g1bc"], writes=[("xn", b)])

        def a_s2(i):
            b = i % NBA
            pbk = i % 4
            for c in range(8):
                S.op("pe", lambda e, c=c: e.transpose(out=pbf(pbk)[:, c * 128:(c + 1) * 128], in_=xn[b][:, c * 128:(c + 1) * 128], identity=ident[:, :]),
                     reads=[("xn", b), cres(ident)], writes=[PS(pbk)])
            S.op("act", lambda e: e.copy(out=hnT[:, :, i * 128:(i + 1) * 128], in_=pbf(pbk).rearrange("p (c n) -> p c n", c=8)),
                 reads=[PS(pbk)], writes=[("hnT", i)])

        for it_ in range(17 + 2):
            if it_ < 17:
                a_s1(it_)
            if 0 <= it_ - 2 < 17:
                a_s2(it_ - 2)
        HNT_ALL = [("hnT", i) for i in range(17)]

        if "hnT" in dbg:
            o = dbg_out("hnT", [128, 8 * TC])
            S.dma("pool", "dbgP", lambda e, o=o: e.dma_start(out=o.rearrange("p (c t) -> p c t", c=8), in_=hnT), reads=HNT_ALL, writes=["dbg_hnT"])

        def load_wblock(dst, src2d, c0, ncols, res, kch=8):
            S.dma("pool", ("wld", res[1]), lambda e: e.dma_start(out=dst, in_=src2d[:, c0:c0 + ncols].rearrange("(c p) n -> p c n", p=128)),
                  writes=[res])

        if active("B0"):
            S.barrier()
            rt.reset()
            wblk = [rt.get([8, 512], BF16) for _ in range(2)]
            u32 = [rt.get([TC], F32) for _ in range(2)]
            tmpA = rt.get([TC], F32)
            tmpB = rt.get([TC], F32)
            psrot = [0]

            def nextbank(lo, hi):
                b_ = lo + psrot[0] % (hi - lo)
                psrot[0] += 1
                return b_

            def b0_proj(g):
                ub = u32[g % 2]
                ures = ("u32", g % 2)
                for jt in range(5):
                    c0, n = (0, 128) if jt == 0 else (ROFF + (jt - 1) * 512, 512)
                    bk = nextbank(0, 4)
                    for c in range(8):
                        S.op("pe", lambda e, c=c, g=g, c0=c0, n=n, bk=bk: e.matmul(pbanks[bk][:, 0:n], lhsT=wblk[0][:, c, g * 128:(g + 1) * 128],
                                                                                 rhs=hnT[:, c, c0:c0 + n], start=(c == 0), stop=(c == 7)),
                             reads=[("wblk", 0)] + HNT_ALL, writes=[PS(bk)])
                    S.op("act", lambda e, ub=ub, c0=c0, n=n, bk=bk: e.copy(out=ub[:, c0:c0 + n], in_=pbanks[bk][:, 0:n]),
                         reads=[PS(bk)], writes=[ures])

            def b0_pool(g):
                ub = u32[g % 2]
                ures = ("u32", g % 2)
                w = POOL_W[g]
                L = TC
                cur, cur_res = ub, ures
                step = 1
                bufs = [(tmpA, "tmpA"), (tmpB, "tmpB")]
                k = 0
                while step < w:
                    dst, dres = bufs[k % 2]
                    lo = 2 * step - 1
                    S.op("dve", lambda e, dst=dst, cur=cur, lo=lo, step=step: e.tensor_tensor(out=dst[:, lo:L], in0=cur[:, lo:L], in1=cur[:, lo - step:L - step], op=ALU.add),
                         reads=[cur_res], writes=[dres])
                    cur, cur_res = dst, dres
                    step *= 2
                    k += 1
                ydst, yres = bufs[k % 2]
                ybf = ydst[:, 0:SEQ // 2].bitcast(BF16)
                S.op("dve", lambda e, ybf=ybf, cur=cur, ub=ub, w=w: e.scalar_tensor_tensor(out=ybf, in0=cur[:, ROFF:TC], scalar=1.0 / w, in1=ub[:, ROFF:TC],
                                                                                            op0=ALU.mult, op1=ALU.subtract),
                     reads=[cur_res, ures], writes=[yres])
                for jt in range(4):
                    bk = nextbank(0, 4)
                    S.op("pe", lambda e, g=g, jt=jt, bk=bk, ybf=ybf: e.matmul(pbanks[bk][:, :], lhsT=poolw[:, g, :], rhs=ybf[:, jt * 512:(jt + 1) * 512], start=True, stop=True),
                         reads=[yres, cres(poolw)], writes=[PS(bk)])
                    S.op("act", lambda e, g=g, jt=jt, bk=bk: e.activation(out=PT[:, g, jt * 512:(jt + 1) * 512], in_=pbanks[bk][:, :], func=AF.Identity, scale=pscale[:, g:g + 1]),
                         reads=[PS(bk), cres(pscale)], writes=[("PT", g)])

            for it_ in range(NPG + 1):
                if it_ < NPG:
                    b0_proj(it_)
                if it_ >= 1:
                    b0_pool(it_ - 1)
            if "PT" in dbg:
                o = dbg_out("PT", [128, NPG * SEQ])
                S.dma("pool", "dbgP", lambda e, o=o: e.dma_start(out=o.rearrange("p (c t) -> p c t", c=NPG), in_=PT), reads=[("PT", g) for g in range(NPG)], writes=["dbg_PT"])

        if active("B1"):
            S.barrier()
            rt.reset()
            wblk = [rt.get([8, 512], BF16) for _ in range(2)]
            cosT = rt.get([TC], F32)
            sinT = rt.get([TC], F32)
            zb = [rt.get([512], BF16) for _ in range(2)]
            t1 = [rt.get([512], F32) for _ in range(2)]
            t2 = [rt.get([512], F32) for _ in range(2)]
            S.dma("sp", "miscS", lambda e: e.dma_start(out=cosT, in_=cos_d), writes=["cosT"])
            S.dma("sp", "miscS", lambda e: e.dma_start(out=sinT, in_=sin_d), writes=["sinT"])
            blocks = [("q", 0), ("q", 1), ("k", 0), ("k", 1), ("v", 0), ("v", 1)]
            col_of = {"q": O_Q, "k": O_K, "v": O_V}
            items = []

            def stage1(it):
                kind, h, c0, n, wb, nchunk, slot = it
                bk = slot % 2
                for c in range(8):
                    S.op("pe", lambda e, c=c: e.matmul(pbanks[bk][:, 0:n], lhsT=wblk[wb][:, c, nchunk * 128:(nchunk + 1) * 128], rhs=hnT[:, c, c0:c0 + n],
                                                       start=(c == 0), stop=(c == 7)),
                         reads=[("wblk", wb)] + HNT_ALL, writes=[PS(bk)])
                S.op("act", lambda e: e.copy(out=zb[bk][:, 0:n], in_=pbanks[bk][:, 0:n]), reads=[PS(bk)], writes=[("zb", bk)])

            def stage2(it):
                kind, h, c0, n, wb, nchunk, slot = it
                bk = slot % 2
                rb = 2 + slot % 2
                S.op("pe", lambda e: e.matmul(pbanks[rb][:, 0:n], lhsT=permm[:, :], rhs=zb[bk][:, 0:n], start=True, stop=True),
                     reads=[("zb", bk), cres(permm)], writes=[PS(rb)])
                S.op("dve", lambda e: e.tensor_tensor(out=t1[bk][:, 0:n], in0=pbanks[bk][:, 0:n], in1=cosT[:, c0:c0 + n], op=ALU.mult),
                     reads=[PS(bk), "cosT"], writes=[("t1", bk)])
                S.op("dve", lambda e: e.tensor_tensor(out=t2[bk][:, 0:n], in0=pbanks[rb][:, 0:n], in1=sinT[:, c0:c0 + n], op=ALU.mult),
                     reads=[PS(rb), "sinT"], writes=[("t2", bk)])
                if kind == "q":
                    dst = QT[:, h, c0 - ROFF:c0 - ROFF + n]
                    dres = ("QT", h, (c0 - ROFF) // 512)
                else:
                    dst = KT[:, h, c0:c0 + n]
                    dres = ("KT", h)
                S.op("pool", lambda e: e.tensor_tensor(out=dst, in0=t1[bk][:, 0:n], in1=t2[bk][:, 0:n], op=ALU.add),
                     reads=[("t1", bk), ("t2", bk)], writes=[dres])

            slot = 0
            prev = None
            for bi, (kind, half) in enumerate(blocks):
                wb = (bi + 1) % 2
                if bi > 0:
                    load_wblock(wblk[wb], w_in_d, col_of[kind] + half * 512, 512, ("wblk", wb))
                if kind in ("q", "k"):
                    for nchunk in range(4):
                        h = half * 4 + nchunk
                        tiles = ([(0, 128)] if kind == "k" else []) + [(ROFF + jt * 512, 512) for jt in range(4)]
                        for (c0, n) in tiles:
                            it = (kind, h, c0, n, wb, nchunk, slot)
                            slot += 1
                            stage1(it)
                            if prev is not None:
                                stage2(prev)
                            prev = it
                else:
                    if prev is not None:
                        stage2(prev)
                        prev = None
                    for i in range(17):
                        for sub in range(1):
                            bk = 4 + (i % 2)
                            for c in range(8):
                                S.op("pe", lambda e, c=c, i=i, bk=bk, wb=wb: e.matmul(pbanks[bk][:, :], lhsT=hnT[:, c, i * 128:(i + 1) * 128], rhs=wblk[wb][:, c, :],
                                                                                     start=(c == 0), stop=(c == 7)),
                                     reads=[("wblk", wb), ("hnT", i)], writes=[PS(bk)])
                            S.op("act", lambda e, i=i, bk=bk, half=half: e.copy(out=Vt[:, i, half * 512:(half + 1) * 512], in_=pbanks[bk][:, :]),
                                 reads=[PS(bk)], writes=[("V", i)])
            if prev is not None:
                stage2(prev)
                prev = None
            for nm, tns, shp, res in (("QT", QT, [128, 8 * SEQ], [("QT", h, j) for h in range(8) for j in range(4)]),
                                      ("KT", KT, [128, 8 * TC], [("KT", h) for h in range(8)]),
                                      ("V", Vt, [128, 17 * D], [("V", i) for i in range(17)])):
                if nm in dbg:
                    o = dbg_out(nm, shp)
                    S.dma("pool", "dbgP", lambda e, o=o, tns=tns: e.dma_start(out=o.rearrange("p (c t) -> p c t", c=tns.shape[1]), in_=tns), reads=res, writes=["dbg_" + nm])

        if active("D"):
            S.barrier()
            rt.reset()
            QZ = [[rt.get([SEQ], BF16) for c in range(2)] for _ in range(2)]
            pTb = [rt.get([2, 512], BF16) for _ in range(3)]
            r2 = rt.get([2, 512], F32)
            o_t = [rt.get([512], F32) for _ in range(2)]
            a_t = rt.get([512], F32)
            sq_t = rt.get([512], BF16)
            ln_t = rt.get([512], F32)
            for par in range(2):
                S.op("pool", lambda e, par=par: e.memset(QZ[par][0], 0.0), writes=[("QZ", par, 0)])
                S.op("pool", lambda e, par=par: e.memset(QZ[par][1], 0.0), writes=[("QZ", par, 1)])
            ACC_O = (0, 2)
            ACC_S = (1, 3)
            SCALE = 1.0 / math.sqrt(HD)
            cnts = {"s": 0, "pt": 0}

            def build_qz(h):
                par = h % 2
                S.op("dve", lambda e: e.tensor_copy(out=QZ[par][0][0:64, :], in_=QT[0:64, h, :]),
                     reads=[("QT", h, j) for j in range(4)], writes=[("QZ", par, 0)])
                S.op("dve", lambda e: e.tensor_copy(out=QZ[par][1][64:128, :], in_=QT[64:128, h, :]),
                     reads=[("QT", h, j) for j in range(4)], writes=[("QZ", par, 1)])

            items = []
            prev_hj = None
            for h in range(NH):
                for j in range(4):
                    kts = [(0, 0, 0, False)]
                    for i in range(4 * j):
                        kts.append((1 + i, ROFF + i * 128, 0, False))
                    for r in range(4):
                        i = 4 * j + r
                        kts.append((1 + i, ROFF + i * 128, 128 * r, True))
                    for idx, kt in enumerate(kts):
                        items.append(("kt", h, j, kt, idx == 0, idx == len(kts) - 1))
                        if idx == min(5, len(kts) - 2) and prev_hj is not None:
                            items.append(("norm",) + prev_hj)
                    prev_hj = (h, j)
            final_norm = ("norm",) + prev_hj

            def stage_s(it):
                p = cnts["s"] % 2
                cnts["s"] += 1
                if it[0] == "norm":
                    _, h, j = it
                    S.op("pe", lambda e: e.matmul(pS[p][:, 0:512], lhsT=ones[:, :], rhs=sq_t, start=True, stop=True),
                         reads=["sq_t", cres(ones)], writes=[PS(4 + 2 * p)])
                    S.op("act", lambda e: e.activation(out=ln_t, in_=pS[p][:, 0:512], func=AF.Ln, scale=1.0 / VD, bias=EPS),
                         reads=[PS(4 + 2 * p)], writes=["ln_t"])
                    S.op("act", lambda e: e.activation(out=ln_t, in_=ln_t, func=AF.Exp, scale=-0.5), reads=["ln_t"], writes=["ln_t"])
                    return None
                _, h, j, (vt_i, kc0, q0, diag), first, last = it
                par = h % 2
                pb_i = cnts["pt"] % 3
                cnts["pt"] += 1
                for c in range(2):
                    S.op("pe", lambda e, c=c: e.matmul(pS[p][:, c * 512 + q0:(c + 1) * 512], lhsT=KT[c * 64:(c + 1) * 64, h, kc0:kc0 + 128],
                                                       rhs=QT[c * 64:(c + 1) * 64, h, j * 512 + q0:(j + 1) * 512], start=True, stop=not diag),
                         reads=[("KT", h), ("QT", h, j)], writes=[PS(4 + 2 * p + c)])
                    if diag:
                        S.op("pe", lambda e, c=c: e.matmul(pS[p][:, c * 512 + q0:c * 512 + q0 + 128], lhsT=ident[:, :], rhs=negm[:, :], start=False, stop=True),
                             reads=[cres(ident), cres(negm)], writes=[PS(4 + 2 * p + c)])
                S.op("act", lambda e: e.activation(out=pTb[pb_i][:, :, q0:512], in_=pS[p][:, :].rearrange("p (c n) -> p c n", c=2)[:, :, q0:512],
                                                   func=AF.Exp, scale=SCALE),
                     reads=[PS(4 + 2 * p), PS(4 + 2 * p + 1)], writes=[("pT", pb_i)])
                return pb_i

            def stage_av(it, pb_i):
                if it[0] == "norm":
                    _, h, j = it
                    S.op("dve", lambda e: e.scalar_tensor_tensor(out=QT[:, h, j * 512:(j + 1) * 512], in0=a_t, scalar=gsub, in1=ln_t, op0=ALU.mult, op1=ALU.mult),
                         reads=["a_t", "ln_t", ("lams", 5)], writes=[("QT", h, j)])
                    return
                _, h, j, (vt_i, kc0, q0, diag), first, last = it
                on = ones16 if vt_i == 0 else ones
                for c in range(2):
                    S.op("pe", lambda e, c=c: e.matmul(pbanks[ACC_O[c]][:, q0:512], lhsT=Vt[:, vt_i, h * 128:(h + 1) * 128], rhs=pTb[pb_i][:, c, q0:512],
                                                       start=first, stop=last),
                         reads=[("V", vt_i), ("pT", pb_i)], writes=[PS(ACC_O[c])])
                    S.op("pe", lambda e, c=c: e.matmul(pbanks[ACC_S[c]][:, q0:512], lhsT=on[:, :], rhs=pTb[pb_i][:, c, q0:512],
                                                       start=first, stop=last),
                         reads=[cres(on), ("pT", pb_i)], writes=[PS(ACC_S[c])])
                if last:
                    for c in range(2):
                        S.op("act", lambda e, c=c: e.activation(out=r2[:, c, :], in_=pbanks[ACC_S[c]], func=AF.Ln), reads=[PS(ACC_S[c])], writes=["r2"])
                        S.op("dve", lambda e, c=c: e.tensor_copy(out=o_t[c], in_=pbanks[ACC_O[c]]), reads=[PS(ACC_O[c])], writes=[("o_t", c)])
                    S.op("act", lambda e: e.activation(out=r2[:, :, :], in_=r2[:, :, :], func=AF.Exp, scale=-1.0), reads=["r2"], writes=["r2"])
                    for c in range(2):
                        S.op("dve", lambda e, c=c: e.tensor_tensor(out=o_t[c], in0=o_t[c], in1=r2[:, c, :], op=ALU.mult),
                             reads=[("o_t", c), "r2"], writes=[("o_t", c)])
                    S.op("dve", lambda e: e.scalar_tensor_tensor(out=a_t, in0=o_t[1], scalar=neglam, in1=o_t[0], op0=ALU.mult, op1=ALU.add),
                         reads=[("o_t", 0), ("o_t", 1), ("lams", 4)], writes=["a_t"])
                    S.op("pool", lambda e: e.tensor_tensor(out=sq_t, in0=a_t, in1=a_t, op=ALU.mult), reads=["a_t"], writes=["sq_t"])

            prev_it, prev_pb = None, None
            for it in items + [None]:
                pb_new = stage_s(it) if it is not None else None
                if prev_it is not None:
                    stage_av(prev_it, prev_pb)
                prev_it, prev_pb = it, pb_new
            stage_s(final_norm)
            stage_av(final_norm, None)
            if "AT" in dbg:
                o = dbg_out("AT", [128, 8 * SEQ])
                S.dma("pool", "dbgP", lambda e, o=o: e.dma_start(out=o.rearrange("p (c t) -> p c t", c=8), in_=QT), reads=[("QT", h, j) for h in range(8) for j in range(4)], writes=["dbg_AT"])

        if active("E1"):
            S.barrier()
            rt.reset()
            wout_sb = rt.get([8, D], BF16)
            wa = [rt.get([8, 128], BF16) for _ in range(2)]
            wp = [rt.get([4, 128], BF16) for _ in range(2)]
            wg0 = [rt.get([8, 128], BF16) for _ in range(2)]
            wg1 = [rt.get([8, 128], BF16) for _ in range(2)]
            gt = [[rt.get([512], F32) for _ in range(2)] for _ in range(2)]
            mt = [[rt.get([512], F32) for _ in range(2)] for _ in range(2)]
            ATR = [("QT", h, j) for h in range(8) for j in range(4)]

            def load_e1(dc):
                b = dc % 2
                S.dma("pool", ("e1w", b), lambda e: e.dma_start(out=wa[b], in_=wattn_d[:, dc * 128:(dc + 1) * 128].rearrange("(c p) n -> p c n", p=128)), writes=[("wa", b)])
                S.dma("pool", ("e1w", b), lambda e: e.dma_start(out=wp[b], in_=wpool_d[:, dc * 128:(dc + 1) * 128].rearrange("(c p) n -> p c n", p=128)), writes=[("wp", b)])
                S.dma("pool", ("e1w", b), lambda e: e.dma_start(out=wg0[b], in_=w_in_d[:, O_G + dc * 128:O_G + (dc + 1) * 128].rearrange("(c p) n -> p c n", p=128)), writes=[("wg0", b)])
                S.dma("pool", ("e1w", b), lambda e: e.dma_start(out=wg1[b], in_=w_in_d[:, O_G + D + dc * 128:O_G + D + (dc + 1) * 128].rearrange("(c p) n -> p c n", p=128)), writes=[("wg1", b)])

            def e1_tile(dc, j, b, pr):
                cs = slice(ROFF + j * 512, ROFF + (j + 1) * 512)
                qs = slice(j * 512, (j + 1) * 512)
                bk_g0, bk_a, bk_g1, bk_p = (0 + pr, 2 + pr, 4 + pr, 6 + pr)
                for c in range(8):
                    S.op("pe", lambda e, c=c: e.matmul(pbanks[bk_g0][:, :], lhsT=wg0[b][:, c, :], rhs=hnT[:, c, cs], start=(c == 0), stop=(c == 7)),
                         reads=[("wg0", b)] + HNT_ALL, writes=[PS(bk_g0)])
                S.op("act", lambda e: e.activation(out=gt[0][pr], in_=pbanks[bk_g0][:, :], func=AF.Sigmoid, bias=bgate[:, dc:dc + 1]),
                     reads=[PS(bk_g0), cres(bgate)], writes=[("gt", 0, pr)])
                for c in range(8):
                    S.op("pe", lambda e, c=c: e.matmul(pbanks[bk_a][:, :], lhsT=wa[b][:, c, :], rhs=QT[:, c, qs], start=(c == 0), stop=(c == 7)),
                         reads=[("wa", b)] + ATR, writes=[PS(bk_a)])
                S.op("dve", lambda e: e.tensor_tensor(out=mt[0][pr], in0=pbanks[bk_a][:, :], in1=gt[0][pr], op=ALU.mult),
                     reads=[PS(bk_a), ("gt", 0, pr)], writes=[("mt", 0, pr)])
                for c in range(8):
                    S.op("pe", lambda e, c=c: e.matmul(pbanks[bk_g1][:, :], lhsT=wg1[b][:, c, :], rhs=hnT[:, c, cs], start=(c == 0), stop=(c == 7)),
                         reads=[("wg1", b)] + HNT_ALL, writes=[PS(bk_g1)])
                S.op("act", lambda e: e.activation(out=gt[1][pr], in_=pbanks[bk_g1][:, :], func=AF.Sigmoid, bias=bgate[:, 8 + dc:9 + dc]),
                     reads=[PS(bk_g1), cres(bgate)], writes=[("gt", 1, pr)])
                for c in range(4):
                    S.op("pe", lambda e, c=c: e.matmul(pbanks[bk_p][:, :], lhsT=wp[b][:, c, :], rhs=PT[:, c, qs], start=(c == 0), stop=(c == 3)),
                         reads=[("wp", b)] + [("PT", g) for g in range(NPG)], writes=[PS(bk_p)])
                S.op("dve", lambda e: e.tensor_tensor(out=mt[1][pr], in0=pbanks[bk_p][:, :], in1=gt[1][pr], op=ALU.mult),
                     reads=[PS(bk_p), ("gt", 1, pr)], writes=[("mt", 1, pr)])
                S.op("pool", lambda e: e.tensor_tensor(out=mixT[:, dc, qs], in0=mt[0][pr], in1=mt[1][pr], op=ALU.add),
                     reads=[("mt", 0, pr), ("mt", 1, pr)], writes=[("mixT", j)])

            load_e1(0)
            cnt = 0
            for dc in range(8):
                b = dc % 2
                if dc + 1 < 8:
                    load_e1(dc + 1)
                if dc == 1:
                    S.dma("pool", "miscP", lambda e: e.dma_start(out=wout_sb, in_=wout_d.rearrange("(c p) n -> p c n", p=128)), writes=["wout"])
                for j in range(4):
                    e1_tile(dc, j, b, cnt % 2)
                    cnt += 1
            if "mixT" in dbg:
                o = dbg_out("mixT", [128, 8 * SEQ])
                S.dma("pool", "dbgP", lambda e, o=o: e.dma_start(out=o.rearrange("p (c t) -> p c t", c=8), in_=mixT), reads=[("mixT", j) for j in range(4)], writes=["dbg_mixT"])

        if active("E2"):
            S.barrier()
            rt.reset()
            wout_sb = rt.get([8, D], BF16)
            x2 = [rt.get([D], F32) for _ in range(2)]
            g2bc = rt.get([D], F32)
            junk2 = rt.get([D], BF16)
            hn2 = [rt.get([D], BF16) for _ in range(2)]
            S.dma("sp", "miscS", lambda e: e.dma_start(out=g2bc, in_=g2_d.to_broadcast([128, D])), writes=["g2bc"])
            if active("F"):
                S.dma("pool", ("exw", 1), lambda e: e.dma_start(out=wup[1], in_=weu_d[0].rearrange("(c p) n -> p c n", p=128)), writes=[("wup", 1)])
                S.dma("pool", ("exw", 1), lambda e: e.dma_start(out=wdn[1], in_=wed_d[0].rearrange("(c p) n -> p c n", p=128)), writes=[("wdn", 1)])
            MIXR = [("mixT", j) for j in range(4)]
            def e2_s1(i):
                b = i % 2
                S.dma("sp", ("x2", b), lambda e: e.dma_start(out=x2[b], in_=x_d[i * 128:(i + 1) * 128, :]), writes=[("x2", b)])
                for half in range(2):
                    bk = (2 * i + half) % 4
                    for c in range(8):
                        S.op("pe", lambda e, c=c, half=half, bk=bk: e.matmul(pbanks[bk][:, :], lhsT=mixT[:, c, i * 128:(i + 1) * 128],
                                                                             rhs=wout_sb[:, c, half * 512:(half + 1) * 512], start=(c == 0), stop=(c == 7)),
                             reads=["wout"] + MIXR, writes=[PS(bk)])
                    S.op("dve", lambda e, half=half, bk=bk: e.tensor_tensor(out=h1[:, i, half * 512:(half + 1) * 512], in0=pbanks[bk][:, :],
                                                                            in1=x2[b][:, half * 512:(half + 1) * 512], op=ALU.add),
                         reads=[PS(bk), ("x2", b)], writes=[("h1", i)])
                S.op("act", lambda e: e.activation(out=junk2, in_=h1[:, i, :], func=AF.Square, accum_out=stats2[:, i, 0:1]),
                     reads=[("h1", i)], writes=["junk2", ("s2", i)])
                S.op("act", lambda e: e.activation(out=stats2[:, i, 1:2], in_=stats2[:, i, 0:1], func=AF.Sqrt, scale=1.0 / D, bias=EPS),
                     reads=[("s2", i)], writes=[("s21", i)])
                S.op("dve", lambda e: e.reciprocal(out=stats2[:, i, 1:2], in_=stats2[:, i, 1:2]), reads=[("s21", i)], writes=[("s21", i)])
                S.op("dve", lambda e: e.scalar_tensor_tensor(out=hn2[b], in0=h1[:, i, :], scalar=stats2[:, i, 1:2], in1=g2bc, op0=ALU.mult, op1=ALU.mult),
                     reads=[("h1", i), ("s21", i), "g2bc"], writes=[("hn2", b)])

            def e2_s2(i):
                b = i % 2
                pbk = 4 + i % 2
                for c in range(8):
                    S.op("pe", lambda e, c=c: e.transpose(out=pbf(pbk)[:, c * 128:(c + 1) * 128], in_=hn2[b][:, c * 128:(c + 1) * 128], identity=ident[:, :]),
                         reads=[("hn2", b), cres(ident)], writes=[PS(pbk)])
                S.op("act", lambda e: e.copy(out=hn2T[:, :, i * 128:(i + 1) * 128], in_=pbf(pbk).rearrange("p (c n) -> p c n", c=8)),
                     reads=[PS(pbk)], writes=[("hn2T", i)])

            def e2_s3(i):
                rbk = 6 + i % 2
                for c in range(8):
                    S.op("pe", lambda e, c=c: e.matmul(pbanks[rbk][:, 0:20], lhsT=hn2T[:, c, i * 128:(i + 1) * 128], rhs=wr_sb[:, c, :],
                                                       start=(c == 0), stop=(c == 7)),
                         reads=[("hn2T", i), cres(wr_sb)], writes=[PS(rbk)])
                S.op("dve", lambda e: e.tensor_tensor(out=rl[:, i, :], in0=pbanks[rbk][:, 0:20], in1=br_sb[:, :], op=ALU.add),
                     reads=[PS(rbk), cres(br_sb)], writes=[("rl", i)])

            for it_ in range(16 + 2):
                if it_ < 16:
                    e2_s1(it_)
                if 0 <= it_ - 1 < 16:
                    e2_s2(it_ - 1)
                if 0 <= it_ - 2 < 16:
                    e2_s3(it_ - 2)
            if "h1" in dbg:
                o = dbg_out("h1", [128, 16 * D])
                S.dma("sp", "dbgS", lambda e, o=o: e.dma_start(out=o.rearrange("p (c t) -> p c t", c=16), in_=h1), reads=[("h1", i) for i in range(16)], writes=["dbg_h1"])
            if "rl" in dbg:
                o = dbg_out("rl", [128, 16 * 20])
                S.dma("sp", "dbgS", lambda e, o=o: e.dma_start(out=o.rearrange("p (c t) -> p c t", c=16), in_=rl), reads=[("rl", i) for i in range(16)], writes=["dbg_rl"])

        if active("F"):
            S.barrier()
            rt.reset()
            RL = [("rl", i) for i in range(16)]
            glog = rl[:, :, 0:4]
            gmx = rw[:, :, 0:1]
            gex = rw[:, :, 4:8]
            gsum = rw[:, :, 8:9]
            goh = rw[:, :, 12:16]
            esel = rw[:, :, 16:20]
            etmp = rw[:, :, 20:24]
            m1 = rw[:, :, 24:25]
            oh1 = rw[:, :, 28:32]
            e2v = rw[:, :, 32:36]
            m2 = rw[:, :, 36:37]
            oh2 = rw[:, :, 40:44]
            dd = rw[:, :, 44:45]
            w1 = rw[:, :, 45:46]
            w2_ = rw[:, :, 46:47]
            c4 = rw[:, :, 48:52]
            RW = "rw"
            V_ = S.op
            V_("dve", lambda e: e.tensor_reduce(out=gmx, in_=glog, axis=AX.X, op=ALU.max), reads=RL, writes=[RW])
            V_("dve", lambda e: e.tensor_tensor(out=gex, in0=glog, in1=gmx.to_broadcast([128, 16, 4]), op=ALU.subtract), reads=RL + [RW], writes=[RW])
            V_("dve", lambda e: e.tensor_tensor(out=goh, in0=glog, in1=gmx.to_broadcast([128, 16, 4]), op=ALU.is_ge), reads=RL + [RW], writes=[RW])
            V_("act", lambda e: e.activation(out=gex, in_=gex, func=AF.Exp), reads=[RW], writes=[RW])
            V_("dve", lambda e: e.tensor_reduce(out=gsum, in_=gex, axis=AX.X, op=ALU.add), reads=[RW], writes=[RW])
            V_("dve", lambda e: e.reciprocal(out=gsum, in_=gsum), reads=[RW], writes=[RW])
            for g in range(4):
                if g == 0:
                    V_("dve", lambda e: e.tensor_tensor(out=esel, in0=rl[:, :, 4:8], in1=goh[:, :, 0:1].to_broadcast([128, 16, 4]), op=ALU.mult), reads=RL + [RW], writes=[RW])
                else:
                    V_("dve", lambda e, g=g: e.tensor_tensor(out=etmp, in0=rl[:, :, 4 + 4 * g:8 + 4 * g], in1=goh[:, :, g:g + 1].to_broadcast([128, 16, 4]), op=ALU.mult),
                       reads=RL + [RW], writes=[RW])
                    V_("dve", lambda e: e.tensor_tensor(out=esel, in0=esel, in1=etmp, op=ALU.add), reads=[RW], writes=[RW])
            V_("dve", lambda e: e.tensor_reduce(out=m1, in_=esel, axis=AX.X, op=ALU.max), reads=[RW], writes=[RW])
            V_("dve", lambda e: e.tensor_tensor(out=oh1, in0=esel, in1=m1.to_broadcast([128, 16, 4]), op=ALU.is_ge), reads=[RW], writes=[RW])
            V_("dve", lambda e: e.scalar_tensor_tensor(out=e2v, in0=oh1, scalar=-1e30, in1=esel, op0=ALU.mult, op1=ALU.add), reads=[RW], writes=[RW])
            V_("dve", lambda e: e.tensor_reduce(out=m2, in_=e2v, axis=AX.X, op=ALU.max), reads=[RW], writes=[RW])
            V_("dve", lambda e: e.tensor_tensor(out=oh2, in0=e2v, in1=m2.to_broadcast([128, 16, 4]), op=ALU.is_ge), reads=[RW], writes=[RW])
            V_("dve", lambda e: e.tensor_tensor(out=dd, in0=m2, in1=m1, op=ALU.subtract), reads=[RW], writes=[RW])
            V_("act", lambda e: e.activation(out=dd, in_=dd, func=AF.Exp), reads=[RW], writes=[RW])
            V_("dve", lambda e: e.tensor_scalar(out=w1, in0=dd, scalar1=1.0, scalar2=None, op0=ALU.add), reads=[RW], writes=[RW])
            V_("dve", lambda e: e.reciprocal(out=w1, in_=w1), reads=[RW], writes=[RW])
            V_("dve", lambda e: e.tensor_tensor(out=w1, in0=w1, in1=gsum, op=ALU.mult), reads=[RW], writes=[RW])
            V_("dve", lambda e: e.tensor_tensor(out=w2_, in0=w1, in1=dd, op=ALU.mult), reads=[RW], writes=[RW])
            V_("dve", lambda e: e.tensor_tensor(out=c4, in0=oh1, in1=w1.to_broadcast([128, 16, 4]), op=ALU.mult), reads=[RW], writes=[RW])
            V_("dve", lambda e: e.tensor_tensor(out=etmp, in0=oh2, in1=w2_.to_broadcast([128, 16, 4]), op=ALU.mult), reads=[RW], writes=[RW])
            V_("dve", lambda e: e.tensor_tensor(out=c4, in0=c4, in1=etmp, op=ALU.add), reads=[RW], writes=[RW])
            for g in range(4):
                V_("dve", lambda e, g=g: e.tensor_tensor(out=comb[:, :, 4 * g:4 * g + 4], in0=c4, in1=goh[:, :, g:g + 1].to_broadcast([128, 16, 4]), op=ALU.mult),
                   reads=[RW], writes=["comb"])
            if "comb" in dbg:
                o = dbg_out("comb", [128, 16 * 16])
                S.dma("sp", "dbgS", lambda e, o=o: e.dma_start(out=o.rearrange("p (c t) -> p c t", c=16), in_=comb), reads=["comb"], writes=["dbg_comb"])

            sg = [rt.get([512], F32) for _ in range(2)]
            At = [rt.get([4, 512], BF16) for _ in range(2)]
            HN2R = [("hn2T", i) for i in range(16)]

            def load_expert(ex):
                b = (ex + 1) % 2
                S.dma("pool", ("exw", b), lambda e: e.dma_start(out=wgt[b], in_=weg_d[ex].rearrange("(c p) n -> p c n", p=128)), writes=[("wgt", b)])
                if ex == 0:
                    return
                S.dma("pool", ("exw", b), lambda e: e.dma_start(out=wup[b], in_=weu_d[ex].rearrange("(c p) n -> p c n", p=128)), writes=[("wup", b)])
                S.dma("pool", ("exw", b), lambda e: e.dma_start(out=wdn[b], in_=wed_d[ex].rearrange("(c p) n -> p c n", p=128)), writes=[("wdn", b)])

            do_g = active("G")
            if do_g:
                gfbc = rt.get([D], F32)
                junk3 = rt.get([D], BF16)
                ot = [rt.get([D], F32) for _ in range(2)]
                S.dma("sp", "miscS", lambda e: e.dma_start(out=gfbc, in_=gf_d.to_broadcast([128, D])), writes=["gfbc"])

            def g_tile(i):
                b = i % 2
                S.op("act", lambda e: e.activation(out=junk3, in_=h1[:, i, :], func=AF.Square, accum_out=stats3[:, i, 0:1]),
                     reads=[("h1", i)], writes=["junk3", ("s3", i)])
                S.op("act", lambda e: e.activation(out=stats3[:, i, 1:2], in_=stats3[:, i, 0:1], func=AF.Sqrt, scale=1.0 / D, bias=EPS),
                     reads=[("s3", i)], writes=[("s31", i)])
                S.op("dve", lambda e: e.reciprocal(out=stats3[:, i, 1:2], in_=stats3[:, i, 1:2]), reads=[("s31", i)], writes=[("s31", i)])
                S.op("dve", lambda e: e.scalar_tensor_tensor(out=ot[b], in0=h1[:, i, :], scalar=stats3[:, i, 1:2], in1=gfbc, op0=ALU.mult, op1=ALU.mult),
                     reads=[("h1", i), ("s31", i), "gfbc"], writes=[("ot", b)])
                S.dma("sp", ("st", b), lambda e: e.dma_start(out=out_d[i * 128:(i + 1) * 128, :], in_=ot[b]), reads=[("ot", b)], writes=[("out", i)])

            cnt_f = [0, 0]
            def f_gu(ex, j, b, ab):
                ts_ = slice(j * 512, (j + 1) * 512)
                for f in range(4):
                    pr = cnt_f[0] % 2
                    cnt_f[0] += 1
                    bg, bu = 0 + pr, 2 + pr
                    for c in range(8):
                        S.op("pe", lambda e, c=c, f=f, bg=bg: e.matmul(pbanks[bg][:, :], lhsT=wgt[b][:, c, f * 128:(f + 1) * 128], rhs=hn2T[:, c, ts_], start=(c == 0), stop=(c == 7)),
                             reads=[("wgt", b)] + HN2R, writes=[PS(bg)])
                    S.op("act", lambda e, pr=pr, bg=bg: e.activation(out=sg[pr], in_=pbanks[bg][:, :], func=AF.Silu), reads=[PS(bg)], writes=[("sg", pr)])
                    for c in range(8):
                        S.op("pe", lambda e, c=c, f=f, bu=bu: e.matmul(pbanks[bu][:, :], lhsT=wup[b][:, c, f * 128:(f + 1) * 128], rhs=hn2T[:, c, ts_], start=(c == 0), stop=(c == 7)),
                             reads=[("wup", b)] + HN2R, writes=[PS(bu)])
                    S.op("dve", lambda e, pr=pr, f=f, bu=bu: e.tensor_tensor(out=At[ab][:, f, :], in0=pbanks[bu][:, :], in1=sg[pr], op=ALU.mult),
                         reads=[PS(bu), ("sg", pr)], writes=[("At", ab)])

            def f_down(ex, j, b, ab):
                for sub in range(4):
                    i = 4 * j + sub
                    for half in range(2):
                        bd = 4 + cnt_f[1] % 4
                        cnt_f[1] += 1
                        for f in range(4):
                            S.op("pe", lambda e, f=f, sub=sub, half=half, bd=bd: e.matmul(pbanks[bd][:, :], lhsT=At[ab][:, f, sub * 128:(sub + 1) * 128],
                                                                                          rhs=wdn[b][:, f, half * 512:(half + 1) * 512], start=(f == 0), stop=(f == 3)),
                                 reads=[("At", ab), ("wdn", b)], writes=[PS(bd)])
                        S.op("dve", lambda e, i=i, half=half, bd=bd, ex=ex: e.scalar_tensor_tensor(out=h1[:, i, half * 512:(half + 1) * 512], in0=pbanks[bd][:, :],
                                                                                                  scalar=comb[:, i, ex:ex + 1], in1=h1[:, i, half * 512:(half + 1) * 512],
                                                                                                  op0=ALU.mult, op1=ALU.add),
                             reads=[PS(bd), "comb", ("h1", i)], writes=[("h1", i)])
                    if do_g and ex == NE - 1:
                        g_tile(i)

            load_expert(0)
            fitems = [(ex, j) for ex in range(NE) for j in range(4)]
            for idx in range(len(fitems) + 1):
                if idx < len(fitems):
                    ex, j = fitems[idx]
                    f_gu(ex, j, (ex + 1) % 2, idx % 2)
                if idx >= 1:
                    exp_, jp_ = fitems[idx - 1]
                    f_down(exp_, jp_, (exp_ + 1) % 2, (idx - 1) % 2)
                if idx < len(fitems):
                    ex, j = fitems[idx]
                    if j == 0 and ex + 1 < NE:
                        load_expert(ex + 1)
            if "h2" in dbg:
                o = dbg_out("h2", [128, 16 * D])
                S.dma("sp", "dbgS", lambda e, o=o: e.dma_start(out=o.rearrange("p (c t) -> p c t", c=16), in_=h1), reads=[("h1", i) for i in range(16)], writes=["dbg_h2"])

        if active("G") and not active("F"):
            raise RuntimeError("phase G is fused into phase F")

        S.finish("sp")
        nsig = S.emit(nc)
    return nc, dbg_d, nsig


def _consts():
    f32 = np.float32
    inv = (1.0 / (10000.0 ** (np.arange(0, HD, 2, dtype=f32) / f32(HD)))).astype(f32)
    cos_t = np.zeros((128, TC), f32)
    sin_t = np.zeros((128, TC), f32)
    pos = np.arange(SEQ + NMETA, dtype=f32)
    ang = (pos[:, None] * inv[None, :]).astype(f32)
    ang = np.concatenate([ang, ang], axis=-1)
    c = np.cos(ang).astype(f32).T
    s = np.sin(ang).astype(f32).T
    s_signed = s.copy()
    s_signed[:HD // 2] *= -1.0
    for half in range(2):
        cos_t[half * 64:(half + 1) * 64, MOFF:] = c
        sin_t[half * 64:(half + 1) * 64, MOFF:] = s_signed
    ident = np.eye(128, dtype=f32)
    perm = np.zeros((128, 128), f32)
    for n2 in range(128):
        blk, d = divmod(n2, 64)
        perm[blk * 64 + (d + 32) % 64, n2] = 1.0
    k_ = np.arange(128)[:, None]
    q_ = np.arange(128)[None, :]
    tri = (q_ >= k_).astype(f32)
    tri = np.concatenate([tri, tri], axis=1)
    ones = np.ones((128, 128), f32)
    ones16 = np.zeros((128, 128), f32)
    ones16[MOFF:, :] = 1.0
    negm = np.where(q_ >= k_, 0.0, -30000.0).astype(f32)
    return dict(cos_t=cos_t, sin_t=sin_t, c_ident=ident, c_perm=perm, c_tri=tri, c_ones=ones, c_ones16=ones16, c_negm=negm)


def make_in_maps(inputs, n_cores=8):
    f = lambda a: np.ascontiguousarray(np.asarray(a, dtype=np.float32))
    x = f(inputs["x"])
    shared = dict(
        meta=f(inputs["meta"]),
        norm1_g=f(inputs["norm1_g"]).reshape(1, D),
        w_in=f(inputs["w_in"]).reshape(D, IN_W),
        b_gate_t=np.ascontiguousarray(f(inputs["b_gate"]).reshape(16, 128).T),
        lambda_q1=f(inputs["lambda_q1"]).reshape(1, HD),
        lambda_k1=f(inputs["lambda_k1"]).reshape(1, HD),
        lambda_q2=f(inputs["lambda_q2"]).reshape(1, HD),
        lambda_k2=f(inputs["lambda_k2"]).reshape(1, HD),
        subln_g_t=f(inputs["subln_g"]).reshape(128, 1),
        pool_w=f(inputs["pool_w"]).reshape(NPG, 128, 128),
        pool_scale_t=np.ascontiguousarray(f(inputs["pool_scale"]).reshape(NPG, 128).T),
        w_attn_br=f(inputs["w_attn_br"]).reshape(D, D),
        w_pool_br=f(inputs["w_pool_br"]).reshape(512, D),
        w_out=f(inputs["w_out"]).reshape(D, D),
        norm2_g=f(inputs["norm2_g"]).reshape(1, D),
        w_router=np.ascontiguousarray(np.concatenate([f(inputs["w_router_group"]).reshape(D, 4), f(inputs["w_router_expert"]).reshape(D, 16)], axis=1)),
        b_router=np.ascontiguousarray(np.concatenate([f(inputs["b_router_group"]).reshape(1, 4), f(inputs["b_router_expert"]).reshape(1, 16)], axis=1)),
        w_e_gate=f(inputs["w_e_gate"]).reshape(NE, D, DE),
        w_e_up=f(inputs["w_e_up"]).reshape(NE, D, DE),
        w_e_down=f(inputs["w_e_down"]).reshape(NE, DE, D),
        final_g=f(inputs["final_g"]).reshape(1, D),
    )
    shared.update(_consts())
    maps = []
    for b in range(n_cores):
        m = dict(shared)
        m["x"] = np.ascontiguousarray(x[b])
        maps.append(m)
    return maps


_CACHE = {}


def kernel(**inputs):
    if "nc" not in _CACHE:
        _CACHE["nc"] = build_program()[0]
    nc = _CACHE["nc"]
    maps = make_in_maps(inputs, 8)
    res = run_bass_kernel_spmd(nc, maps, core_ids=list(range(8)))
    out = np.stack([np.asarray(r["out"], dtype=np.float32) for r in res.results], axis=0)
    return out
```

```python
import contextlib
import math

import numpy as np

import concourse.bass as bass
import concourse.mybir as mybir
from concourse.bass_utils import run_bass_kernel_spmd

F32 = mybir.dt.float32
BF16 = mybir.dt.bfloat16
AF = mybir.ActivationFunctionType
ALU = mybir.AluOpType
AX = mybir.AxisListType

D = 1024
SEQ = 2048
NMETA = 16
NH = 8
HD = 64
VD = 128
NPG = 4
POOL_W = (2, 4, 8, 16)
IN_W = 5632
NE = 16
DE = 512
EPS = 1e-6
TC = 2176
MOFF = 112
ROFF = 128
O_Q, O_K, O_V, O_P, O_G = 0, 1024, 2048, 3072, 3584
LAMBDA_INIT = 0.8 - 0.6 * math.exp(-0.3 * 0)
EPOCH = 12000


class _Op:
    __slots__ = ("eng", "fn", "waits", "signal", "idx", "dma_stream", "sigcount")

    def __init__(self, eng, fn):
        self.eng = eng
        self.fn = fn
        self.waits = []
        self.signal = False
        self.idx = -1
        self.dma_stream = None
        self.sigcount = 0


class Sched:
    ENGS = ("pe", "act", "dve", "pool", "sp")

    def __init__(self):
        self.ops = {e: [] for e in self.ENGS}
        self.last_w = {}
        self.readers = {}
        self.known = {e: {} for e in self.ENGS}
        self.streams = {}
        self.pending = {e: [] for e in self.ENGS}

    def _deps(self, eng, reads, writes, is_dma=False):
        deps = {}

        def add(kind, src, n, raw):
            if kind == "e" and src == eng and not is_dma:
                if eng == "pe" or eng == "sp":
                    return
            if kind == "d":
                n = self.streams[src]
            key = (kind, src)
            if deps.get(key, -1) < n:
                deps[key] = n

        for r in reads:
            ev = self.last_w.get(r)
            if ev is not None:
                add(ev[0], ev[1], ev[2], True)
        for w in writes:
            ev = self.last_w.get(w)
            if ev is not None:
                add(ev[0], ev[1], ev[2], False)
            rd = self.readers.get(w)
            if rd:
                for key, n in rd.items():
                    add(key[0], key[1], n, False)
        for (kind, src, n) in self.pending[eng]:
            if kind == "e" and src == eng and not is_dma:
                continue
            key = (kind, src)
            if deps.get(key, -1) < n:
                deps[key] = n
        self.pending[eng] = []
        out = []
        kn = self.known[eng]
        for key, n in deps.items():
            if kn.get(key, -1) >= n:
                continue
            kn[key] = n
            out.append((key[0], key[1], n))
        return out

    def _commit(self, ev, reads, writes):
        for w in writes:
            self.last_w[w] = ev
            self.readers[w] = {}
        key = (ev[0], ev[1])
        for r in reads:
            d = self.readers.setdefault(r, {})
            if d.get(key, -1) < ev[2]:
                d[key] = ev[2]

    def _add(self, eng, fn, reads, writes, stream):
        ps_reads = [r for r in reads if isinstance(r, tuple) and r[0] == "ps"]
        if ps_reads:
            writes = list(writes) + ps_reads
        o = _Op(eng, fn)
        o.waits = self._deps(eng, reads, writes, is_dma=stream is not None)
        lst = self.ops[eng]
        o.idx = len(lst)
        lst.append(o)
        for (kind, src, n) in o.waits:
            if kind == "e":
                self.ops[src][n].signal = True
        if stream is None:
            ev = ("e", eng, o.idx)
        else:
            c = self.streams.get(stream, 0) + 1
            self.streams[stream] = c
            o.dma_stream = stream
            ev = ("d", stream, c)
        self._commit(ev, reads, writes)
        return o

    def op(self, eng, fn, reads=(), writes=()):
        return self._add(eng, fn, reads, writes, None)

    def dma(self, queue, stream, fn, reads=(), writes=()):
        return self._add(queue, fn, reads, writes, stream)

    def barrier(self):
        evs = []
        for e in self.ENGS:
            if self.ops[e]:
                evs.append(("e", e, len(self.ops[e]) - 1))
        for s, c in self.streams.items():
            evs.append(("d", s, c))
        for e in self.ENGS:
            self.pending[e] = list(evs)
        fixed = []
        for (kind, src, n) in evs:
            if kind == "e":
                i = n
                while i >= 0 and self.ops[src][i].dma_stream is not None:
                    i -= 1
                if i < 0:
                    continue
                fixed.append(("e", src, i))
            else:
                fixed.append((kind, src, n))
        for e in self.ENGS:
            self.pending[e] = list(fixed)

    def finish(self, eng="sp"):
        self.barrier()
        o = _Op(eng, None)
        o.waits = self._deps(eng, (), ())
        for (kind, src, n) in o.waits:
            if kind == "e":
                self.ops[src][n].signal = True
        o.idx = len(self.ops[eng])
        self.ops[eng].append(o)

    def emit(self, nc):
        for e, lst in self.ops.items():
            c = 0
            for o in lst:
                if o.signal:
                    c += 1
                o.sigcount = c
        nsig = {e: (lst[-1].sigcount if lst else 0) for e, lst in self.ops.items()}
        with contextlib.ExitStack() as st:
            esems = {}
            for e in self.ENGS:
                n_ep = nsig[e] // EPOCH + 1
                esems[e] = [st.enter_context(nc.semaphore(f"s_{e}_{k}")) for k in range(n_ep)]
            dsems = {s: st.enter_context(nc.semaphore("d_" + "_".join(str(x) for x in (s if isinstance(s, tuple) else (s,))))) for s in self.streams}
            block = st.enter_context(nc.Block())

            def run(engname):
                def body(eng):
                    for o in self.ops[engname]:
                        for (kind, src, n) in o.waits:
                            if kind == "e":
                                sc = self.ops[src][n].sigcount
                                ep, v = (sc - 1) // EPOCH, (sc - 1) % EPOCH + 1
                                eng.wait_ge(esems[src][ep], v)
                            else:
                                eng.wait_ge(dsems[src], 16 * n)
                        if o.fn is None:
                            continue
                        ins = o.fn(eng)
                        if o.dma_stream is not None:
                            ins.then_inc(dsems[o.dma_stream], 16)
                        elif o.signal:
                            ep = (o.sigcount - 1) // EPOCH
                            ins.then_inc(esems[engname][ep], 1)
                return body

            block.tensor(run("pe"))
            block.scalar(run("act"))
            block.vector(run("dve"))
            block.gpsimd(run("pool"))
            block.sync(run("sp"))
        return nsig


def build_program(dbg=(), stop_after=None):
    nc = bass.Bass("TRN2", target_bir_lowering=False)
    dbg = set(dbg)

    def din(name, shape, dt=F32):
        return nc.dram_tensor(name, list(shape), dt, kind="ExternalInput").ap()

    x_d = din("x", [SEQ, D])
    meta_d = din("meta", [NMETA, D])
    g1_d = din("norm1_g", [1, D])
    w_in_d = din("w_in", [D, IN_W])
    bgate_d = din("b_gate_t", [128, 16])
    lq1_d = din("lambda_q1", [1, HD])
    lk1_d = din("lambda_k1", [1, HD])
    lq2_d = din("lambda_q2", [1, HD])
    lk2_d = din("lambda_k2", [1, HD])
    subg_d = din("subln_g_t", [128, 1])
    poolw_d = din("pool_w", [NPG, 128, 128])
    pscale_d = din("pool_scale_t", [128, NPG])
    wattn_d = din("w_attn_br", [D, D])
    wpool_d = din("w_pool_br", [512, D])
    wout_d = din("w_out", [D, D])
    g2_d = din("norm2_g", [1, D])
    wr_d = din("w_router", [D, 20])
    br_d = din("b_router", [1, 20])
    weg_d = din("w_e_gate", [NE, D, DE])
    weu_d = din("w_e_up", [NE, D, DE])
    wed_d = din("w_e_down", [NE, DE, D])
    gf_d = din("final_g", [1, D])
    cos_d = din("cos_t", [128, TC])
    sin_d = din("sin_t", [128, TC])
    ident_d = din("c_ident", [128, 128])
    perm_d = din("c_perm", [128, 128])
    tri_d = din("c_tri", [128, 256])
    ones_d = din("c_ones", [128, 128])
    ones16_d = din("c_ones16", [128, 128])
    negm_d = din("c_negm", [128, 128])
    out_d = nc.dram_tensor("out", [SEQ, D], F32, kind="ExternalOutput").ap()
    dbg_d = {}

    def dbg_out(name, shape):
        dbg_d[name] = nc.dram_tensor("dbg_" + name, list(shape), F32, kind="ExternalOutput").ap()
        return dbg_d[name]

    S = Sched()
    phases = ["A", "B0", "B1", "D", "E1", "E2", "F", "G"]
    last_phase = stop_after or "G"

    def active(ph):
        return phases.index(ph) <= phases.index(last_phase)

    with contextlib.ExitStack() as st:
        def sb(name, shape, dt):
            return st.enter_context(nc.sbuf_tensor(name, list(shape), dt))

        R1 = sb("R1", [128, 34816 // 4], F32)
        R42 = sb("R42", [128, 67584 // 4], F32)
        R35 = sb("R35", [128, 51200 // 4], F32)
        RT_BYTES = 50 * 1024
        RT = sb("RT", [128, RT_BYTES // 4], F32)

        def view(region, off_bytes, shape, dt):
            n = 1
            for s_ in shape:
                n *= s_
            if dt == F32:
                assert off_bytes % 4 == 0
                a = region[:, off_bytes // 4: off_bytes // 4 + n]
            else:
                assert off_bytes % 4 == 0 and (n * 2) % 4 == 0
                a = region[:, off_bytes // 4: off_bytes // 4 + (n * 2) // 4].bitcast(BF16)
            if len(shape) == 2:
                a = a.rearrange("p (a b) -> p a b", a=shape[0])
            elif len(shape) == 3:
                a = a.rearrange("p (a b c) -> p a b c", a=shape[0], b=shape[1])
            return a

        class RTAlloc:
            def __init__(self):
                self.off = 0

            def reset(self):
                self.off = 0

            def get(self, shape, dt):
                n = 1
                for s_ in shape:
                    n *= s_
                nb = n * (4 if dt == F32 else 2)
                nb = (nb + 31) // 32 * 32
                v = view(RT, self.off, shape, dt)
                self.off += nb
                assert self.off <= RT_BYTES, ("RT overflow", self.off)
                return v

        rt = RTAlloc()
        _tail = RT_BYTES - (1280 + 4096 + 1024)
        rl = view(RT, _tail, [16, 20], F32)
        rw = view(RT, _tail + 1280, [16, 64], F32)
        comb = view(RT, _tail + 1280 + 4096, [16, 16], F32)

        hnT = view(R1, 0, [8, TC], BF16)
        hn2T = view(R1, 0, [8, SEQ], BF16)
        Vt = view(R42, 0, [17, D], BF16)
        QT = view(R42, 34816, [8, SEQ], BF16)
        h1 = view(R42, 0, [16, D], F32)
        KT = view(R35, 0, [8, TC], BF16)
        mixT = view(R35, 0, [8, SEQ], BF16)
        PT = view(R35, 34816, [NPG, SEQ], BF16)
        WB = 24576
        wgt = [view(R35, k * WB, [8, DE], BF16) for k in range(2)]
        wup = [view(R35, k * WB + 8192, [8, DE], BF16) for k in range(2)]
        wdn = [view(R35, k * WB + 16384, [4, D], BF16) for k in range(2)]

        ident = sb("ident", [128, 128], BF16)
        permm = sb("permm", [128, 128], BF16)
        tri = sb("tri", [128, 2, 128], BF16)
        ones = sb("ones", [128, 128], BF16)
        ones16 = sb("ones16", [128, 128], BF16)
        negm = sb("negm", [128, 128], BF16)
        poolw = sb("poolw", [128, NPG, 128], BF16)
        bgate = sb("bgate", [128, 16], F32)
        pscale = sb("pscale", [128, NPG], F32)
        subg = sb("subg", [128, 1], F32)
        lamt = sb("lamt", [128, 4 * HD], F32)
        lams = sb("lams", [128, 8], F32)
        stats = sb("stats", [128, 17, 2], F32)
        stats2 = sb("stats2", [128, 16, 2], F32)
        stats3 = sb("stats3", [128, 16, 2], F32)
        wr_sb = sb("wr_sb", [128, 8, 20], BF16)
        br_sb = sb("br_sb", [128, 20], F32)

        _pb = [st.enter_context(nc.psum_tensor(f"pb{i}", [128, 512], F32)) for i in range(4)]
        pS = [st.enter_context(nc.psum_tensor(f"pS{i}", [128, 1024], F32)) for i in range(2)]
        pbanks = [t[:, :] for t in _pb] + [pS[0][:, 0:512], pS[0][:, 512:1024], pS[1][:, 0:512], pS[1][:, 512:1024]]

        def pbf(i):
            return pbanks[i].bitcast(BF16)

        cres = lambda t: ("c", t.name)
        for t, d_ in ((ident, ident_d), (permm, perm_d), (ones, ones_d), (ones16, ones16_d), (negm, negm_d)):
            S.dma("pool", "constP", lambda e, t=t, d_=d_: e.dma_start(out=t[:, :], in_=d_), writes=[cres(t)])
        S.dma("pool", "constP", lambda e: e.dma_start(out=tri[:, :, :], in_=tri_d.rearrange("p (a b) -> p a b", a=2)), writes=[cres(tri)])
        S.dma("pool", "constP", lambda e: e.dma_start(out=poolw[:, :, :], in_=poolw_d.rearrange("g c d -> c g d")), writes=[cres(poolw)])
        S.dma("pool", "constP", lambda e: e.dma_start(out=wr_sb[:, :, :], in_=wr_d.rearrange("(c p) n -> p c n", p=128)), writes=[cres(wr_sb)])
        for t, d_ in ((bgate, bgate_d), (pscale, pscale_d), (subg, subg_d)):
            S.dma("sp", "constS", lambda e, t=t, d_=d_: e.dma_start(out=t[:, :], in_=d_), writes=[cres(t)])
        S.dma("sp", "constS", lambda e: e.dma_start(out=br_sb[:, :], in_=br_d.to_broadcast([128, 20])), writes=[cres(br_sb)])
        for i, d_ in enumerate((lq1_d, lk1_d, lq2_d, lk2_d)):
            S.dma("sp", "constS", lambda e, i=i, d_=d_: e.dma_start(out=lamt[:, i * HD:(i + 1) * HD], in_=d_.to_broadcast([128, HD])), writes=[("lamt", i)])

        S.op("dve", lambda e: e.tensor_tensor(out=lamt[:, 0:HD], in0=lamt[:, 0:HD], in1=lamt[:, HD:2 * HD], op=ALU.mult),
             reads=[("lamt", 0), ("lamt", 1)], writes=[("lamt", 0)])
        S.op("dve", lambda e: e.tensor_tensor(out=lamt[:, 2 * HD:3 * HD], in0=lamt[:, 2 * HD:3 * HD], in1=lamt[:, 3 * HD:4 * HD], op=ALU.mult),
             reads=[("lamt", 2), ("lamt", 3)], writes=[("lamt", 2)])
        S.op("dve", lambda e: e.reduce_sum(out=lams[:, 0:1], in_=lamt[:, 0:HD], axis=AX.X), reads=[("lamt", 0)], writes=[("lams", 0)])
        S.op("dve", lambda e: e.reduce_sum(out=lams[:, 1:2], in_=lamt[:, 2 * HD:3 * HD], axis=AX.X), reads=[("lamt", 2)], writes=[("lams", 1)])
        S.op("act", lambda e: e.activation(out=lams[:, 2:4], in_=lams[:, 0:2], func=AF.Exp), reads=[("lams", 0), ("lams", 1)], writes=[("lams", 2)])
        S.op("dve", lambda e: e.scalar_tensor_tensor(out=lams[:, 4:5], in0=lams[:, 3:4], scalar=-LAMBDA_INIT, in1=lams[:, 2:3],
                                                     op0=ALU.add, op1=ALU.subtract), reads=[("lams", 2)], writes=[("lams", 4)])
        S.op("dve", lambda e: e.tensor_scalar(out=lams[:, 5:6], in0=subg[:, 0:1], scalar1=1.0 - LAMBDA_INIT, scalar2=None, op0=ALU.mult),
             reads=[cres(subg)], writes=[("lams", 5)])
        neglam = lams[:, 4:5]
        gsub = lams[:, 5:6]

        def PS(i):
            return ("ps", i)

        rt.reset()
        wblk = [rt.get([8, 512], BF16) for _ in range(2)]
        S.dma("pool", ("wld", 0), lambda e: e.dma_start(out=wblk[0], in_=w_in_d[:, O_P:O_P + 512].rearrange("(c p) n -> p c n", p=128)), writes=[("wblk", 0)])
        S.dma("pool", ("wld", 1), lambda e: e.dma_start(out=wblk[1], in_=w_in_d[:, O_Q:O_Q + 512].rearrange("(c p) n -> p c n", p=128)), writes=[("wblk", 1)])
        g1bc = rt.get([D], F32)
        NBA = 4
        xt = [rt.get([D], F32) for _ in range(NBA)]
        junk = rt.get([D], BF16)
        xn = [rt.get([D], BF16) for _ in range(NBA)]
        S.dma("sp", "miscS", lambda e: e.dma_start(out=g1bc, in_=g1_d.to_broadcast([128, D])), writes=["g1bc"])
        S.op("pool", lambda e: e.memset(xt[0], 0.0), writes=[("xt", 0)])
        def a_s1(i):
            b = i % NBA
            if i == 0:
                S.dma("sp", ("xt", 0), lambda e: e.dma_start(out=xt[0][MOFF:128, :], in_=meta_d), writes=[("xt", 0)], reads=[])
            else:
                S.dma("sp", ("xt", b), lambda e: e.dma_start(out=xt[b], in_=x_d[(i - 1) * 128:i * 128, :]), writes=[("xt", b)])
            S.op("act", lambda e: e.activation(out=junk, in_=xt[b], func=AF.Square, accum_out=stats[:, i, 0:1]),
                 reads=[("xt", b)], writes=["junk", ("st", i)])
            S.op("act", lambda e: e.activation(out=stats[:, i, 1:2], in_=stats[:, i, 0:1], func=AF.Sqrt, scale=1.0 / D, bias=EPS),
                 reads=[("st", i)], writes=[("st1", i)])
            S.op("dve", lambda e: e.reciprocal(out=stats[:, i, 1:2], in_=stats[:, i, 1:2]), reads=[("st1", i)], writes=[("st1", i)])
            S.op("dve", lambda e: e.scalar_tensor_tensor(out=xn[b], in0=xt[b], scalar=stats[:, i, 1:2], in1=g1bc, op0=ALU.mult, op1=ALU.mult),
                 reads=[("xt", b), ("st1", i), "g1bc"], writes=[("xn", b)])

        def a_s2(i):
            b = i % NBA
            pbk = i % 4
            for c in range(8):
                S.op("pe", lambda e, c=c: e.transpose(out=pbf(pbk)[:, c * 128:(c + 1) * 128], in_=xn[b][:, c * 128:(c + 1) * 128], identity=ident[:, :]),
                     reads=[("xn", b), cres(ident)], writes=[PS(pbk)])
            S.op("act", lambda e: e.copy(out=hnT[:, :, i * 128:(i + 1) * 128], in_=pbf(pbk).rearrange("p (c n) -> p c n", c=8)),
                 reads=[PS(pbk)], writes=[("hnT", i)])

        for it_ in range(17 + 2):
            if it_ < 17:
                a_s1(it_)
            if 0 <= it_ - 2 < 17:
                a_s2(it_ - 2)
        HNT_ALL = [("hnT", i) for i in range(17)]

        if "hnT" in dbg:
            o = dbg_out("hnT", [128, 8 * TC])
            S.dma("pool", "dbgP", lambda e, o=o: e.dma_start(out=o.rearrange("p (c t) -> p c t", c=8), in_=hnT), reads=HNT_ALL, writes=["dbg_hnT"])

        def load_wblock(dst, src2d, c0, ncols, res, kch=8):
            S.dma("pool", ("wld", res[1]), lambda e: e.dma_start(out=dst, in_=src2d[:, c0:c0 + ncols].rearrange("(c p) n -> p c n", p=128)),
                  writes=[res])

        if active("B0"):
            S.barrier()
            rt.reset()
            wblk = [rt.get([8, 512], BF16) for _ in range(2)]
            u32 = [rt.get([TC], F32) for _ in range(2)]
            tmpA = rt.get([TC], F32)
            tmpB = rt.get([TC], F32)
            psrot = [0]

            def nextbank(lo, hi):
                b_ = lo + psrot[0] % (hi - lo)
                psrot[0] += 1
                return b_

            def b0_proj(g):
                ub = u32[g % 2]
                ures = ("u32", g % 2)
                for jt in range(5):
                    c0, n = (0, 128) if jt == 0 else (ROFF + (jt - 1) * 512, 512)
                    bk = nextbank(0, 4)
                    for c in range(8):
                        S.op("pe", lambda e, c=c, g=g, c0=c0, n=n, bk=bk: e.matmul(pbanks[bk][:, 0:n], lhsT=wblk[0][:, c, g * 128:(g + 1) * 128],
                                                                                 rhs=hnT[:, c, c0:c0 + n], start=(c == 0), stop=(c == 7)),
                             reads=[("wblk", 0)] + HNT_ALL, writes=[PS(bk)])
                    S.op("act", lambda e, ub=ub, c0=c0, n=n, bk=bk: e.copy(out=ub[:, c0:c0 + n], in_=pbanks[bk][:, 0:n]),
                         reads=[PS(bk)], writes=[ures])

            def b0_pool(g):
                ub = u32[g % 2]
                ures = ("u32", g % 2)
                w = POOL_W[g]
                L = TC
                cur, cur_res = ub, ures
                step = 1
                bufs = [(tmpA, "tmpA"), (tmpB, "tmpB")]
                k = 0
                while step < w:
                    dst, dres = bufs[k % 2]
                    lo = 2 * step - 1
                    S.op("dve", lambda e, dst=dst, cur=cur, lo=lo, step=step: e.tensor_tensor(out=dst[:, lo:L], in0=cur[:, lo:L], in1=cur[:, lo - step:L - step], op=ALU.add),
                         reads=[cur_res], writes=[dres])
                    cur, cur_res = dst, dres
                    step *= 2
                    k += 1
                ydst, yres = bufs[k % 2]
                ybf = ydst[:, 0:SEQ // 2].bitcast(BF16)
                S.op("dve", lambda e, ybf=ybf, cur=cur, ub=ub, w=w: e.scalar_tensor_tensor(out=ybf, in0=cur[:, ROFF:TC], scalar=1.0 / w, in1=ub[:, ROFF:TC],
                                                                                            op0=ALU.mult, op1=ALU.subtract),
                     reads=[cur_res, ures], writes=[yres])
                for jt in range(4):
                    bk = nextbank(0, 4)
                    S.op("pe", lambda e, g=g, jt=jt, bk=bk, ybf=ybf: e.matmul(pbanks[bk][:, :], lhsT=poolw[:, g, :], rhs=ybf[:, jt * 512:(jt + 1) * 512], start=True, stop=True),
                         reads=[yres, cres(poolw)], writes=[PS(bk)])
                    S.op("act", lambda e, g=g, jt=jt, bk=bk: e.activation(out=PT[:, g, jt * 512:(jt + 1) * 512], in_=pbanks[bk][:, :], func=AF.Identity, scale=pscale[:, g:g + 1]),
                         reads=[PS(bk), cres(pscale)], writes=[("PT", g)])

            for it_ in range(NPG + 1):
                if it_ < NPG:
                    b0_proj(it_)
                if it_ >= 1:
                    b0_pool(it_ - 1)
            if "PT" in dbg:
                o = dbg_out("PT", [128, NPG * SEQ])
                S.dma("pool", "dbgP", lambda e, o=o: e.dma_start(out=o.rearrange("p (c t) -> p c t", c=NPG), in_=PT), reads=[("PT", g) for g in range(NPG)], writes=["dbg_PT"])

        if active("B1"):
            S.barrier()
            rt.reset()
            wblk = [rt.get([8, 512], BF16) for _ in range(2)]
            cosT = rt.get([TC], F32)
            sinT = rt.get([TC], F32)
            zb = [rt.get([512], BF16) for _ in range(3)]
            t1 = [rt.get([512], F32) for _ in range(3)]
            t2 = [rt.get([512], F32) for _ in range(3)]
            ZB_ = (0, 1, 6)
            RB_ = (2, 3, 7)
            S.dma("sp", "miscS", lambda e: e.dma_start(out=cosT, in_=cos_d), writes=["cosT"])
            S.dma("sp", "miscS", lambda e: e.dma_start(out=sinT, in_=sin_d), writes=["sinT"])
            blocks = [("q", 0), ("q", 1), ("k", 0), ("k", 1), ("v", 0), ("v", 1)]
            col_of = {"q": O_Q, "k": O_K, "v": O_V}
            items = []

            def stage1(it):
                kind, h, c0, n, wb, nchunk, slot = it
                sl = slot % 3
                bk = ZB_[sl]
                for c in range(8):
                    S.op("pe", lambda e, c=c: e.matmul(pbanks[bk][:, 0:n], lhsT=wblk[wb][:, c, nchunk * 128:(nchunk + 1) * 128], rhs=hnT[:, c, c0:c0 + n],
                                                       start=(c == 0), stop=(c == 7)),
                         reads=[("wblk", wb)] + HNT_ALL, writes=[PS(bk)])
                S.op("act", lambda e: e.copy(out=zb[sl][:, 0:n], in_=pbanks[bk][:, 0:n]), reads=[PS(bk)], writes=[("zb", sl)])

            def stage2(it):
                kind, h, c0, n, wb, nchunk, slot = it
                sl = slot % 3
                bk = ZB_[sl]
                rb = RB_[sl]
                S.op("pe", lambda e: e.matmul(pbanks[rb][:, 0:n], lhsT=permm[:, :], rhs=zb[sl][:, 0:n], start=True, stop=True),
                     reads=[("zb", sl), cres(permm)], writes=[PS(rb)])
                S.op("dve", lambda e: e.tensor_tensor(out=t1[sl][:, 0:n], in0=pbanks[bk][:, 0:n], in1=cosT[:, c0:c0 + n], op=ALU.mult),
                     reads=[PS(bk), "cosT"], writes=[("t1", sl)])
                S.op("dve", lambda e: e.tensor_tensor(out=t2[sl][:, 0:n], in0=pbanks[rb][:, 0:n], in1=sinT[:, c0:c0 + n], op=ALU.mult),
                     reads=[PS(rb), "sinT"], writes=[("t2", sl)])
                if kind == "q":
                    dst = QT[:, h, c0 - ROFF:c0 - ROFF + n]
                    dres = ("QT", h, (c0 - ROFF) // 512)
                else:
                    dst = KT[:, h, c0:c0 + n]
                    dres = ("KT", h)
                S.op("pool", lambda e: e.tensor_tensor(out=dst, in0=t1[sl][:, 0:n], in1=t2[sl][:, 0:n], op=ALU.add),
                     reads=[("t1", sl), ("t2", sl)], writes=[dres])

            slot = 0
            prev = None
            for bi, (kind, half) in enumerate(blocks):
                wb = (bi + 1) % 2
                if bi > 0:
                    load_wblock(wblk[wb], w_in_d, col_of[kind] + half * 512, 512, ("wblk", wb))
                if kind in ("q", "k"):
                    for nchunk in range(4):
                        h = half * 4 + nchunk
                        tiles = ([(0, 128)] if kind == "k" else []) + [(ROFF + jt * 512, 512) for jt in range(4)]
                        for (c0, n) in tiles:
                            it = (kind, h, c0, n, wb, nchunk, slot)
                            slot += 1
                            stage1(it)
                            if prev is not None:
                                stage2(prev)
                            prev = it
                else:
                    if prev is not None:
                        stage2(prev)
                        prev = None
                    for i in range(17):
                        for sub in range(1):
                            bk = 4 + (i % 2)
                            for c in range(8):
                                S.op("pe", lambda e, c=c, i=i, bk=bk, wb=wb: e.matmul(pbanks[bk][:, :], lhsT=hnT[:, c, i * 128:(i + 1) * 128], rhs=wblk[wb][:, c, :],
                                                                                     start=(c == 0), stop=(c == 7)),
                                     reads=[("wblk", wb), ("hnT", i)], writes=[PS(bk)])
                            S.op("act", lambda e, i=i, bk=bk, half=half: e.copy(out=Vt[:, i, half * 512:(half + 1) * 512], in_=pbanks[bk][:, :]),
                                 reads=[PS(bk)], writes=[("V", i)])
            if prev is not None:
                stage2(prev)
                prev = None
            for nm, tns, shp, res in (("QT", QT, [128, 8 * SEQ], [("QT", h, j) for h in range(8) for j in range(4)]),
                                      ("KT", KT, [128, 8 * TC], [("KT", h) for h in range(8)]),
                                      ("V", Vt, [128, 17 * D], [("V", i) for i in range(17)])):
                if nm in dbg:
                    o = dbg_out(nm, shp)
                    S.dma("pool", "dbgP", lambda e, o=o, tns=tns: e.dma_start(out=o.rearrange("p (c t) -> p c t", c=tns.shape[1]), in_=tns), reads=res, writes=["dbg_" + nm])

        if active("D"):
            S.barrier()
            rt.reset()
            QZ = [[rt.get([SEQ], BF16) for c in range(2)] for _ in range(2)]
            pTb = [rt.get([2, 512], BF16) for _ in range(3)]
            r2 = rt.get([2, 512], F32)
            o_t = [rt.get([512], F32) for _ in range(2)]
            a_t = rt.get([512], F32)
            sq_t = rt.get([512], BF16)
            ln_t = rt.get([512], F32)
            for par in range(2):
                S.op("pool", lambda e, par=par: e.memset(QZ[par][0], 0.0), writes=[("QZ", par, 0)])
                S.op("pool", lambda e, par=par: e.memset(QZ[par][1], 0.0), writes=[("QZ", par, 1)])
            ACC_O = (0, 2)
            ACC_S = (1, 3)
            SCALE = 1.0 / math.sqrt(HD)
            cnts = {"s": 0, "pt": 0}

            def build_qz(h):
                par = h % 2
                S.op("dve", lambda e: e.tensor_copy(out=QZ[par][0][0:64, :], in_=QT[0:64, h, :]),
                     reads=[("QT", h, j) for j in range(4)], writes=[("QZ", par, 0)])
                S.op("dve", lambda e: e.tensor_copy(out=QZ[par][1][64:128, :], in_=QT[64:128, h, :]),
                     reads=[("QT", h, j) for j in range(4)], writes=[("QZ", par, 1)])

            items = []
            prev_hj = None
            for h in range(NH):
                for j in range(4):
                    kts = [(0, 0, 0, False)]
                    for i in range(4 * j):
                        kts.append((1 + i, ROFF + i * 128, 0, False))
                    for r in range(4):
                        i = 4 * j + r
                        kts.append((1 + i, ROFF + i * 128, 128 * r, True))
                    for idx, kt in enumerate(kts):
                        items.append(("kt", h, j, kt, idx == 0, idx == len(kts) - 1))
                        if idx == min(5, len(kts) - 2) and prev_hj is not None:
                            items.append(("norm",) + prev_hj)
                    prev_hj = (h, j)
            final_norm = ("norm",) + prev_hj

            def stage_s(it):
                p = cnts["s"] % 2
                cnts["s"] += 1
                if it[0] == "norm":
                    _, h, j = it
                    S.op("pe", lambda e: e.matmul(pS[p][:, 0:512], lhsT=ones[:, :], rhs=sq_t, start=True, stop=True),
                         reads=["sq_t", cres(ones)], writes=[PS(4 + 2 * p)])
                    S.op("act", lambda e: e.activation(out=ln_t, in_=pS[p][:, 0:512], func=AF.Ln, scale=1.0 / VD, bias=EPS),
                         reads=[PS(4 + 2 * p)], writes=["ln_t"])
                    S.op("act", lambda e: e.activation(out=ln_t, in_=ln_t, func=AF.Exp, scale=-0.5), reads=["ln_t"], writes=["ln_t"])
                    return None
                _, h, j, (vt_i, kc0, q0, diag), first, last = it
                par = h % 2
                pb_i = cnts["pt"] % 3
                cnts["pt"] += 1
                for c in range(2):
                    S.op("pe", lambda e, c=c: e.matmul(pS[p][:, c * 512 + q0:(c + 1) * 512], lhsT=KT[c * 64:(c + 1) * 64, h, kc0:kc0 + 128],
                                                       rhs=QT[c * 64:(c + 1) * 64, h, j * 512 + q0:(j + 1) * 512], start=True, stop=not diag),
                         reads=[("KT", h), ("QT", h, j)], writes=[PS(4 + 2 * p + c)])
                    if diag:
                        S.op("pe", lambda e, c=c: e.matmul(pS[p][:, c * 512 + q0:c * 512 + q0 + 128], lhsT=ident[:, :], rhs=negm[:, :], start=False, stop=True),
                             reads=[cres(ident), cres(negm)], writes=[PS(4 + 2 * p + c)])
                S.op("act", lambda e: e.activation(out=pTb[pb_i][:, :, q0:512], in_=pS[p][:, :].rearrange("p (c n) -> p c n", c=2)[:, :, q0:512],
                                                   func=AF.Exp, scale=SCALE),
                     reads=[PS(4 + 2 * p), PS(4 + 2 * p + 1)], writes=[("pT", pb_i)])
                return pb_i

            def stage_av(it, pb_i):
                if it[0] == "norm":
                    _, h, j = it
                    S.op("dve", lambda e: e.scalar_tensor_tensor(out=QT[:, h, j * 512:(j + 1) * 512], in0=a_t, scalar=gsub, in1=ln_t, op0=ALU.mult, op1=ALU.mult),
                         reads=["a_t", "ln_t", ("lams", 5)], writes=[("QT", h, j)])
                    return
                _, h, j, (vt_i, kc0, q0, diag), first, last = it
                on = ones16 if vt_i == 0 else ones
                for c in range(2):
                    S.op("pe", lambda e, c=c: e.matmul(pbanks[ACC_O[c]][:, q0:512], lhsT=Vt[:, vt_i, h * 128:(h + 1) * 128], rhs=pTb[pb_i][:, c, q0:512],
                                                       start=first, stop=last),
                         reads=[("V", vt_i), ("pT", pb_i)], writes=[PS(ACC_O[c])])
                    S.op("pe", lambda e, c=c: e.matmul(pbanks[ACC_S[c]][:, q0:512], lhsT=on[:, :], rhs=pTb[pb_i][:, c, q0:512],
                                                       start=first, stop=last),
                         reads=[cres(on), ("pT", pb_i)], writes=[PS(ACC_S[c])])
                if last:
                    for c in range(2):
                        S.op("act", lambda e, c=c: e.activation(out=r2[:, c, :], in_=pbanks[ACC_S[c]], func=AF.Ln), reads=[PS(ACC_S[c])], writes=["r2"])
                        S.op("dve", lambda e, c=c: e.tensor_copy(out=o_t[c], in_=pbanks[ACC_O[c]]), reads=[PS(ACC_O[c])], writes=[("o_t", c)])
                    S.op("act", lambda e: e.activation(out=r2[:, :, :], in_=r2[:, :, :], func=AF.Exp, scale=-1.0), reads=["r2"], writes=["r2"])
                    for c in range(2):
                        S.op("dve", lambda e, c=c: e.tensor_tensor(out=o_t[c], in0=o_t[c], in1=r2[:, c, :], op=ALU.mult),
                             reads=[("o_t", c), "r2"], writes=[("o_t", c)])
                    S.op("dve", lambda e: e.scalar_tensor_tensor(out=a_t, in0=o_t[1], scalar=neglam, in1=o_t[0], op0=ALU.mult, op1=ALU.add),
                         reads=[("o_t", 0), ("o_t", 1), ("lams", 4)], writes=["a_t"])
                    S.op("pool", lambda e: e.tensor_tensor(out=sq_t, in0=a_t, in1=a_t, op=ALU.mult), reads=["a_t"], writes=["sq_t"])

            prev_it, prev_pb = None, None
            for it in items + [None]:
                pb_new = stage_s(it) if it is not None else None
                if prev_it is not None:
                    stage_av(prev_it, prev_pb)
                prev_it, prev_pb = it, pb_new
            stage_s(final_norm)
            stage_av(final_norm, None)
            if "AT" in dbg:
                o = dbg_out("AT", [128, 8 * SEQ])
                S.dma("pool", "dbgP", lambda e, o=o: e.dma_start(out=o.rearrange("p (c t) -> p c t", c=8), in_=QT), reads=[("QT", h, j) for h in range(8) for j in range(4)], writes=["dbg_AT"])

        if active("E1"):
            S.barrier()
            rt.reset()
            wout_sb = rt.get([8, D], BF16)
            wa = [rt.get([8, 128], BF16) for _ in range(2)]
            wp = [rt.get([4, 128], BF16) for _ in range(2)]
            wg0 = [rt.get([8, 128], BF16) for _ in range(2)]
            wg1 = [rt.get([8, 128], BF16) for _ in range(2)]
            gt = [[rt.get([512], F32) for _ in range(2)] for _ in range(2)]
            mt = [[rt.get([512], F32) for _ in range(2)] for _ in range(2)]
            ATR = [("QT", h, j) for h in range(8) for j in range(4)]

            def load_e1(dc):
                b = dc % 2
                S.dma("pool", ("e1w", b), lambda e: e.dma_start(out=wa[b], in_=wattn_d[:, dc * 128:(dc + 1) * 128].rearrange("(c p) n -> p c n", p=128)), writes=[("wa", b)])
                S.dma("pool", ("e1w", b), lambda e: e.dma_start(out=wp[b], in_=wpool_d[:, dc * 128:(dc + 1) * 128].rearrange("(c p) n -> p c n", p=128)), writes=[("wp", b)])
                S.dma("pool", ("e1w", b), lambda e: e.dma_start(out=wg0[b], in_=w_in_d[:, O_G + dc * 128:O_G + (dc + 1) * 128].rearrange("(c p) n -> p c n", p=128)), writes=[("wg0", b)])
                S.dma("pool", ("e1w", b), lambda e: e.dma_start(out=wg1[b], in_=w_in_d[:, O_G + D + dc * 128:O_G + D + (dc + 1) * 128].rearrange("(c p) n -> p c n", p=128)), writes=[("wg1", b)])

            def e1_tile(dc, j, b, pr):
                cs = slice(ROFF + j * 512, ROFF + (j + 1) * 512)
                qs = slice(j * 512, (j + 1) * 512)
                bk_g0, bk_a, bk_g1, bk_p = (0 + pr, 2 + pr, 4 + pr, 6 + pr)
                for c in range(8):
                    S.op("pe", lambda e, c=c: e.matmul(pbanks[bk_g0][:, :], lhsT=wg0[b][:, c, :], rhs=hnT[:, c, cs], start=(c == 0), stop=(c == 7)),
                         reads=[("wg0", b)] + HNT_ALL, writes=[PS(bk_g0)])
                S.op("act", lambda e: e.activation(out=gt[0][pr], in_=pbanks[bk_g0][:, :], func=AF.Sigmoid, bias=bgate[:, dc:dc + 1]),
                     reads=[PS(bk_g0), cres(bgate)], writes=[("gt", 0, pr)])
                for c in range(8):
                    S.op("pe", lambda e, c=c: e.matmul(pbanks[bk_a][:, :], lhsT=wa[b][:, c, :], rhs=QT[:, c, qs], start=(c == 0), stop=(c == 7)),
                         reads=[("wa", b)] + ATR, writes=[PS(bk_a)])
                S.op("dve", lambda e: e.tensor_tensor(out=mt[0][pr], in0=pbanks[bk_a][:, :], in1=gt[0][pr], op=ALU.mult),
                     reads=[PS(bk_a), ("gt", 0, pr)], writes=[("mt", 0, pr)])
                for c in range(8):
                    S.op("pe", lambda e, c=c: e.matmul(pbanks[bk_g1][:, :], lhsT=wg1[b][:, c, :], rhs=hnT[:, c, cs], start=(c == 0), stop=(c == 7)),
                         reads=[("wg1", b)] + HNT_ALL, writes=[PS(bk_g1)])
                S.op("act", lambda e: e.activation(out=gt[1][pr], in_=pbanks[bk_g1][:, :], func=AF.Sigmoid, bias=bgate[:, 8 + dc:9 + dc]),
                     reads=[PS(bk_g1), cres(bgate)], writes=[("gt", 1, pr)])
                for c in range(4):
                    S.op("pe", lambda e, c=c: e.matmul(pbanks[bk_p][:, :], lhsT=wp[b][:, c, :], rhs=PT[:, c, qs], start=(c == 0), stop=(c == 3)),
                         reads=[("wp", b)] + [("PT", g) for g in range(NPG)], writes=[PS(bk_p)])
                S.op("dve", lambda e: e.tensor_tensor(out=mt[1][pr], in0=pbanks[bk_p][:, :], in1=gt[1][pr], op=ALU.mult),
                     reads=[PS(bk_p), ("gt", 1, pr)], writes=[("mt", 1, pr)])
                S.op("pool", lambda e: e.tensor_tensor(out=mixT[:, dc, qs], in0=mt[0][pr], in1=mt[1][pr], op=ALU.add),
                     reads=[("mt", 0, pr), ("mt", 1, pr)], writes=[("mixT", j)])

            load_e1(0)
            cnt = 0
            for dc in range(8):
                b = dc % 2
                if dc + 1 < 8:
                    load_e1(dc + 1)
                if dc == 1:
                    S.dma("pool", "miscP", lambda e: e.dma_start(out=wout_sb, in_=wout_d.rearrange("(c p) n -> p c n", p=128)), writes=["wout"])
                for j in range(4):
                    e1_tile(dc, j, b, cnt % 2)
                    cnt += 1
            if "mixT" in dbg:
                o = dbg_out("mixT", [128, 8 * SEQ])
                S.dma("pool", "dbgP", lambda e, o=o: e.dma_start(out=o.rearrange("p (c t) -> p c t", c=8), in_=mixT), reads=[("mixT", j) for j in range(4)], writes=["dbg_mixT"])

        if active("E2"):
            S.barrier()
            rt.reset()
            wout_sb = rt.get([8, D], BF16)
            x2 = [rt.get([D], F32) for _ in range(2)]
            g2bc = rt.get([D], F32)
            junk2 = rt.get([D], BF16)
            hn2 = [rt.get([D], BF16) for _ in range(2)]
            S.dma("sp", "miscS", lambda e: e.dma_start(out=g2bc, in_=g2_d.to_broadcast([128, D])), writes=["g2bc"])
            if active("F"):
                S.dma("pool", ("exw", 1), lambda e: e.dma_start(out=wup[1], in_=weu_d[0].rearrange("(c p) n -> p c n", p=128)), writes=[("wup", 1)])
                S.dma("pool", ("exw", 1), lambda e: e.dma_start(out=wdn[1], in_=wed_d[0].rearrange("(c p) n -> p c n", p=128)), writes=[("wdn", 1)])
            MIXR = [("mixT", j) for j in range(4)]
            def e2_s1(i):
                b = i % 2
                S.dma("sp", ("x2", b), lambda e: e.dma_start(out=x2[b], in_=x_d[i * 128:(i + 1) * 128, :]), writes=[("x2", b)])
                for half in range(2):
                    bk = (2 * i + half) % 4
                    for c in range(8):
                        S.op("pe", lambda e, c=c, half=half, bk=bk: e.matmul(pbanks[bk][:, :], lhsT=mixT[:, c, i * 128:(i + 1) * 128],
                                                                             rhs=wout_sb[:, c, half * 512:(half + 1) * 512], start=(c == 0), stop=(c == 7)),
                             reads=["wout"] + MIXR, writes=[PS(bk)])
                    S.op("dve", lambda e, half=half, bk=bk: e.tensor_tensor(out=h1[:, i, half * 512:(half + 1) * 512], in0=pbanks[bk][:, :],
                                                                            in1=x2[b][:, half * 512:(half + 1) * 512], op=ALU.add),
                         reads=[PS(bk), ("x2", b)], writes=[("h1", i)])
                S.op("act", lambda e: e.activation(out=junk2, in_=h1[:, i, :], func=AF.Square, accum_out=stats2[:, i, 0:1]),
                     reads=[("h1", i)], writes=["junk2", ("s2", i)])
                S.op("act", lambda e: e.activation(out=stats2[:, i, 1:2], in_=stats2[:, i, 0:1], func=AF.Sqrt, scale=1.0 / D, bias=EPS),
                     reads=[("s2", i)], writes=[("s21", i)])
                S.op("dve", lambda e: e.reciprocal(out=stats2[:, i, 1:2], in_=stats2[:, i, 1:2]), reads=[("s21", i)], writes=[("s21", i)])
                S.op("dve", lambda e: e.scalar_tensor_tensor(out=hn2[b], in0=h1[:, i, :], scalar=stats2[:, i, 1:2], in1=g2bc, op0=ALU.mult, op1=ALU.mult),
                     reads=[("h1", i), ("s21", i), "g2bc"], writes=[("hn2", b)])

            def e2_s2(i):
                b = i % 2
                pbk = 4 + i % 2
                for c in range(8):
                    S.op("pe", lambda e, c=c: e.transpose(out=pbf(pbk)[:, c * 128:(c + 1) * 128], in_=hn2[b][:, c * 128:(c + 1) * 128], identity=ident[:, :]),
                         reads=[("hn2", b), cres(ident)], writes=[PS(pbk)])
                S.op("act", lambda e: e.copy(out=hn2T[:, :, i * 128:(i + 1) * 128], in_=pbf(pbk).rearrange("p (c n) -> p c n", c=8)),
                     reads=[PS(pbk)], writes=[("hn2T", i)])

            def e2_s3(i):
                rbk = 6 + i % 2
                for c in range(8):
                    S.op("pe", lambda e, c=c: e.matmul(pbanks[rbk][:, 0:20], lhsT=hn2T[:, c, i * 128:(i + 1) * 128], rhs=wr_sb[:, c, :],
                                                       start=(c == 0), stop=(c == 7)),
                         reads=[("hn2T", i), cres(wr_sb)], writes=[PS(rbk)])
                S.op("dve", lambda e: e.tensor_tensor(out=rl[:, i, :], in0=pbanks[rbk][:, 0:20], in1=br_sb[:, :], op=ALU.add),
                     reads=[PS(rbk), cres(br_sb)], writes=[("rl", i)])

            for it_ in range(16 + 2):
                if it_ < 16:
                    e2_s1(it_)
                if 0 <= it_ - 1 < 16:
                    e2_s2(it_ - 1)
                if 0 <= it_ - 2 < 16:
                    e2_s3(it_ - 2)
            if "h1" in dbg:
                o = dbg_out("h1", [128, 16 * D])
                S.dma("sp", "dbgS", lambda e, o=o: e.dma_start(out=o.rearrange("p (c t) -> p c t", c=16), in_=h1), reads=[("h1", i) for i in range(16)], writes=["dbg_h1"])
            if "rl" in dbg:
                o = dbg_out("rl", [128, 16 * 20])
                S.dma("sp", "dbgS", lambda e, o=o: e.dma_start(out=o.rearrange("p (c t) -> p c t", c=16), in_=rl), reads=[("rl", i) for i in range(16)], writes=["dbg_rl"])

        if active("F"):
            S.barrier()
            rt.reset()
            RL = [("rl", i) for i in range(16)]
            glog = rl[:, :, 0:4]
            gmx = rw[:, :, 0:1]
            gex = rw[:, :, 4:8]
            gsum = rw[:, :, 8:9]
            goh = rw[:, :, 12:16]
            esel = rw[:, :, 16:20]
            etmp = rw[:, :, 20:24]
            m1 = rw[:, :, 24:25]
            oh1 = rw[:, :, 28:32]
            e2v = rw[:, :, 32:36]
            m2 = rw[:, :, 36:37]
            oh2 = rw[:, :, 40:44]
            dd = rw[:, :, 44:45]
            w1 = rw[:, :, 45:46]
            w2_ = rw[:, :, 46:47]
            c4 = rw[:, :, 48:52]
            RW = "rw"
            V_ = S.op
            V_("dve", lambda e: e.tensor_reduce(out=gmx, in_=glog, axis=AX.X, op=ALU.max), reads=RL, writes=[RW])
            V_("dve", lambda e: e.tensor_tensor(out=gex, in0=glog, in1=gmx.to_broadcast([128, 16, 4]), op=ALU.subtract), reads=RL + [RW], writes=[RW])
            V_("dve", lambda e: e.tensor_tensor(out=goh, in0=glog, in1=gmx.to_broadcast([128, 16, 4]), op=ALU.is_ge), reads=RL + [RW], writes=[RW])
            V_("act", lambda e: e.activation(out=gex, in_=gex, func=AF.Exp), reads=[RW], writes=[RW])
            V_("dve", lambda e: e.tensor_reduce(out=gsum, in_=gex, axis=AX.X, op=ALU.add), reads=[RW], writes=[RW])
            V_("dve", lambda e: e.reciprocal(out=gsum, in_=gsum), reads=[RW], writes=[RW])
            for g in range(4):
                if g == 0:
                    V_("dve", lambda e: e.tensor_tensor(out=esel, in0=rl[:, :, 4:8], in1=goh[:, :, 0:1].to_broadcast([128, 16, 4]), op=ALU.mult), reads=RL + [RW], writes=[RW])
                else:
                    V_("dve", lambda e, g=g: e.tensor_tensor(out=etmp, in0=rl[:, :, 4 + 4 * g:8 + 4 * g], in1=goh[:, :, g:g + 1].to_broadcast([128, 16, 4]), op=ALU.mult),
                       reads=RL + [RW], writes=[RW])
                    V_("dve", lambda e: e.tensor_tensor(out=esel, in0=esel, in1=etmp, op=ALU.add), reads=[RW], writes=[RW])
            V_("dve", lambda e: e.tensor_reduce(out=m1, in_=esel, axis=AX.X, op=ALU.max), reads=[RW], writes=[RW])
            V_("dve", lambda e: e.tensor_tensor(out=oh1, in0=esel, in1=m1.to_broadcast([128, 16, 4]), op=ALU.is_ge), reads=[RW], writes=[RW])
            V_("dve", lambda e: e.scalar_tensor_tensor(out=e2v, in0=oh1, scalar=-1e30, in1=esel, op0=ALU.mult, op1=ALU.add), reads=[RW], writes=[RW])
            V_("dve", lambda e: e.tensor_reduce(out=m2, in_=e2v, axis=AX.X, op=ALU.max), reads=[RW], writes=[RW])
            V_("dve", lambda e: e.tensor_tensor(out=oh2, in0=e2v, in1=m2.to_broadcast([128, 16, 4]), op=ALU.is_ge), reads=[RW], writes=[RW])
            V_("dve", lambda e: e.tensor_tensor(out=dd, in0=m2, in1=m1, op=ALU.subtract), reads=[RW], writes=[RW])
            V_("act", lambda e: e.activation(out=dd, in_=dd, func=AF.Exp), reads=[RW], writes=[RW])
            V_("dve", lambda e: e.tensor_scalar(out=w1, in0=dd, scalar1=1.0, scalar2=None, op0=ALU.add), reads=[RW], writes=[RW])
            V_("dve", lambda e: e.reciprocal(out=w1, in_=w1), reads=[RW], writes=[RW])
            V_("dve", lambda e: e.tensor_tensor(out=w1, in0=w1, in1=gsum, op=ALU.mult), reads=[RW], writes=[RW])
            V_("dve", lambda e: e.tensor_tensor(out=w2_, in0=w1, in1=dd, op=ALU.mult), reads=[RW], writes=[RW])
            V_("dve", lambda e: e.tensor_tensor(out=c4, in0=oh1, in1=w1.to_broadcast([128, 16, 4]), op=ALU.mult), reads=[RW], writes=[RW])
            V_("dve", lambda e: e.tensor_tensor(out=etmp, in0=oh2, in1=w2_.to_broadcast([128, 16, 4]), op=ALU.mult), reads=[RW], writes=[RW])
            V_("dve", lambda e: e.tensor_tensor(out=c4, in0=c4, in1=etmp, op=ALU.add), reads=[RW], writes=[RW])
            for g in range(4):
                V_("dve", lambda e, g=g: e.tensor_tensor(out=comb[:, :, 4 * g:4 * g + 4], in0=c4, in1=goh[:, :, g:g + 1].to_broadcast([128, 16, 4]), op=ALU.mult),
                   reads=[RW], writes=["comb"])
            if "comb" in dbg:
                o = dbg_out("comb", [128, 16 * 16])
                S.dma("sp", "dbgS", lambda e, o=o: e.dma_start(out=o.rearrange("p (c t) -> p c t", c=16), in_=comb), reads=["comb"], writes=["dbg_comb"])

            sg = [rt.get([512], F32) for _ in range(2)]
            At = [rt.get([4, 512], BF16) for _ in range(2)]
            HN2R = [("hn2T", i) for i in range(16)]

            def load_expert(ex):
                b = (ex + 1) % 2
                S.dma("pool", ("exw", b), lambda e: e.dma_start(out=wgt[b], in_=weg_d[ex].rearrange("(c p) n -> p c n", p=128)), writes=[("wgt", b)])
                if ex == 0:
                    return
                S.dma("pool", ("exw", b), lambda e: e.dma_start(out=wup[b], in_=weu_d[ex].rearrange("(c p) n -> p c n", p=128)), writes=[("wup", b)])
                S.dma("pool", ("exw", b), lambda e: e.dma_start(out=wdn[b], in_=wed_d[ex].rearrange("(c p) n -> p c n", p=128)), writes=[("wdn", b)])

            do_g = active("G")
            if do_g:
                gfbc = rt.get([D], F32)
                junk3 = rt.get([D], BF16)
                ot = [rt.get([D], F32) for _ in range(2)]
                S.dma("sp", "miscS", lambda e: e.dma_start(out=gfbc, in_=gf_d.to_broadcast([128, D])), writes=["gfbc"])

            def g_tile(i):
                b = i % 2
                S.op("act", lambda e: e.activation(out=junk3, in_=h1[:, i, :], func=AF.Square, accum_out=stats3[:, i, 0:1]),
                     reads=[("h1", i)], writes=["junk3", ("s3", i)])
                S.op("act", lambda e: e.activation(out=stats3[:, i, 1:2], in_=stats3[:, i, 0:1], func=AF.Sqrt, scale=1.0 / D, bias=EPS),
                     reads=[("s3", i)], writes=[("s31", i)])
                S.op("dve", lambda e: e.reciprocal(out=stats3[:, i, 1:2], in_=stats3[:, i, 1:2]), reads=[("s31", i)], writes=[("s31", i)])
                S.op("dve", lambda e: e.scalar_tensor_tensor(out=ot[b], in0=h1[:, i, :], scalar=stats3[:, i, 1:2], in1=gfbc, op0=ALU.mult, op1=ALU.mult),
                     reads=[("h1", i), ("s31", i), "gfbc"], writes=[("ot", b)])
                S.dma("sp", ("st", b), lambda e: e.dma_start(out=out_d[i * 128:(i + 1) * 128, :], in_=ot[b]), reads=[("ot", b)], writes=[("out", i)])

            cnt_f = [0, 0]
            def f_gu(ex, j, b, ab):
                ts_ = slice(j * 512, (j + 1) * 512)
                for f in range(4):
                    pr = cnt_f[0] % 2
                    cnt_f[0] += 1
                    bg, bu = 0 + pr, 2 + pr
                    for c in range(8):
                        S.op("pe", lambda e, c=c, f=f, bg=bg: e.matmul(pbanks[bg][:, :], lhsT=wgt[b][:, c, f * 128:(f + 1) * 128], rhs=hn2T[:, c, ts_], start=(c == 0), stop=(c == 7)),
                             reads=[("wgt", b)] + HN2R, writes=[PS(bg)])
                    S.op("act", lambda e, pr=pr, bg=bg: e.activation(out=sg[pr], in_=pbanks[bg][:, :], func=AF.Silu), reads=[PS(bg)], writes=[("sg", pr)])
                    for c in range(8):
                        S.op("pe", lambda e, c=c, f=f, bu=bu: e.matmul(pbanks[bu][:, :], lhsT=wup[b][:, c, f * 128:(f + 1) * 128], rhs=hn2T[:, c, ts_], start=(c == 0), stop=(c == 7)),
                             reads=[("wup", b)] + HN2R, writes=[PS(bu)])
                    S.op("dve", lambda e, pr=pr, f=f, bu=bu: e.tensor_tensor(out=At[ab][:, f, :], in0=pbanks[bu][:, :], in1=sg[pr], op=ALU.mult),
                         reads=[PS(bu), ("sg", pr)], writes=[("At", ab)])

            def f_down(ex, j, b, ab):
                for sub in range(4):
                    i = 4 * j + sub
                    for half in range(2):
                        bd = 4 + cnt_f[1] % 4
                        cnt_f[1] += 1
                        for f in range(4):
                            S.op("pe", lambda e, f=f, sub=sub, half=half, bd=bd: e.matmul(pbanks[bd][:, :], lhsT=At[ab][:, f, sub * 128:(sub + 1) * 128],
                                                                                          rhs=wdn[b][:, f, half * 512:(half + 1) * 512], start=(f == 0), stop=(f == 3)),
                                 reads=[("At", ab), ("wdn", b)], writes=[PS(bd)])
                        S.op("dve", lambda e, i=i, half=half, bd=bd, ex=ex: e.scalar_tensor_tensor(out=h1[:, i, half * 512:(half + 1) * 512], in0=pbanks[bd][:, :],
                                                                                                  scalar=comb[:, i, ex:ex + 1], in1=h1[:, i, half * 512:(half + 1) * 512],
                                                                                                  op0=ALU.mult, op1=ALU.add),
                             reads=[PS(bd), "comb", ("h1", i)], writes=[("h1", i)])
                    if do_g and ex == NE - 1:
                        g_tile(i)

            load_expert(0)
            fitems = [(ex, j) for ex in range(NE) for j in range(4)]
            for idx in range(len(fitems) + 1):
                if idx < len(fitems):
                    ex, j = fitems[idx]
                    f_gu(ex, j, (ex + 1) % 2, idx % 2)
                if idx >= 1:
                    exp_, jp_ = fitems[idx - 1]
                    f_down(exp_, jp_, (exp_ + 1) % 2, (idx - 1) % 2)
                if idx < len(fitems):
                    ex, j = fitems[idx]
                    if j == 0 and ex + 1 < NE:
                        load_expert(ex + 1)
            if "h2" in dbg:
                o = dbg_out("h2", [128, 16 * D])
                S.dma("sp", "dbgS", lambda e, o=o: e.dma_start(out=o.rearrange("p (c t) -> p c t", c=16), in_=h1), reads=[("h1", i) for i in range(16)], writes=["dbg_h2"])

        if active("G") and not active("F"):
            raise RuntimeError("phase G is fused into phase F")

        S.finish("sp")
        nsig = S.emit(nc)
    return nc, dbg_d, nsig


def _consts():
    f32 = np.float32
    inv = (1.0 / (10000.0 ** (np.arange(0, HD, 2, dtype=f32) / f32(HD)))).astype(f32)
    cos_t = np.zeros((128, TC), f32)
    sin_t = np.zeros((128, TC), f32)
    pos = np.arange(SEQ + NMETA, dtype=f32)
    ang = (pos[:, None] * inv[None, :]).astype(f32)
    ang = np.concatenate([ang, ang], axis=-1)
    c = np.cos(ang).astype(f32).T
    s = np.sin(ang).astype(f32).T
    s_signed = s.copy()
    s_signed[:HD // 2] *= -1.0
    for half in range(2):
        cos_t[half * 64:(half + 1) * 64, MOFF:] = c
        sin_t[half * 64:(half + 1) * 64, MOFF:] = s_signed
    ident = np.eye(128, dtype=f32)
    perm = np.zeros((128, 128), f32)
    for n2 in range(128):
        blk, d = divmod(n2, 64)
        perm[blk * 64 + (d + 32) % 64, n2] = 1.0
    k_ = np.arange(128)[:, None]
    q_ = np.arange(128)[None, :]
    tri = (q_ >= k_).astype(f32)
    tri = np.concatenate([tri, tri], axis=1)
    ones = np.ones((128, 128), f32)
    ones16 = np.zeros((128, 128), f32)
    ones16[MOFF:, :] = 1.0
    negm = np.where(q_ >= k_, 0.0, -30000.0).astype(f32)
    return dict(cos_t=cos_t, sin_t=sin_t, c_ident=ident, c_perm=perm, c_tri=tri, c_ones=ones, c_ones16=ones16, c_negm=negm)


def make_in_maps(inputs, n_cores=8):
    f = lambda a: np.ascontiguousarray(np.asarray(a, dtype=np.float32))
    x = f(inputs["x"])
    shared = dict(
        meta=f(inputs["meta"]),
        norm1_g=f(inputs["norm1_g"]).reshape(1, D),
        w_in=f(inputs["w_in"]).reshape(D, IN_W),
        b_gate_t=np.ascontiguousarray(f(inputs["b_gate"]).reshape(16, 128).T),
        lambda_q1=f(inputs["lambda_q1"]).reshape(1, HD),
        lambda_k1=f(inputs["lambda_k1"]).reshape(1, HD),
        lambda_q2=f(inputs["lambda_q2"]).reshape(1, HD),
        lambda_k2=f(inputs["lambda_k2"]).reshape(1, HD),
        subln_g_t=f(inputs["subln_g"]).reshape(128, 1),
        pool_w=f(inputs["pool_w"]).reshape(NPG, 128, 128),
        pool_scale_t=np.ascontiguousarray(f(inputs["pool_scale"]).reshape(NPG, 128).T),
        w_attn_br=f(inputs["w_attn_br"]).reshape(D, D),
        w_pool_br=f(inputs["w_pool_br"]).reshape(512, D),
        w_out=f(inputs["w_out"]).reshape(D, D),
        norm2_g=f(inputs["norm2_g"]).reshape(1, D),
        w_router=np.ascontiguousarray(np.concatenate([f(inputs["w_router_group"]).reshape(D, 4), f(inputs["w_router_expert"]).reshape(D, 16)], axis=1)),
        b_router=np.ascontiguousarray(np.concatenate([f(inputs["b_router_group"]).reshape(1, 4), f(inputs["b_router_expert"]).reshape(1, 16)], axis=1)),
        w_e_gate=f(inputs["w_e_gate"]).reshape(NE, D, DE),
        w_e_up=f(inputs["w_e_up"]).reshape(NE, D, DE),
        w_e_down=f(inputs["w_e_down"]).reshape(NE, DE, D),
        final_g=f(inputs["final_g"]).reshape(1, D),
    )
    shared.update(_consts())
    maps = []
    for b in range(n_cores):
        m = dict(shared)
        m["x"] = np.ascontiguousarray(x[b])
        maps.append(m)
    return maps


_CACHE = {}


def kernel(**inputs):
    if "nc" not in _CACHE:
        _CACHE["nc"] = build_program()[0]
    nc = _CACHE["nc"]
    maps = make_in_maps(inputs, 8)
    res = run_bass_kernel_spmd(nc, maps, core_ids=list(range(8)))
    out = np.stack([np.asarray(r["out"], dtype=np.float32) for r in res.results], axis=0)
    return out
```

```python
import contextlib
import math

import numpy as np

import concourse.bass as bass
import concourse.mybir as mybir
from concourse.bass_utils import run_bass_kernel_spmd

F32 = mybir.dt.float32
BF16 = mybir.dt.bfloat16
AF = mybir.ActivationFunctionType
ALU = mybir.AluOpType
AX = mybir.AxisListType

D = 1024
SEQ = 2048
NMETA = 16
NH = 8
HD = 64
VD = 128
NPG = 4
POOL_W = (2, 4, 8, 16)
IN_W = 5632
NE = 16
DE = 512
EPS = 1e-6
TC = 2176
MOFF = 112
ROFF = 128
O_Q, O_K, O_V, O_P, O_G = 0, 1024, 2048, 3072, 3584
LAMBDA_INIT = 0.8 - 0.6 * math.exp(-0.3 * 0)
EPOCH = 12000


class _Op:
    __slots__ = ("eng", "fn", "waits", "signal", "idx", "dma_stream", "sigcount")

    def __init__(self, eng, fn):
        self.eng = eng
        self.fn = fn
        self.waits = []
        self.signal = False
        self.idx = -1
        self.dma_stream = None
        self.sigcount = 0


class Sched:
    ENGS = ("pe", "act", "dve", "pool", "sp")

    def __init__(self):
        self.ops = {e: [] for e in self.ENGS}
        self.last_w = {}
        self.readers = {}
        self.known = {e: {} for e in self.ENGS}
        self.streams = {}
        self.pending = {e: [] for e in self.ENGS}

    def _deps(self, eng, reads, writes, is_dma=False):
        deps = {}

        def add(kind, src, n, raw):
            if kind == "e" and src == eng and not is_dma:
                if eng == "pe" or eng == "sp":
                    return
            if kind == "d":
                n = self.streams[src]
            key = (kind, src)
            if deps.get(key, -1) < n:
                deps[key] = n

        for r in reads:
            ev = self.last_w.get(r)
            if ev is not None:
                add(ev[0], ev[1], ev[2], True)
        for w in writes:
            ev = self.last_w.get(w)
            if ev is not None:
                add(ev[0], ev[1], ev[2], False)
            rd = self.readers.get(w)
            if rd:
                for key, n in rd.items():
                    add(key[0], key[1], n, False)
        for (kind, src, n) in self.pending[eng]:
            if kind == "e" and src == eng and not is_dma:
                continue
            key = (kind, src)
            if deps.get(key, -1) < n:
                deps[key] = n
        self.pending[eng] = []
        out = []
        kn = self.known[eng]
        for key, n in deps.items():
            if kn.get(key, -1) >= n:
                continue
            kn[key] = n
            out.append((key[0], key[1], n))
        return out

    def _commit(self, ev, reads, writes):
        for w in writes:
            self.last_w[w] = ev
            self.readers[w] = {}
        key = (ev[0], ev[1])
        for r in reads:
            d = self.readers.setdefault(r, {})
            if d.get(key, -1) < ev[2]:
                d[key] = ev[2]

    def _add(self, eng, fn, reads, writes, stream):
        ps_reads = [r for r in reads if isinstance(r, tuple) and r[0] == "ps"]
        if ps_reads:
            writes = list(writes) + ps_reads
        o = _Op(eng, fn)
        o.waits = self._deps(eng, reads, writes, is_dma=stream is not None)
        lst = self.ops[eng]
        o.idx = len(lst)
        lst.append(o)
        for (kind, src, n) in o.waits:
            if kind == "e":
                self.ops[src][n].signal = True
        if stream is None:
            ev = ("e", eng, o.idx)
        else:
            c = self.streams.get(stream, 0) + 1
            self.streams[stream] = c
            o.dma_stream = stream
            ev = ("d", stream, c)
        self._commit(ev, reads, writes)
        return o

    def op(self, eng, fn, reads=(), writes=()):
        return self._add(eng, fn, reads, writes, None)

    def dma(self, queue, stream, fn, reads=(), writes=()):
        return self._add(queue, fn, reads, writes, stream)

    def barrier(self):
        evs = []
        for e in self.ENGS:
            if self.ops[e]:
                evs.append(("e", e, len(self.ops[e]) - 1))
        for s, c in self.streams.items():
            if isinstance(s, tuple) and s[0] == "xwld":
                continue
            evs.append(("d", s, c))
        for e in self.ENGS:
            self.pending[e] = list(evs)
        fixed = []
        for (kind, src, n) in evs:
            if kind == "e":
                i = n
                while i >= 0 and self.ops[src][i].dma_stream is not None:
                    i -= 1
                if i < 0:
                    continue
                fixed.append(("e", src, i))
            else:
                fixed.append((kind, src, n))
        for e in self.ENGS:
            self.pending[e] = list(fixed)

    def finish(self, eng="sp"):
        self.barrier()
        o = _Op(eng, None)
        o.waits = self._deps(eng, (), ())
        for (kind, src, n) in o.waits:
            if kind == "e":
                self.ops[src][n].signal = True
        o.idx = len(self.ops[eng])
        self.ops[eng].append(o)

    def emit(self, nc):
        for e, lst in self.ops.items():
            c = 0
            for o in lst:
                if o.signal:
                    c += 1
                o.sigcount = c
        nsig = {e: (lst[-1].sigcount if lst else 0) for e, lst in self.ops.items()}
        with contextlib.ExitStack() as st:
            esems = {}
            for e in self.ENGS:
                n_ep = nsig[e] // EPOCH + 1
                esems[e] = [st.enter_context(nc.semaphore(f"s_{e}_{k}")) for k in range(n_ep)]
            dsems = {s: st.enter_context(nc.semaphore("d_" + "_".join(str(x) for x in (s if isinstance(s, tuple) else (s,))))) for s in self.streams}
            block = st.enter_context(nc.Block())

            def run(engname):
                def body(eng):
                    for o in self.ops[engname]:
                        for (kind, src, n) in o.waits:
                            if kind == "e":
                                sc = self.ops[src][n].sigcount
                                ep, v = (sc - 1) // EPOCH, (sc - 1) % EPOCH + 1
                                eng.wait_ge(esems[src][ep], v)
                            else:
                                eng.wait_ge(dsems[src], 16 * n)
                        if o.fn is None:
                            continue
                        ins = o.fn(eng)
                        if o.dma_stream is not None:
                            ins.then_inc(dsems[o.dma_stream], 16)
                        elif o.signal:
                            ep = (o.sigcount - 1) // EPOCH
                            ins.then_inc(esems[engname][ep], 1)
                return body

            block.tensor(run("pe"))
            block.scalar(run("act"))
            block.vector(run("dve"))
            block.gpsimd(run("pool"))
            block.sync(run("sp"))
        return nsig


def build_program(dbg=(), stop_after=None):
    nc = bass.Bass("TRN2", target_bir_lowering=False)
    dbg = set(dbg)

    def din(name, shape, dt=F32):
        return nc.dram_tensor(name, list(shape), dt, kind="ExternalInput").ap()

    x_d = din("x", [SEQ, D])
    meta_d = din("meta", [NMETA, D])
    g1_d = din("norm1_g", [1, D])
    w_in_d = din("w_in", [D, IN_W])
    bgate_d = din("b_gate_t", [128, 16])
    lq1_d = din("lambda_q1", [1, HD])
    lk1_d = din("lambda_k1", [1, HD])
    lq2_d = din("lambda_q2", [1, HD])
    lk2_d = din("lambda_k2", [1, HD])
    subg_d = din("subln_g_t", [128, 1])
    poolw_d = din("pool_w", [NPG, 128, 128])
    pscale_d = din("pool_scale_t", [128, NPG])
    wattn_d = din("w_attn_br", [D, D])
    wpool_d = din("w_pool_br", [512, D])
    wout_d = din("w_out", [D, D])
    g2_d = din("norm2_g", [1, D])
    wr_d = din("w_router", [D, 20])
    br_d = din("b_router", [1, 20])
    weg_d = din("w_e_gate", [NE, D, DE])
    weu_d = din("w_e_up", [NE, D, DE])
    wed_d = din("w_e_down", [NE, DE, D])
    gf_d = din("final_g", [1, D])
    cos_d = din("cos_t", [128, TC])
    sin_d = din("sin_t", [128, TC])
    ident_d = din("c_ident", [128, 128])
    perm_d = din("c_perm", [128, 128])
    tri_d = din("c_tri", [128, 256])
    ones_d = din("c_ones", [128, 128])
    ones16_d = din("c_ones16", [128, 128])
    negm_d = din("c_negm", [128, 128])
    out_d = nc.dram_tensor("out", [SEQ, D], F32, kind="ExternalOutput").ap()
    dbg_d = {}

    def dbg_out(name, shape):
        dbg_d[name] = nc.dram_tensor("dbg_" + name, list(shape), F32, kind="ExternalOutput").ap()
        return dbg_d[name]

    S = Sched()
    phases = ["A", "B0", "B1", "D", "E1", "E2", "F", "G"]
    last_phase = stop_after or "G"

    def active(ph):
        return phases.index(ph) <= phases.index(last_phase)

    with contextlib.ExitStack() as st:
        def sb(name, shape, dt):
            return st.enter_context(nc.sbuf_tensor(name, list(shape), dt))

        R1 = sb("R1", [128, 34816 // 4], F32)
        R42 = sb("R42", [128, 67584 // 4], F32)
        R35 = sb("R35", [128, 51200 // 4], F32)
        RT_BYTES = 50 * 1024
        RT = sb("RT", [128, RT_BYTES // 4], F32)

        def view(region, off_bytes, shape, dt):
            n = 1
            for s_ in shape:
                n *= s_
            if dt == F32:
                assert off_bytes % 4 == 0
                a = region[:, off_bytes // 4: off_bytes // 4 + n]
            else:
                assert off_bytes % 4 == 0 and (n * 2) % 4 == 0
                a = region[:, off_bytes // 4: off_bytes // 4 + (n * 2) // 4].bitcast(BF16)
            if len(shape) == 2:
                a = a.rearrange("p (a b) -> p a b", a=shape[0])
            elif len(shape) == 3:
                a = a.rearrange("p (a b c) -> p a b c", a=shape[0], b=shape[1])
            return a

        class RTAlloc:
            def __init__(self):
                self.off = 0

            def reset(self):
                self.off = 0

            def get(self, shape, dt):
                n = 1
                for s_ in shape:
                    n *= s_
                nb = n * (4 if dt == F32 else 2)
                nb = (nb + 31) // 32 * 32
                v = view(RT, self.off, shape, dt)
                self.off += nb
                assert self.off <= RT_BYTES, ("RT overflow", self.off)
                return v

        rt = RTAlloc()
        _tail = RT_BYTES - (1280 + 4096 + 1024)
        rl = view(RT, _tail, [16, 20], F32)
        rw = view(RT, _tail + 1280, [16, 64], F32)
        comb = view(RT, _tail + 1280 + 4096, [16, 16], F32)
        wg0x = view(RT, 35840, [8, DE], BF16)

        hnT = view(R1, 0, [8, TC], BF16)
        hn2T = view(R1, 0, [8, SEQ], BF16)
        Vt = view(R42, 0, [17, D], BF16)
        QT = view(R42, 34816, [8, SEQ], BF16)
        h1 = view(R42, 0, [16, D], F32)
        KT = view(R35, 0, [8, TC], BF16)
        mixT = view(R35, 0, [8, SEQ], BF16)
        PT = view(R35, 34816, [NPG, SEQ], BF16)
        WB = 24576
        wgt = [view(R35, k * WB, [8, DE], BF16) for k in range(2)]
        wup = [view(R35, k * WB + 8192, [8, DE], BF16) for k in range(2)]
        wdn = [view(R35, k * WB + 16384, [4, D], BF16) for k in range(2)]

        ident = sb("ident", [128, 128], BF16)
        permm = sb("permm", [128, 128], BF16)
        tri = sb("tri", [128, 2, 128], BF16)
        ones = sb("ones", [128, 128], BF16)
        ones16 = sb("ones16", [128, 128], BF16)
        negm = sb("negm", [128, 128], BF16)
        poolw = sb("poolw", [128, NPG, 128], BF16)
        bgate = sb("bgate", [128, 16], F32)
        pscale = sb("pscale", [128, NPG], F32)
        subg = sb("subg", [128, 1], F32)
        lamt = sb("lamt", [128, 4 * HD], F32)
        lams = sb("lams", [128, 8], F32)
        stats = sb("stats", [128, 17, 2], F32)
        stats2 = sb("stats2", [128, 16, 2], F32)
        stats3 = sb("stats3", [128, 16, 2], F32)
        wr_sb = sb("wr_sb", [128, 8, 20], BF16)
        br_sb = sb("br_sb", [128, 20], F32)

        _pb = [st.enter_context(nc.psum_tensor(f"pb{i}", [128, 512], F32)) for i in range(4)]
        pS = [st.enter_context(nc.psum_tensor(f"pS{i}", [128, 1024], F32)) for i in range(2)]
        pbanks = [t[:, :] for t in _pb] + [pS[0][:, 0:512], pS[0][:, 512:1024], pS[1][:, 0:512], pS[1][:, 512:1024]]

        def pbf(i):
            return pbanks[i].bitcast(BF16)

        cres = lambda t: ("c", t.name)
        for t, d_ in ((ident, ident_d), (permm, perm_d), (ones, ones_d), (ones16, ones16_d), (negm, negm_d)):
            S.dma("pool", "constP", lambda e, t=t, d_=d_: e.dma_start(out=t[:, :], in_=d_), writes=[cres(t)])
        S.dma("pool", "constP", lambda e: e.dma_start(out=tri[:, :, :], in_=tri_d.rearrange("p (a b) -> p a b", a=2)), writes=[cres(tri)])
        S.dma("pool", "constP", lambda e: e.dma_start(out=poolw[:, :, :], in_=poolw_d.rearrange("g c d -> c g d")), writes=[cres(poolw)])
        S.dma("pool", "constP", lambda e: e.dma_start(out=wr_sb[:, :, :], in_=wr_d.rearrange("(c p) n -> p c n", p=128)), writes=[cres(wr_sb)])
        for t, d_ in ((bgate, bgate_d), (pscale, pscale_d), (subg, subg_d)):
            S.dma("sp", "constS", lambda e, t=t, d_=d_: e.dma_start(out=t[:, :], in_=d_), writes=[cres(t)])
        S.dma("sp", "constS", lambda e: e.dma_start(out=br_sb[:, :], in_=br_d.to_broadcast([128, 20])), writes=[cres(br_sb)])
        for i, d_ in enumerate((lq1_d, lk1_d, lq2_d, lk2_d)):
            S.dma("sp", "constS", lambda e, i=i, d_=d_: e.dma_start(out=lamt[:, i * HD:(i + 1) * HD], in_=d_.to_broadcast([128, HD])), writes=[("lamt", i)])

        S.op("dve", lambda e: e.tensor_tensor(out=lamt[:, 0:HD], in0=lamt[:, 0:HD], in1=lamt[:, HD:2 * HD], op=ALU.mult),
             reads=[("lamt", 0), ("lamt", 1)], writes=[("lamt", 0)])
        S.op("dve", lambda e: e.tensor_tensor(out=lamt[:, 2 * HD:3 * HD], in0=lamt[:, 2 * HD:3 * HD], in1=lamt[:, 3 * HD:4 * HD], op=ALU.mult),
             reads=[("lamt", 2), ("lamt", 3)], writes=[("lamt", 2)])
        S.op("dve", lambda e: e.reduce_sum(out=lams[:, 0:1], in_=lamt[:, 0:HD], axis=AX.X), reads=[("lamt", 0)], writes=[("lams", 0)])
        S.op("dve", lambda e: e.reduce_sum(out=lams[:, 1:2], in_=lamt[:, 2 * HD:3 * HD], axis=AX.X), reads=[("lamt", 2)], writes=[("lams", 1)])
        S.op("act", lambda e: e.activation(out=lams[:, 2:4], in_=lams[:, 0:2], func=AF.Exp), reads=[("lams", 0), ("lams", 1)], writes=[("lams", 2)])
        S.op("dve", lambda e: e.scalar_tensor_tensor(out=lams[:, 4:5], in0=lams[:, 3:4], scalar=-LAMBDA_INIT, in1=lams[:, 2:3],
                                                     op0=ALU.add, op1=ALU.subtract), reads=[("lams", 2)], writes=[("lams", 4)])
        S.op("dve", lambda e: e.tensor_scalar(out=lams[:, 5:6], in0=subg[:, 0:1], scalar1=1.0 - LAMBDA_INIT, scalar2=None, op0=ALU.mult),
             reads=[cres(subg)], writes=[("lams", 5)])
        neglam = lams[:, 4:5]
        gsub = lams[:, 5:6]

        def PS(i):
            return ("ps", i)

        rt.reset()
        wblk = [rt.get([8, 512], BF16) for _ in range(2)]
        S.dma("pool", ("wld", 0), lambda e: e.dma_start(out=wblk[0], in_=w_in_d[:, O_P:O_P + 512].rearrange("(c p) n -> p c n", p=128)), writes=[("wblk", 0)])
        S.dma("pool", ("wld", 1), lambda e: e.dma_start(out=wblk[1], in_=w_in_d[:, O_Q:O_Q + 512].rearrange("(c p) n -> p c n", p=128)), writes=[("wblk", 1)])
        xw = [view(R42, n * 8192, [8, 512], BF16) for n in range(3)]
        for n, c0_ in enumerate((O_Q + 512, O_K, O_K + 512)):
            S.dma("pool", ("xwld", n), lambda e, n=n, c0_=c0_: e.dma_start(out=xw[n], in_=w_in_d[:, c0_:c0_ + 512].rearrange("(c p) n -> p c n", p=128)),
                  writes=[("xw", n)])
        g1bc = rt.get([D], F32)
        NBA = 4
        xt = [rt.get([D], F32) for _ in range(NBA)]
        junk = rt.get([D], BF16)
        xn = [rt.get([D], BF16) for _ in range(NBA)]
        S.dma("sp", "miscS", lambda e: e.dma_start(out=g1bc, in_=g1_d.to_broadcast([128, D])), writes=["g1bc"])
        S.op("pool", lambda e: e.memset(xt[0], 0.0), writes=[("xt", 0)])
        def a_s1(i):
            b = i % NBA
            if i == 0:
                S.dma("sp", ("xt", 0), lambda e: e.dma_start(out=xt[0][MOFF:128, :], in_=meta_d), writes=[("xt", 0)], reads=[])
            else:
                S.dma("sp", ("xt", b), lambda e: e.dma_start(out=xt[b], in_=x_d[(i - 1) * 128:i * 128, :]), writes=[("xt", b)])
            S.op("act", lambda e: e.activation(out=junk, in_=xt[b], func=AF.Square, accum_out=stats[:, i, 0:1]),
                 reads=[("xt", b)], writes=["junk", ("st", i)])
            S.op("act", lambda e: e.activation(out=stats[:, i, 1:2], in_=stats[:, i, 0:1], func=AF.Sqrt, scale=1.0 / D, bias=EPS),
                 reads=[("st", i)], writes=[("st1", i)])
            S.op("dve", lambda e: e.reciprocal(out=stats[:, i, 1:2], in_=stats[:, i, 1:2]), reads=[("st1", i)], writes=[("st1", i)])
            S.op("dve", lambda e: e.scalar_tensor_tensor(out=xn[b], in0=xt[b], scalar=stats[:, i, 1:2], in1=g1bc, op0=ALU.mult, op1=ALU.mult),
                 reads=[("xt", b), ("st1", i), "g1bc"], writes=[("xn", b)])

        def a_s2(i):
            b = i % NBA
            pbk = i % 4
            for c in range(8):
                S.op("pe", lambda e, c=c: e.transpose(out=pbf(pbk)[:, c * 128:(c + 1) * 128], in_=xn[b][:, c * 128:(c + 1) * 128], identity=ident[:, :]),
                     reads=[("xn", b), cres(ident)], writes=[PS(pbk)])
            S.op("act", lambda e: e.copy(out=hnT[:, :, i * 128:(i + 1) * 128], in_=pbf(pbk).rearrange("p (c n) -> p c n", c=8)),
                 reads=[PS(pbk)], writes=[("hnT", i)])

        for it_ in range(17 + 2):
            if it_ < 17:
                a_s1(it_)
            if 0 <= it_ - 2 < 17:
                a_s2(it_ - 2)
        HNT_ALL = [("hnT", i) for i in range(17)]

        if "hnT" in dbg:
            o = dbg_out("hnT", [128, 8 * TC])
            S.dma("pool", "dbgP", lambda e, o=o: e.dma_start(out=o.rearrange("p (c t) -> p c t", c=8), in_=hnT), reads=HNT_ALL, writes=["dbg_hnT"])

        def load_wblock(dst, src2d, c0, ncols, res, kch=8):
            S.dma("pool", ("wld", res[1]), lambda e: e.dma_start(out=dst, in_=src2d[:, c0:c0 + ncols].rearrange("(c p) n -> p c n", p=128)),
                  writes=[res])

        if active("B0"):
            S.barrier()
            rt.reset()
            wblk = [rt.get([8, 512], BF16) for _ in range(2)]
            u32 = [rt.get([TC], F32) for _ in range(2)]
            tmpA = rt.get([TC], F32)
            tmpB = rt.get([TC], F32)
            psrot = [0]

            def nextbank(lo, hi):
                b_ = lo + psrot[0] % (hi - lo)
                psrot[0] += 1
                return b_

            def b0_proj(g):
                ub = u32[g % 2]
                ures = ("u32", g % 2)
                for jt in range(5):
                    c0, n = (0, 128) if jt == 0 else (ROFF + (jt - 1) * 512, 512)
                    bk = nextbank(0, 4)
                    for c in range(8):
                        S.op("pe", lambda e, c=c, g=g, c0=c0, n=n, bk=bk: e.matmul(pbanks[bk][:, 0:n], lhsT=wblk[0][:, c, g * 128:(g + 1) * 128],
                                                                                 rhs=hnT[:, c, c0:c0 + n], start=(c == 0), stop=(c == 7)),
                             reads=[("wblk", 0)] + HNT_ALL, writes=[PS(bk)])
                    S.op("act", lambda e, ub=ub, c0=c0, n=n, bk=bk: e.copy(out=ub[:, c0:c0 + n], in_=pbanks[bk][:, 0:n]),
                         reads=[PS(bk)], writes=[ures])

            def b0_pool(g):
                ub = u32[g % 2]
                ures = ("u32", g % 2)
                w = POOL_W[g]
                L = TC
                cur, cur_res = ub, ures
                step = 1
                bufs = [(tmpA, "tmpA"), (tmpB, "tmpB")]
                k = 0
                while step < w:
                    dst, dres = bufs[k % 2]
                    lo = 2 * step - 1
                    S.op("dve", lambda e, dst=dst, cur=cur, lo=lo, step=step: e.tensor_tensor(out=dst[:, lo:L], in0=cur[:, lo:L], in1=cur[:, lo - step:L - step], op=ALU.add),
                         reads=[cur_res], writes=[dres])
                    cur, cur_res = dst, dres
                    step *= 2
                    k += 1
                ydst, yres = bufs[k % 2]
                ybf = ydst[:, 0:SEQ // 2].bitcast(BF16)
                S.op("dve", lambda e, ybf=ybf, cur=cur, ub=ub, w=w: e.scalar_tensor_tensor(out=ybf, in0=cur[:, ROFF:TC], scalar=1.0 / w, in1=ub[:, ROFF:TC],
                                                                                            op0=ALU.mult, op1=ALU.subtract),
                     reads=[cur_res, ures], writes=[yres])
                for jt in range(4):
                    bk = nextbank(0, 4)
                    S.op("pe", lambda e, g=g, jt=jt, bk=bk, ybf=ybf: e.matmul(pbanks[bk][:, :], lhsT=poolw[:, g, :], rhs=ybf[:, jt * 512:(jt + 1) * 512], start=True, stop=True),
                         reads=[yres, cres(poolw)], writes=[PS(bk)])
                    S.op("act", lambda e, g=g, jt=jt, bk=bk: e.activation(out=PT[:, g, jt * 512:(jt + 1) * 512], in_=pbanks[bk][:, :], func=AF.Identity, scale=pscale[:, g:g + 1]),
                         reads=[PS(bk), cres(pscale)], writes=[("PT", g)])

            for it_ in range(NPG + 1):
                if it_ < NPG:
                    b0_proj(it_)
                if it_ >= 1:
                    b0_pool(it_ - 1)
            if "PT" in dbg:
                o = dbg_out("PT", [128, NPG * SEQ])
                S.dma("pool", "dbgP", lambda e, o=o: e.dma_start(out=o.rearrange("p (c t) -> p c t", c=NPG), in_=PT), reads=[("PT", g) for g in range(NPG)], writes=["dbg_PT"])

        if active("B1"):
            S.barrier()
            rt.reset()
            wblk = [rt.get([8, 512], BF16) for _ in range(2)]
            cosT = rt.get([TC], F32)
            sinT = rt.get([TC], F32)
            zb = [rt.get([512], BF16) for _ in range(3)]
            t1 = [rt.get([512], F32) for _ in range(3)]
            t2 = [rt.get([512], F32) for _ in range(3)]
            ZB_ = (0, 1, 6)
            RB_ = (2, 3, 7)
            S.dma("sp", "miscS", lambda e: e.dma_start(out=cosT, in_=cos_d), writes=["cosT"])
            S.dma("sp", "miscS", lambda e: e.dma_start(out=sinT, in_=sin_d), writes=["sinT"])
            blocks = [("q", 0), ("q", 1), ("k", 0), ("k", 1), ("v", 0), ("v", 1)]
            col_of = {"q": O_Q, "k": O_K, "v": O_V}
            items = []

            def stage1(it):
                kind, h, c0, n, wb, nchunk, slot = it
                wbuf, wres = wb
                sl = slot % 3
                bk = ZB_[sl]
                for c in range(8):
                    S.op("pe", lambda e, c=c: e.matmul(pbanks[bk][:, 0:n], lhsT=wbuf[:, c, nchunk * 128:(nchunk + 1) * 128], rhs=hnT[:, c, c0:c0 + n],
                                                       start=(c == 0), stop=(c == 7)),
                         reads=[wres] + HNT_ALL, writes=[PS(bk)])
                S.op("act", lambda e: e.copy(out=zb[sl][:, 0:n], in_=pbanks[bk][:, 0:n]), reads=[PS(bk)], writes=[("zb", sl)])

            def stage2(it):
                kind, h, c0, n, wb, nchunk, slot = it
                sl = slot % 3
                bk = ZB_[sl]
                rb = RB_[sl]
                S.op("pe", lambda e: e.matmul(pbanks[rb][:, 0:n], lhsT=permm[:, :], rhs=zb[sl][:, 0:n], start=True, stop=True),
                     reads=[("zb", sl), cres(permm)], writes=[PS(rb)])
                S.op("dve", lambda e: e.tensor_tensor(out=t1[sl][:, 0:n], in0=pbanks[bk][:, 0:n], in1=cosT[:, c0:c0 + n], op=ALU.mult),
                     reads=[PS(bk), "cosT"], writes=[("t1", sl)])
                S.op("dve", lambda e: e.tensor_tensor(out=t2[sl][:, 0:n], in0=pbanks[rb][:, 0:n], in1=sinT[:, c0:c0 + n], op=ALU.mult),
                     reads=[PS(rb), "sinT"], writes=[("t2", sl)])
                if kind == "q":
                    dst = QT[:, h, c0 - ROFF:c0 - ROFF + n]
                    dres = ("QT", h, (c0 - ROFF) // 512)
                else:
                    dst = KT[:, h, c0:c0 + n]
                    dres = ("KT", h)
                S.op("pool", lambda e: e.tensor_tensor(out=dst, in0=t1[sl][:, 0:n], in1=t2[sl][:, 0:n], op=ALU.add),
                     reads=[("t1", sl), ("t2", sl)], writes=[dres])

            slot = 0
            prev = None
            wplan = [(wblk[1], ("wblk", 1)), (xw[0], ("xw", 0)), (xw[1], ("xw", 1)), (xw[2], ("xw", 2)), (wblk[1], ("wblk", 1)), (wblk[0], ("wblk", 0))]
            load_wblock(wblk[0], w_in_d, O_V + 512, 512, ("wblk", 0))
            for bi, (kind, half) in enumerate(blocks):
                wb = wplan[bi]
                if bi == 1:
                    load_wblock(wblk[1], w_in_d, O_V, 512, ("wblk", 1))
                if kind in ("q", "k"):
                    for nchunk in range(4):
                        h = half * 4 + nchunk
                        tiles = ([(0, 128)] if kind == "k" else []) + [(ROFF + jt * 512, 512) for jt in range(4)]
                        for (c0, n) in tiles:
                            it = (kind, h, c0, n, wb, nchunk, slot)
                            slot += 1
                            stage1(it)
                            if prev is not None:
                                stage2(prev)
                            prev = it
                else:
                    if prev is not None:
                        stage2(prev)
                        prev = None
                    for i in range(17):
                        for sub in range(1):
                            bk = 4 + (i % 2)
                            for c in range(8):
                                S.op("pe", lambda e, c=c, i=i, bk=bk, wb=wb: e.matmul(pbanks[bk][:, :], lhsT=hnT[:, c, i * 128:(i + 1) * 128], rhs=wb[0][:, c, :],
                                                                                     start=(c == 0), stop=(c == 7)),
                                     reads=[wb[1], ("hnT", i)], writes=[PS(bk)])
                            S.op("act", lambda e, i=i, bk=bk, half=half: e.copy(out=Vt[:, i, half * 512:(half + 1) * 512], in_=pbanks[bk][:, :]),
                                 reads=[PS(bk)], writes=[("V", i), ("xw", 0), ("xw", 1), ("xw", 2)])
            if prev is not None:
                stage2(prev)
                prev = None
            for nm, tns, shp, res in (("QT", QT, [128, 8 * SEQ], [("QT", h, j) for h in range(8) for j in range(4)]),
                                      ("KT", KT, [128, 8 * TC], [("KT", h) for h in range(8)]),
                                      ("V", Vt, [128, 17 * D], [("V", i) for i in range(17)])):
                if nm in dbg:
                    o = dbg_out(nm, shp)
                    S.dma("pool", "dbgP", lambda e, o=o, tns=tns: e.dma_start(out=o.rearrange("p (c t) -> p c t", c=tns.shape[1]), in_=tns), reads=res, writes=["dbg_" + nm])

        def load_e1(dc, bufs):
            b = dc % 2
            wa_, wp_, wg0_, wg1_ = bufs["wa"], bufs["wp"], bufs["wg0"], bufs["wg1"]
            S.dma("pool", ("e1w", b), lambda e: e.dma_start(out=wa_[b], in_=wattn_d[:, dc * 128:(dc + 1) * 128].rearrange("(c p) n -> p c n", p=128)), writes=[("wa", b)])
            S.dma("pool", ("e1w", b), lambda e: e.dma_start(out=wp_[b], in_=wpool_d[:, dc * 128:(dc + 1) * 128].rearrange("(c p) n -> p c n", p=128)), writes=[("wp", b)])
            S.dma("pool", ("e1w", b), lambda e: e.dma_start(out=wg0_[b], in_=w_in_d[:, O_G + dc * 128:O_G + (dc + 1) * 128].rearrange("(c p) n -> p c n", p=128)), writes=[("wg0", b)])
            S.dma("pool", ("e1w", b), lambda e: e.dma_start(out=wg1_[b], in_=w_in_d[:, O_G + D + dc * 128:O_G + D + (dc + 1) * 128].rearrange("(c p) n -> p c n", p=128)), writes=[("wg1", b)])

        if active("D"):
            S.barrier()
            rt.reset()
            wout_sb = rt.get([8, D], BF16)
            S.dma("pool", "miscP", lambda e: e.dma_start(out=wout_sb, in_=wout_d.rearrange("(c p) n -> p c n", p=128)), writes=["wout"])
            e1bufs = dict(wa=[rt.get([8, 128], BF16) for _ in range(2)], wp=[rt.get([4, 128], BF16) for _ in range(2)],
                          wg0=[rt.get([8, 128], BF16) for _ in range(2)], wg1=[rt.get([8, 128], BF16) for _ in range(2)])
            if active("E1"):
                load_e1(0, e1bufs)
                load_e1(1, e1bufs)
            pTb = [rt.get([2, 512], BF16) for _ in range(3)]
            r2 = rt.get([2, 512], F32)
            o_t = [rt.get([512], F32) for _ in range(2)]
            a_t = rt.get([512], F32)
            sq_t = rt.get([512], BF16)
            ln_t = rt.get([512], F32)
            ACC_O = (0, 2)
            ACC_S = (1, 3)
            SCALE = 1.0 / math.sqrt(HD)
            cnts = {"s": 0, "pt": 0}

            items = []
            prev_hj = None
            for h in range(NH):
                for j in range(4):
                    kts = [(0, 0, 0, False)]
                    for i in range(4 * j):
                        kts.append((1 + i, ROFF + i * 128, 0, False))
                    for r in range(4):
                        i = 4 * j + r
                        kts.append((1 + i, ROFF + i * 128, 128 * r, True))
                    for idx, kt in enumerate(kts):
                        items.append(("kt", h, j, kt, idx == 0, idx == len(kts) - 1))
                        if idx == min(5, len(kts) - 2) and prev_hj is not None:
                            items.append(("norm",) + prev_hj)
                    prev_hj = (h, j)
            final_norm = ("norm",) + prev_hj

            def stage_s(it):
                p = cnts["s"] % 2
                cnts["s"] += 1
                if it[0] == "norm":
                    _, h, j = it
                    S.op("pe", lambda e: e.matmul(pS[p][:, 0:512], lhsT=ones[:, :], rhs=sq_t, start=True, stop=True),
                         reads=["sq_t", cres(ones)], writes=[PS(4 + 2 * p)])
                    S.op("act", lambda e: e.activation(out=ln_t, in_=pS[p][:, 0:512], func=AF.Ln, scale=1.0 / VD, bias=EPS),
                         reads=[PS(4 + 2 * p)], writes=["ln_t"])
                    S.op("act", lambda e: e.activation(out=ln_t, in_=ln_t, func=AF.Exp, scale=-0.5), reads=["ln_t"], writes=["ln_t"])
                    return None
                _, h, j, (vt_i, kc0, q0, diag), first, last = it
                par = h % 2
                pb_i = cnts["pt"] % 3
                cnts["pt"] += 1
                for c in range(2):
                    S.op("pe", lambda e, c=c: e.matmul(pS[p][:, c * 512 + q0:(c + 1) * 512], lhsT=KT[c * 64:(c + 1) * 64, h, kc0:kc0 + 128],
                                                       rhs=QT[c * 64:(c + 1) * 64, h, j * 512 + q0:(j + 1) * 512], start=True, stop=not diag),
                         reads=[("KT", h), ("QT", h, j)], writes=[PS(4 + 2 * p + c)])
                    if diag:
                        S.op("pe", lambda e, c=c: e.matmul(pS[p][:, c * 512 + q0:c * 512 + q0 + 128], lhsT=ident[:, :], rhs=negm[:, :], start=False, stop=True),
                             reads=[cres(ident), cres(negm)], writes=[PS(4 + 2 * p + c)])
                S.op("act", lambda e: e.activation(out=pTb[pb_i][:, :, q0:512], in_=pS[p][:, :].rearrange("p (c n) -> p c n", c=2)[:, :, q0:512],
                                                   func=AF.Exp, scale=SCALE),
                     reads=[PS(4 + 2 * p), PS(4 + 2 * p + 1)], writes=[("pT", pb_i)])
                return pb_i

            def stage_av(it, pb_i):
                if it[0] == "norm":
                    _, h, j = it
                    S.op("dve", lambda e: e.scalar_tensor_tensor(out=QT[:, h, j * 512:(j + 1) * 512], in0=a_t, scalar=gsub, in1=ln_t, op0=ALU.mult, op1=ALU.mult),
                         reads=["a_t", "ln_t", ("lams", 5)], writes=[("QT", h, j)])
                    return
                _, h, j, (vt_i, kc0, q0, diag), first, last = it
                on = ones16 if vt_i == 0 else ones
                for c in range(2):
                    S.op("pe", lambda e, c=c: e.matmul(pbanks[ACC_O[c]][:, q0:512], lhsT=Vt[:, vt_i, h * 128:(h + 1) * 128], rhs=pTb[pb_i][:, c, q0:512],
                                                       start=first, stop=last),
                         reads=[("V", vt_i), ("pT", pb_i)], writes=[PS(ACC_O[c])])
                    S.op("pe", lambda e, c=c: e.matmul(pbanks[ACC_S[c]][:, q0:512], lhsT=on[:, :], rhs=pTb[pb_i][:, c, q0:512],
                                                       start=first, stop=last),
                         reads=[cres(on), ("pT", pb_i)], writes=[PS(ACC_S[c])])
                if last:
                    for c in range(2):
                        S.op("act", lambda e, c=c: e.activation(out=r2[:, c, :], in_=pbanks[ACC_S[c]], func=AF.Ln), reads=[PS(ACC_S[c])], writes=["r2"])
                        S.op("dve", lambda e, c=c: e.tensor_copy(out=o_t[c], in_=pbanks[ACC_O[c]]), reads=[PS(ACC_O[c])], writes=[("o_t", c)])
                    S.op("act", lambda e: e.activation(out=r2[:, :, :], in_=r2[:, :, :], func=AF.Exp, scale=-1.0), reads=["r2"], writes=["r2"])
                    for c in range(2):
                        S.op("dve", lambda e, c=c: e.tensor_tensor(out=o_t[c], in0=o_t[c], in1=r2[:, c, :], op=ALU.mult),
                             reads=[("o_t", c), "r2"], writes=[("o_t", c)])
                    S.op("dve", lambda e: e.scalar_tensor_tensor(out=a_t, in0=o_t[1], scalar=neglam, in1=o_t[0], op0=ALU.mult, op1=ALU.add),
                         reads=[("o_t", 0), ("o_t", 1), ("lams", 4)], writes=["a_t"])
                    S.op("pool", lambda e: e.tensor_tensor(out=sq_t, in0=a_t, in1=a_t, op=ALU.mult), reads=["a_t"], writes=["sq_t"])

            prev_it, prev_pb = None, None
            for it in items + [None]:
                pb_new = stage_s(it) if it is not None else None
                if prev_it is not None:
                    stage_av(prev_it, prev_pb)
                prev_it, prev_pb = it, pb_new
            stage_s(final_norm)
            stage_av(final_norm, None)
            if "AT" in dbg:
                o = dbg_out("AT", [128, 8 * SEQ])
                S.dma("pool", "dbgP", lambda e, o=o: e.dma_start(out=o.rearrange("p (c t) -> p c t", c=8), in_=QT), reads=[("QT", h, j) for h in range(8) for j in range(4)], writes=["dbg_AT"])

        if active("E1"):
            S.barrier()
            rt.reset()
            wout_sb = rt.get([8, D], BF16)
            wa = [rt.get([8, 128], BF16) for _ in range(2)]
            wp = [rt.get([4, 128], BF16) for _ in range(2)]
            wg0 = [rt.get([8, 128], BF16) for _ in range(2)]
            wg1 = [rt.get([8, 128], BF16) for _ in range(2)]
            gt = [[rt.get([512], F32) for _ in range(2)] for _ in range(2)]
            mt = [[rt.get([512], F32) for _ in range(2)] for _ in range(2)]
            ATR = [("QT", h, j) for h in range(8) for j in range(4)]

            def e1_tile(dc, j, b, pr):
                cs = slice(ROFF + j * 512, ROFF + (j + 1) * 512)
                qs = slice(j * 512, (j + 1) * 512)
                bk_g0, bk_a, bk_g1, bk_p = (0 + pr, 2 + pr, 4 + pr, 6 + pr)
                for c in range(8):
                    S.op("pe", lambda e, c=c: e.matmul(pbanks[bk_g0][:, :], lhsT=wg0[b][:, c, :], rhs=hnT[:, c, cs], start=(c == 0), stop=(c == 7)),
                         reads=[("wg0", b)] + HNT_ALL, writes=[PS(bk_g0)])
                S.op("act", lambda e: e.activation(out=gt[0][pr], in_=pbanks[bk_g0][:, :], func=AF.Sigmoid, bias=bgate[:, dc:dc + 1]),
                     reads=[PS(bk_g0), cres(bgate)], writes=[("gt", 0, pr)])
                for c in range(8):
                    S.op("pe", lambda e, c=c: e.matmul(pbanks[bk_a][:, :], lhsT=wa[b][:, c, :], rhs=QT[:, c, qs], start=(c == 0), stop=(c == 7)),
                         reads=[("wa", b)] + ATR, writes=[PS(bk_a)])
                S.op("dve", lambda e: e.tensor_tensor(out=mt[0][pr], in0=pbanks[bk_a][:, :], in1=gt[0][pr], op=ALU.mult),
                     reads=[PS(bk_a), ("gt", 0, pr)], writes=[("mt", 0, pr)])
                for c in range(8):
                    S.op("pe", lambda e, c=c: e.matmul(pbanks[bk_g1][:, :], lhsT=wg1[b][:, c, :], rhs=hnT[:, c, cs], start=(c == 0), stop=(c == 7)),
                         reads=[("wg1", b)] + HNT_ALL, writes=[PS(bk_g1)])
                S.op("act", lambda e: e.activation(out=gt[1][pr], in_=pbanks[bk_g1][:, :], func=AF.Sigmoid, bias=bgate[:, 8 + dc:9 + dc]),
                     reads=[PS(bk_g1), cres(bgate)], writes=[("gt", 1, pr)])
                for c in range(4):
                    S.op("pe", lambda e, c=c: e.matmul(pbanks[bk_p][:, :], lhsT=wp[b][:, c, :], rhs=PT[:, c, qs], start=(c == 0), stop=(c == 3)),
                         reads=[("wp", b)] + [("PT", g) for g in range(NPG)], writes=[PS(bk_p)])
                S.op("dve", lambda e: e.tensor_tensor(out=mt[1][pr], in0=pbanks[bk_p][:, :], in1=gt[1][pr], op=ALU.mult),
                     reads=[PS(bk_p), ("gt", 1, pr)], writes=[("mt", 1, pr)])
                S.op("pool", lambda e: e.tensor_tensor(out=mixT[:, dc, qs], in0=mt[0][pr], in1=mt[1][pr], op=ALU.add),
                     reads=[("mt", 0, pr), ("mt", 1, pr)], writes=[("mixT", j)])

            e1bufs = dict(wa=wa, wp=wp, wg0=wg0, wg1=wg1)
            if not active("D"):
                load_e1(0, e1bufs)
                load_e1(1, e1bufs)
            cnt = 0
            for dc in range(8):
                b = dc % 2
                if dc >= 1 and dc + 1 < 8:
                    load_e1(dc + 1, e1bufs)
                for j in range(4):
                    e1_tile(dc, j, b, cnt % 2)
                    cnt += 1
            if "mixT" in dbg:
                o = dbg_out("mixT", [128, 8 * SEQ])
                S.dma("pool", "dbgP", lambda e, o=o: e.dma_start(out=o.rearrange("p (c t) -> p c t", c=8), in_=mixT), reads=[("mixT", j) for j in range(4)], writes=["dbg_mixT"])

        if active("E2"):
            S.barrier()
            rt.reset()
            wout_sb = rt.get([8, D], BF16)
            x2 = [rt.get([D], F32) for _ in range(2)]
            g2bc = rt.get([D], F32)
            junk2 = rt.get([D], BF16)
            hn2 = [rt.get([D], BF16) for _ in range(2)]
            S.dma("sp", "miscS", lambda e: e.dma_start(out=g2bc, in_=g2_d.to_broadcast([128, D])), writes=["g2bc"])
            if active("F"):
                S.dma("pool", "exg0", lambda e: e.dma_start(out=wg0x, in_=weg_d[0].rearrange("(c p) n -> p c n", p=128)), writes=["wg0x"])
                S.dma("pool", ("exw", 1), lambda e: e.dma_start(out=wup[1], in_=weu_d[0].rearrange("(c p) n -> p c n", p=128)), writes=[("wup", 1)])
                S.dma("pool", ("exw", 1), lambda e: e.dma_start(out=wdn[1], in_=wed_d[0].rearrange("(c p) n -> p c n", p=128)), writes=[("wdn", 1)])
            MIXR = [("mixT", j) for j in range(4)]
            def e2_s1(i):
                b = i % 2
                S.dma("sp", ("x2", b), lambda e: e.dma_start(out=x2[b], in_=x_d[i * 128:(i + 1) * 128, :]), writes=[("x2", b)])
                for half in range(2):
                    bk = (2 * i + half) % 4
                    for c in range(8):
                        S.op("pe", lambda e, c=c, half=half, bk=bk: e.matmul(pbanks[bk][:, :], lhsT=mixT[:, c, i * 128:(i + 1) * 128],
                                                                             rhs=wout_sb[:, c, half * 512:(half + 1) * 512], start=(c == 0), stop=(c == 7)),
                             reads=["wout"] + MIXR, writes=[PS(bk)])
                    S.op("dve", lambda e, half=half, bk=bk: e.tensor_tensor(out=h1[:, i, half * 512:(half + 1) * 512], in0=pbanks[bk][:, :],
                                                                            in1=x2[b][:, half * 512:(half + 1) * 512], op=ALU.add),
                         reads=[PS(bk), ("x2", b)], writes=[("h1", i)])
                S.op("act", lambda e: e.activation(out=junk2, in_=h1[:, i, :], func=AF.Square, accum_out=stats2[:, i, 0:1]),
                     reads=[("h1", i)], writes=["junk2", ("s2", i)])
                S.op("act", lambda e: e.activation(out=stats2[:, i, 1:2], in_=stats2[:, i, 0:1], func=AF.Sqrt, scale=1.0 / D, bias=EPS),
                     reads=[("s2", i)], writes=[("s21", i)])
                S.op("dve", lambda e: e.reciprocal(out=stats2[:, i, 1:2], in_=stats2[:, i, 1:2]), reads=[("s21", i)], writes=[("s21", i)])
                S.op("dve", lambda e: e.scalar_tensor_tensor(out=hn2[b], in0=h1[:, i, :], scalar=stats2[:, i, 1:2], in1=g2bc, op0=ALU.mult, op1=ALU.mult),
                     reads=[("h1", i), ("s21", i), "g2bc"], writes=[("hn2", b)])

            def e2_s2(i):
                b = i % 2
                pbk = 4 + i % 2
                for c in range(8):
                    S.op("pe", lambda e, c=c: e.transpose(out=pbf(pbk)[:, c * 128:(c + 1) * 128], in_=hn2[b][:, c * 128:(c + 1) * 128], identity=ident[:, :]),
                         reads=[("hn2", b), cres(ident)], writes=[PS(pbk)])
                S.op("act", lambda e: e.copy(out=hn2T[:, :, i * 128:(i + 1) * 128], in_=pbf(pbk).rearrange("p (c n) -> p c n", c=8)),
                     reads=[PS(pbk)], writes=[("hn2T", i)])

            def e2_s3(i):
                rbk = 6 + i % 2
                for c in range(8):
                    S.op("pe", lambda e, c=c: e.matmul(pbanks[rbk][:, 0:20], lhsT=hn2T[:, c, i * 128:(i + 1) * 128], rhs=wr_sb[:, c, :],
                                                       start=(c == 0), stop=(c == 7)),
                         reads=[("hn2T", i), cres(wr_sb)], writes=[PS(rbk)])
                S.op("dve", lambda e: e.tensor_tensor(out=rl[:, i, :], in0=pbanks[rbk][:, 0:20], in1=br_sb[:, :], op=ALU.add),
                     reads=[PS(rbk), cres(br_sb)], writes=[("rl", i)])

            for it_ in range(16 + 2):
                if it_ < 16:
                    e2_s1(it_)
                if 0 <= it_ - 1 < 16:
                    e2_s2(it_ - 1)
                if 0 <= it_ - 2 < 16:
                    e2_s3(it_ - 2)
            if "h1" in dbg:
                o = dbg_out("h1", [128, 16 * D])
                S.dma("sp", "dbgS", lambda e, o=o: e.dma_start(out=o.rearrange("p (c t) -> p c t", c=16), in_=h1), reads=[("h1", i) for i in range(16)], writes=["dbg_h1"])
            if "rl" in dbg:
                o = dbg_out("rl", [128, 16 * 20])
                S.dma("sp", "dbgS", lambda e, o=o: e.dma_start(out=o.rearrange("p (c t) -> p c t", c=16), in_=rl), reads=[("rl", i) for i in range(16)], writes=["dbg_rl"])

        if active("F"):
            S.barrier()
            rt.reset()
            RL = [("rl", i) for i in range(16)]
            glog = rl[:, :, 0:4]
            gmx = rw[:, :, 0:1]
            gex = rw[:, :, 4:8]
            gsum = rw[:, :, 8:9]
            goh = rw[:, :, 12:16]
            esel = rw[:, :, 16:20]
            etmp = rw[:, :, 20:24]
            m1 = rw[:, :, 24:25]
            oh1 = rw[:, :, 28:32]
            e2v = rw[:, :, 32:36]
            m2 = rw[:, :, 36:37]
            oh2 = rw[:, :, 40:44]
            dd = rw[:, :, 44:45]
            w1 = rw[:, :, 45:46]
            w2_ = rw[:, :, 46:47]
            c4 = rw[:, :, 48:52]
            RW = "rw"
            V_ = S.op
            V_("dve", lambda e: e.tensor_reduce(out=gmx, in_=glog, axis=AX.X, op=ALU.max), reads=RL, writes=[RW])
            V_("dve", lambda e: e.tensor_tensor(out=gex, in0=glog, in1=gmx.to_broadcast([128, 16, 4]), op=ALU.subtract), reads=RL + [RW], writes=[RW])
            V_("dve", lambda e: e.tensor_tensor(out=goh, in0=glog, in1=gmx.to_broadcast([128, 16, 4]), op=ALU.is_ge), reads=RL + [RW], writes=[RW])
            V_("act", lambda e: e.activation(out=gex, in_=gex, func=AF.Exp), reads=[RW], writes=[RW])
            V_("dve", lambda e: e.tensor_reduce(out=gsum, in_=gex, axis=AX.X, op=ALU.add), reads=[RW], writes=[RW])
            V_("dve", lambda e: e.reciprocal(out=gsum, in_=gsum), reads=[RW], writes=[RW])
            for g in range(4):
                if g == 0:
                    V_("dve", lambda e: e.tensor_tensor(out=esel, in0=rl[:, :, 4:8], in1=goh[:, :, 0:1].to_broadcast([128, 16, 4]), op=ALU.mult), reads=RL + [RW], writes=[RW])
                else:
                    V_("dve", lambda e, g=g: e.tensor_tensor(out=etmp, in0=rl[:, :, 4 + 4 * g:8 + 4 * g], in1=goh[:, :, g:g + 1].to_broadcast([128, 16, 4]), op=ALU.mult),
                       reads=RL + [RW], writes=[RW])
                    V_("dve", lambda e: e.tensor_tensor(out=esel, in0=esel, in1=etmp, op=ALU.add), reads=[RW], writes=[RW])
            V_("dve", lambda e: e.tensor_reduce(out=m1, in_=esel, axis=AX.X, op=ALU.max), reads=[RW], writes=[RW])
            V_("dve", lambda e: e.tensor_tensor(out=oh1, in0=esel, in1=m1.to_broadcast([128, 16, 4]), op=ALU.is_ge), reads=[RW], writes=[RW])
            V_("dve", lambda e: e.scalar_tensor_tensor(out=e2v, in0=oh1, scalar=-1e30, in1=esel, op0=ALU.mult, op1=ALU.add), reads=[RW], writes=[RW])
            V_("dve", lambda e: e.tensor_reduce(out=m2, in_=e2v, axis=AX.X, op=ALU.max), reads=[RW], writes=[RW])
            V_("dve", lambda e: e.tensor_tensor(out=oh2, in0=e2v, in1=m2.to_broadcast([128, 16, 4]), op=ALU.is_ge), reads=[RW], writes=[RW])
            V_("dve", lambda e: e.tensor_tensor(out=dd, in0=m2, in1=m1, op=ALU.subtract), reads=[RW], writes=[RW])
            V_("act", lambda e: e.activation(out=dd, in_=dd, func=AF.Exp), reads=[RW], writes=[RW])
            V_("dve", lambda e: e.tensor_scalar(out=w1, in0=dd, scalar1=1.0, scalar2=None, op0=ALU.add), reads=[RW], writes=[RW])
            V_("dve", lambda e: e.reciprocal(out=w1, in_=w1), reads=[RW], writes=[RW])
            V_("dve", lambda e: e.tensor_tensor(out=w1, in0=w1, in1=gsum, op=ALU.mult), reads=[RW], writes=[RW])
            V_("dve", lambda e: e.tensor_tensor(out=w2_, in0=w1, in1=dd, op=ALU.mult), reads=[RW], writes=[RW])
            V_("dve", lambda e: e.tensor_tensor(out=c4, in0=oh1, in1=w1.to_broadcast([128, 16, 4]), op=ALU.mult), reads=[RW], writes=[RW])
            V_("dve", lambda e: e.tensor_tensor(out=etmp, in0=oh2, in1=w2_.to_broadcast([128, 16, 4]), op=ALU.mult), reads=[RW], writes=[RW])
            V_("dve", lambda e: e.tensor_tensor(out=c4, in0=c4, in1=etmp, op=ALU.add), reads=[RW], writes=[RW])
            for g in range(4):
                V_("dve", lambda e, g=g: e.tensor_tensor(out=comb[:, :, 4 * g:4 * g + 4], in0=c4, in1=goh[:, :, g:g + 1].to_broadcast([128, 16, 4]), op=ALU.mult),
                   reads=[RW], writes=["comb"])
            if "comb" in dbg:
                o = dbg_out("comb", [128, 16 * 16])
                S.dma("sp", "dbgS", lambda e, o=o: e.dma_start(out=o.rearrange("p (c t) -> p c t", c=16), in_=comb), reads=["comb"], writes=["dbg_comb"])

            sg = [rt.get([512], F32) for _ in range(2)]
            At = [rt.get([4, 512], BF16) for _ in range(2)]
            HN2R = [("hn2T", i) for i in range(16)]

            def load_expert(ex):
                b = (ex + 1) % 2
                if ex == 0:
                    return
                S.dma("pool", ("exw", b), lambda e: e.dma_start(out=wgt[b], in_=weg_d[ex].rearrange("(c p) n -> p c n", p=128)), writes=[("wgt", b)])
                S.dma("pool", ("exw", b), lambda e: e.dma_start(out=wup[b], in_=weu_d[ex].rearrange("(c p) n -> p c n", p=128)), writes=[("wup", b)])
                S.dma("pool", ("exw", b), lambda e: e.dma_start(out=wdn[b], in_=wed_d[ex].rearrange("(c p) n -> p c n", p=128)), writes=[("wdn", b)])

            do_g = active("G")
            if do_g:
                gfbc = rt.get([D], F32)
                junk3 = rt.get([D], BF16)
                ot = [rt.get([D], F32) for _ in range(2)]
                S.dma("sp", "miscS", lambda e: e.dma_start(out=gfbc, in_=gf_d.to_broadcast([128, D])), writes=["gfbc"])

            def g_tile(i):
                b = i % 2
                S.op("act", lambda e: e.activation(out=junk3, in_=h1[:, i, :], func=AF.Square, accum_out=stats3[:, i, 0:1]),
                     reads=[("h1", i)], writes=["junk3", ("s3", i)])
                S.op("act", lambda e: e.activation(out=stats3[:, i, 1:2], in_=stats3[:, i, 0:1], func=AF.Sqrt, scale=1.0 / D, bias=EPS),
                     reads=[("s3", i)], writes=[("s31", i)])
                S.op("dve", lambda e: e.reciprocal(out=stats3[:, i, 1:2], in_=stats3[:, i, 1:2]), reads=[("s31", i)], writes=[("s31", i)])
                S.op("dve", lambda e: e.scalar_tensor_tensor(out=ot[b], in0=h1[:, i, :], scalar=stats3[:, i, 1:2], in1=gfbc, op0=ALU.mult, op1=ALU.mult),
                     reads=[("h1", i), ("s31", i), "gfbc"], writes=[("ot", b)])
                S.dma("sp", ("st", b), lambda e: e.dma_start(out=out_d[i * 128:(i + 1) * 128, :], in_=ot[b]), reads=[("ot", b)], writes=[("out", i)])

            cnt_f = [0, 0]
            def f_gu(ex, j, b, ab):
                ts_ = slice(j * 512, (j + 1) * 512)
                for f in range(4):
                    pr = cnt_f[0] % 2
                    cnt_f[0] += 1
                    bg, bu = 0 + pr, 2 + pr
                    for c in range(8):
                        S.op("pe", lambda e, c=c, f=f, bg=bg: e.matmul(pbanks[bg][:, :], lhsT=(wg0x if ex == 0 else wgt[b])[:, c, f * 128:(f + 1) * 128], rhs=hn2T[:, c, ts_],
                                                                       start=(c == 0), stop=(c == 7)),
                             reads=[("wg0x" if ex == 0 else ("wgt", b))] + HN2R, writes=[PS(bg)])
                    S.op("act", lambda e, pr=pr, bg=bg: e.activation(out=sg[pr], in_=pbanks[bg][:, :], func=AF.Silu), reads=[PS(bg)], writes=[("sg", pr)])
                    for c in range(8):
                        S.op("pe", lambda e, c=c, f=f, bu=bu: e.matmul(pbanks[bu][:, :], lhsT=wup[b][:, c, f * 128:(f + 1) * 128], rhs=hn2T[:, c, ts_], start=(c == 0), stop=(c == 7)),
                             reads=[("wup", b)] + HN2R, writes=[PS(bu)])
                    S.op("dve", lambda e, pr=pr, f=f, bu=bu: e.tensor_tensor(out=At[ab][:, f, :], in0=pbanks[bu][:, :], in1=sg[pr], op=ALU.mult),
                         reads=[PS(bu), ("sg", pr)], writes=[("At", ab)])

            def f_down(ex, j, b, ab):
                for sub in range(4):
                    i = 4 * j + sub
                    for half in range(2):
                        bd = 4 + cnt_f[1] % 4
                        cnt_f[1] += 1
                        for f in range(4):
                            S.op("pe", lambda e, f=f, sub=sub, half=half, bd=bd: e.matmul(pbanks[bd][:, :], lhsT=At[ab][:, f, sub * 128:(sub + 1) * 128],
                                                                                          rhs=wdn[b][:, f, half * 512:(half + 1) * 512], start=(f == 0), stop=(f == 3)),
                                 reads=[("At", ab), ("wdn", b)], writes=[PS(bd)])
                        S.op("dve", lambda e, i=i, half=half, bd=bd, ex=ex: e.scalar_tensor_tensor(out=h1[:, i, half * 512:(half + 1) * 512], in0=pbanks[bd][:, :],
                                                                                                  scalar=comb[:, i, ex:ex + 1], in1=h1[:, i, half * 512:(half + 1) * 512],
                                                                                                  op0=ALU.mult, op1=ALU.add),
                             reads=[PS(bd), "comb", ("h1", i)], writes=[("h1", i)])
                    if do_g and ex == NE - 1:
                        g_tile(i)

            load_expert(0)
            fitems = [(ex, j) for ex in range(NE) for j in range(4)]
            for idx in range(len(fitems) + 1):
                if idx < len(fitems):
                    ex, j = fitems[idx]
                    f_gu(ex, j, (ex + 1) % 2, idx % 2)
                if idx >= 1:
                    exp_, jp_ = fitems[idx - 1]
                    f_down(exp_, jp_, (exp_ + 1) % 2, (idx - 1) % 2)
                if idx < len(fitems):
                    ex, j = fitems[idx]
                    if j == 0 and ex + 1 < NE:
                        load_expert(ex + 1)
            if "h2" in dbg:
                o = dbg_out("h2", [128, 16 * D])
                S.dma("sp", "dbgS", lambda e, o=o: e.dma_start(out=o.rearrange("p (c t) -> p c t", c=16), in_=h1), reads=[("h1", i) for i in range(16)], writes=["dbg_h2"])

        if active("G") and not active("F"):
            raise RuntimeError("phase G is fused into phase F")

        S.finish("sp")
        nsig = S.emit(nc)
    return nc, dbg_d, nsig


def _consts():
    f32 = np.float32
    inv = (1.0 / (10000.0 ** (np.arange(0, HD, 2, dtype=f32) / f32(HD)))).astype(f32)
    cos_t = np.zeros((128, TC), f32)
    sin_t = np.zeros((128, TC), f32)
    pos = np.arange(SEQ + NMETA, dtype=f32)
    ang = (pos[:, None] * inv[None, :]).astype(f32)
    ang = np.concatenate([ang, ang], axis=-1)
    c = np.cos(ang).astype(f32).T
    s = np.sin(ang).astype(f32).T
    s_signed = s.copy()
    s_signed[:HD // 2] *= -1.0
    for half in range(2):
        cos_t[half * 64:(half + 1) * 64, MOFF:] = c
        sin_t[half * 64:(half + 1) * 64, MOFF:] = s_signed
    ident = np.eye(128, dtype=f32)
    perm = np.zeros((128, 128), f32)
    for n2 in range(128):
        blk, d = divmod(n2, 64)
        perm[blk * 64 + (d + 32) % 64, n2] = 1.0
    k_ = np.arange(128)[:, None]
    q_ = np.arange(128)[None, :]
    tri = (q_ >= k_).astype(f32)
    tri = np.concatenate([tri, tri], axis=1)
    ones = np.ones((128, 128), f32)
    ones16 = np.zeros((128, 128), f32)
    ones16[MOFF:, :] = 1.0
    negm = np.where(q_ >= k_, 0.0, -30000.0).astype(f32)
    return dict(cos_t=cos_t, sin_t=sin_t, c_ident=ident, c_perm=perm, c_tri=tri, c_ones=ones, c_ones16=ones16, c_negm=negm)


def make_in_maps(inputs, n_cores=8):
    f = lambda a: np.ascontiguousarray(np.asarray(a, dtype=np.float32))
    x = f(inputs["x"])
    shared = dict(
        meta=f(inputs["meta"]),
        norm1_g=f(inputs["norm1_g"]).reshape(1, D),
        w_in=f(inputs["w_in"]).reshape(D, IN_W),
        b_gate_t=np.ascontiguousarray(f(inputs["b_gate"]).reshape(16, 128).T),
        lambda_q1=f(inputs["lambda_q1"]).reshape(1, HD),
        lambda_k1=f(inputs["lambda_k1"]).reshape(1, HD),
        lambda_q2=f(inputs["lambda_q2"]).reshape(1, HD),
        lambda_k2=f(inputs["lambda_k2"]).reshape(1, HD),
        subln_g_t=f(inputs["subln_g"]).reshape(128, 1),
        pool_w=f(inputs["pool_w"]).reshape(NPG, 128, 128),
        pool_scale_t=np.ascontiguousarray(f(inputs["pool_scale"]).reshape(NPG, 128).T),
        w_attn_br=f(inputs["w_attn_br"]).reshape(D, D),
        w_pool_br=f(inputs["w_pool_br"]).reshape(512, D),
        w_out=f(inputs["w_out"]).reshape(D, D),
        norm2_g=f(inputs["norm2_g"]).reshape(1, D),
        w_router=np.ascontiguousarray(np.concatenate([f(inputs["w_router_group"]).reshape(D, 4), f(inputs["w_router_expert"]).reshape(D, 16)], axis=1)),
        b_router=np.ascontiguousarray(np.concatenate([f(inputs["b_router_group"]).reshape(1, 4), f(inputs["b_router_expert"]).reshape(1, 16)], axis=1)),
        w_e_gate=f(inputs["w_e_gate"]).reshape(NE, D, DE),
        w_e_up=f(inputs["w_e_up"]).reshape(NE, D, DE),
        w_e_down=f(inputs["w_e_down"]).reshape(NE, DE, D),
        final_g=f(inputs["final_g"]).reshape(1, D),
    )
    shared.update(_consts())
    maps = []
    for b in range(n_cores):
        m = dict(shared)
        m["x"] = np.ascontiguousarray(x[b])
        maps.append(m)
    return maps


_CACHE = {}


def kernel(**inputs):
    if "nc" not in _CACHE:
        _CACHE["nc"] = build_program()[0]
    nc = _CACHE["nc"]
    maps = make_in_maps(inputs, 8)
    res = run_bass_kernel_spmd(nc, maps, core_ids=list(range(8)))
    out = np.stack([np.asarray(r["out"], dtype=np.float32) for r in res.results], axis=0)
    return out
```

```python
import contextlib
import math

import numpy as np

import concourse.bass as bass
import concourse.mybir as mybir
from concourse.bass_utils import run_bass_kernel_spmd

F32 = mybir.dt.float32
BF16 = mybir.dt.bfloat16
AF = mybir.ActivationFunctionType
ALU = mybir.AluOpType
AX = mybir.AxisListType

D = 1024
SEQ = 2048
NMETA = 16
NH = 8
HD = 64
VD = 128
NPG = 4
POOL_W = (2, 4, 8, 16)
IN_W = 5632
NE = 16
DE = 512
EPS = 1e-6
TC = 2176
MOFF = 112
ROFF = 128
O_Q, O_K, O_V, O_P, O_G = 0, 1024, 2048, 3072, 3584
LAMBDA_INIT = 0.8 - 0.6 * math.exp(-0.3 * 0)
EPOCH = 12000


class _Op:
    __slots__ = ("eng", "fn", "waits", "signal", "idx", "dma_stream", "sigcount")

    def __init__(self, eng, fn):
        self.eng = eng
        self.fn = fn
        self.waits = []
        self.signal = False
        self.idx = -1
        self.dma_stream = None
        self.sigcount = 0


class Sched:
    ENGS = ("pe", "act", "dve", "pool", "sp")

    def __init__(self):
        self.ops = {e: [] for e in self.ENGS}
        self.last_w = {}
        self.readers = {}
        self.known = {e: {} for e in self.ENGS}
        self.streams = {}
        self.pending = {e: [] for e in self.ENGS}

    def _deps(self, eng, reads, writes, is_dma=False):
        deps = {}

        def add(kind, src, n, raw):
            if kind == "e" and src == eng and not is_dma:
                if eng == "pe" or eng == "sp":
                    return
            if kind == "d":
                n = self.streams[src]
            key = (kind, src)
            if deps.get(key, -1) < n:
                deps[key] = n

        for r in reads:
            ev = self.last_w.get(r)
            if ev is not None:
                add(ev[0], ev[1], ev[2], True)
        for w in writes:
            ev = self.last_w.get(w)
            if ev is not None:
                add(ev[0], ev[1], ev[2], False)
            rd = self.readers.get(w)
            if rd:
                for key, n in rd.items():
                    add(key[0], key[1], n, False)
        for (kind, src, n) in self.pending[eng]:
            if kind == "e" and src == eng and not is_dma:
                continue
            key = (kind, src)
            if deps.get(key, -1) < n:
                deps[key] = n
        self.pending[eng] = []
        out = []
        kn = self.known[eng]
        for key, n in deps.items():
            if kn.get(key, -1) >= n:
                continue
            kn[key] = n
            out.append((key[0], key[1], n))
        return out

    def _commit(self, ev, reads, writes):
        for w in writes:
            self.last_w[w] = ev
            self.readers[w] = {}
        key = (ev[0], ev[1])
        for r in reads:
            d = self.readers.setdefault(r, {})
            if d.get(key, -1) < ev[2]:
                d[key] = ev[2]

    def _add(self, eng, fn, reads, writes, stream):
        ps_reads = [r for r in reads if isinstance(r, tuple) and r[0] == "ps"]
        if ps_reads:
            writes = list(writes) + ps_reads
        o = _Op(eng, fn)
        o.waits = self._deps(eng, reads, writes, is_dma=stream is not None)
        lst = self.ops[eng]
        o.idx = len(lst)
        lst.append(o)
        for (kind, src, n) in o.waits:
            if kind == "e":
                self.ops[src][n].signal = True
        if stream is None:
            ev = ("e", eng, o.idx)
        else:
            c = self.streams.get(stream, 0) + 1
            self.streams[stream] = c
            o.dma_stream = stream
            ev = ("d", stream, c)
        self._commit(ev, reads, writes)
        return o

    def op(self, eng, fn, reads=(), writes=()):
        return self._add(eng, fn, reads, writes, None)

    def dma(self, queue, stream, fn, reads=(), writes=()):
        return self._add(queue, fn, reads, writes, stream)

    def barrier(self):
        evs = []
        for e in self.ENGS:
            if self.ops[e]:
                evs.append(("e", e, len(self.ops[e]) - 1))
        for s, c in self.streams.items():
            if isinstance(s, tuple) and s[0] == "xwld":
                continue
            evs.append(("d", s, c))
        for e in self.ENGS:
            self.pending[e] = list(evs)
        fixed = []
        for (kind, src, n) in evs:
            if kind == "e":
                i = n
                while i >= 0 and self.ops[src][i].dma_stream is not None:
                    i -= 1
                if i < 0:
                    continue
                fixed.append(("e", src, i))
            else:
                fixed.append((kind, src, n))
        for e in self.ENGS:
            self.pending[e] = list(fixed)

    def finish(self, eng="sp"):
        self.barrier()
        o = _Op(eng, None)
        o.waits = self._deps(eng, (), ())
        for (kind, src, n) in o.waits:
            if kind == "e":
                self.ops[src][n].signal = True
        o.idx = len(self.ops[eng])
        self.ops[eng].append(o)

    def emit(self, nc):
        for e, lst in self.ops.items():
            c = 0
            for o in lst:
                if o.signal:
                    c += 1
                o.sigcount = c
        nsig = {e: (lst[-1].sigcount if lst else 0) for e, lst in self.ops.items()}
        with contextlib.ExitStack() as st:
            esems = {}
            for e in self.ENGS:
                n_ep = nsig[e] // EPOCH + 1
                esems[e] = [st.enter_context(nc.semaphore(f"s_{e}_{k}")) for k in range(n_ep)]
            dsems = {s: st.enter_context(nc.semaphore("d_" + "_".join(str(x) for x in (s if isinstance(s, tuple) else (s,))))) for s in self.streams}
            block = st.enter_context(nc.Block())

            def run(engname):
                def body(eng):
                    for o in self.ops[engname]:
                        for (kind, src, n) in o.waits:
                            if kind == "e":
                                sc = self.ops[src][n].sigcount
                                ep, v = (sc - 1) // EPOCH, (sc - 1) % EPOCH + 1
                                eng.wait_ge(esems[src][ep], v)
                            else:
                                eng.wait_ge(dsems[src], 16 * n)
                        if o.fn is None:
                            continue
                        ins = o.fn(eng)
                        if o.dma_stream is not None:
                            ins.then_inc(dsems[o.dma_stream], 16)
                        elif o.signal:
                            ep = (o.sigcount - 1) // EPOCH
                            ins.then_inc(esems[engname][ep], 1)
                return body

            block.tensor(run("pe"))
            block.scalar(run("act"))
            block.vector(run("dve"))
            block.gpsimd(run("pool"))
            block.sync(run("sp"))
        return nsig


def build_program(dbg=(), stop_after=None):
    nc = bass.Bass("TRN2", target_bir_lowering=False)
    dbg = set(dbg)

    def din(name, shape, dt=F32):
        return nc.dram_tensor(name, list(shape), dt, kind="ExternalInput").ap()

    x_d = din("x", [SEQ, D])
    meta_d = din("meta", [NMETA, D])
    g1_d = din("norm1_g", [1, D])
    w_in_d = din("w_in", [D, IN_W])
    bgate_d = din("b_gate_t", [128, 16])
    lq1_d = din("lambda_q1", [1, HD])
    lk1_d = din("lambda_k1", [1, HD])
    lq2_d = din("lambda_q2", [1, HD])
    lk2_d = din("lambda_k2", [1, HD])
    subg_d = din("subln_g_t", [128, 1])
    poolw_d = din("pool_w", [NPG, 128, 128])
    pscale_d = din("pool_scale_t", [128, NPG])
    wattn_d = din("w_attn_br", [D, D])
    wpool_d = din("w_pool_br", [512, D])
    wout_d = din("w_out", [D, D])
    g2_d = din("norm2_g", [1, D])
    wr_d = din("w_router", [D, 20])
    br_d = din("b_router", [1, 20])
    weg_d = din("w_e_gate", [NE, D, DE])
    weu_d = din("w_e_up", [NE, D, DE])
    wed_d = din("w_e_down", [NE, DE, D])
    gf_d = din("final_g", [1, D])
    cos_d = din("cos_t", [128, TC])
    sin_d = din("sin_t", [128, TC])
    ident_d = din("c_ident", [128, 128])
    perm_d = din("c_perm", [128, 128])
    tri_d = din("c_tri", [128, 256])
    ones_d = din("c_ones", [128, 128])
    ones16_d = din("c_ones16", [128, 128])
    negm_d = din("c_negm", [128, 128])
    out_d = nc.dram_tensor("out", [SEQ, D], F32, kind="ExternalOutput").ap()
    dbg_d = {}

    def dbg_out(name, shape):
        dbg_d[name] = nc.dram_tensor("dbg_" + name, list(shape), F32, kind="ExternalOutput").ap()
        return dbg_d[name]

    S = Sched()
    phases = ["A", "B0", "B1", "D", "E1", "E2", "F", "G"]
    last_phase = stop_after or "G"

    def active(ph):
        return phases.index(ph) <= phases.index(last_phase)

    with contextlib.ExitStack() as st:
        def sb(name, shape, dt):
            return st.enter_context(nc.sbuf_tensor(name, list(shape), dt))

        R1 = sb("R1", [128, 34816 // 4], F32)
        R42 = sb("R42", [128, 67584 // 4], F32)
        R35 = sb("R35", [128, 51200 // 4], F32)
        RT_BYTES = 50 * 1024
        RT = sb("RT", [128, RT_BYTES // 4], F32)

        def view(region, off_bytes, shape, dt):
            n = 1
            for s_ in shape:
                n *= s_
            if dt == F32:
                assert off_bytes % 4 == 0
                a = region[:, off_bytes // 4: off_bytes // 4 + n]
            else:
                assert off_bytes % 4 == 0 and (n * 2) % 4 == 0
                a = region[:, off_bytes // 4: off_bytes // 4 + (n * 2) // 4].bitcast(BF16)
            if len(shape) == 2:
                a = a.rearrange("p (a b) -> p a b", a=shape[0])
            elif len(shape) == 3:
                a = a.rearrange("p (a b c) -> p a b c", a=shape[0], b=shape[1])
            return a

        class RTAlloc:
            def __init__(self):
                self.off = 0

            def reset(self):
                self.off = 0

            def get(self, shape, dt):
                n = 1
                for s_ in shape:
                    n *= s_
                nb = n * (4 if dt == F32 else 2)
                nb = (nb + 31) // 32 * 32
                v = view(RT, self.off, shape, dt)
                self.off += nb
                assert self.off <= RT_BYTES, ("RT overflow", self.off)
                return v

        rt = RTAlloc()
        _tail = RT_BYTES - (1280 + 4096 + 1024)
        rl = view(RT, _tail, [16, 20], F32)
        rw = view(RT, _tail + 1280, [16, 64], F32)
        comb = view(RT, _tail + 1280 + 4096, [16, 16], F32)
        wg0x = view(RT, 35840, [8, DE], BF16)

        hnT = view(R1, 0, [8, TC], BF16)
        hn2T = view(R1, 0, [8, SEQ], BF16)
        Vt = view(R42, 0, [17, D], BF16)
        QT = view(R42, 34816, [8, SEQ], BF16)
        h1 = view(R42, 0, [16, D], F32)
        KT = view(R35, 0, [8, TC], BF16)
        mixT = view(R35, 0, [8, SEQ], BF16)
        PT = view(R35, 34816, [NPG, SEQ], BF16)
        WB = 24576
        wgt = [view(R35, k * WB, [8, DE], BF16) for k in range(2)]
        wup = [view(R35, k * WB + 8192, [8, DE], BF16) for k in range(2)]
        wdn = [view(R35, k * WB + 16384, [4, D], BF16) for k in range(2)]

        ident = sb("ident", [128, 128], BF16)
        permm = sb("permm", [128, 128], BF16)
        tri = sb("tri", [128, 2, 128], BF16)
        ones = sb("ones", [128, 128], BF16)
        ones16 = sb("ones16", [128, 128], BF16)
        negm = sb("negm", [128, 128], BF16)
        poolw = sb("poolw", [128, NPG, 128], BF16)
        bgate = sb("bgate", [128, 16], F32)
        pscale = sb("pscale", [128, NPG], F32)
        subg = sb("subg", [128, 1], F32)
        lamt = sb("lamt", [128, 4 * HD], F32)
        lams = sb("lams", [128, 8], F32)
        stats = sb("stats", [128, 17, 2], F32)
        stats2 = sb("stats2", [128, 16, 2], F32)
        stats3 = sb("stats3", [128, 16, 2], F32)
        wr_sb = sb("wr_sb", [128, 8, 20], BF16)
        br_sb = sb("br_sb", [128, 20], F32)

        _pb = [st.enter_context(nc.psum_tensor(f"pb{i}", [128, 512], F32)) for i in range(4)]
        pS = [st.enter_context(nc.psum_tensor(f"pS{i}", [128, 1024], F32)) for i in range(2)]
        pbanks = [t[:, :] for t in _pb] + [pS[0][:, 0:512], pS[0][:, 512:1024], pS[1][:, 0:512], pS[1][:, 512:1024]]

        def pbf(i):
            return pbanks[i].bitcast(BF16)

        cres = lambda t: ("c", t.name)
        for t, d_ in ((ident, ident_d), (permm, perm_d), (ones, ones_d), (ones16, ones16_d), (negm, negm_d)):
            S.dma("pool", "constP", lambda e, t=t, d_=d_: e.dma_start(out=t[:, :], in_=d_), writes=[cres(t)])
        S.dma("pool", "constP", lambda e: e.dma_start(out=tri[:, :, :], in_=tri_d.rearrange("p (a b) -> p a b", a=2)), writes=[cres(tri)])
        S.dma("pool", "constP", lambda e: e.dma_start(out=poolw[:, :, :], in_=poolw_d.rearrange("g c d -> c g d")), writes=[cres(poolw)])
        S.dma("pool", "constP", lambda e: e.dma_start(out=wr_sb[:, :, :], in_=wr_d.rearrange("(c p) n -> p c n", p=128)), writes=[cres(wr_sb)])
        for t, d_ in ((bgate, bgate_d), (pscale, pscale_d), (subg, subg_d)):
            S.dma("sp", "constS", lambda e, t=t, d_=d_: e.dma_start(out=t[:, :], in_=d_), writes=[cres(t)])
        S.dma("sp", "constS", lambda e: e.dma_start(out=br_sb[:, :], in_=br_d.to_broadcast([128, 20])), writes=[cres(br_sb)])
        for i, d_ in enumerate((lq1_d, lk1_d, lq2_d, lk2_d)):
            S.dma("sp", "constS", lambda e, i=i, d_=d_: e.dma_start(out=lamt[:, i * HD:(i + 1) * HD], in_=d_.to_broadcast([128, HD])), writes=[("lamt", i)])

        S.op("dve", lambda e: e.tensor_tensor(out=lamt[:, 0:HD], in0=lamt[:, 0:HD], in1=lamt[:, HD:2 * HD], op=ALU.mult),
             reads=[("lamt", 0), ("lamt", 1)], writes=[("lamt", 0)])
        S.op("dve", lambda e: e.tensor_tensor(out=lamt[:, 2 * HD:3 * HD], in0=lamt[:, 2 * HD:3 * HD], in1=lamt[:, 3 * HD:4 * HD], op=ALU.mult),
             reads=[("lamt", 2), ("lamt", 3)], writes=[("lamt", 2)])
        S.op("dve", lambda e: e.reduce_sum(out=lams[:, 0:1], in_=lamt[:, 0:HD], axis=AX.X), reads=[("lamt", 0)], writes=[("lams", 0)])
        S.op("dve", lambda e: e.reduce_sum(out=lams[:, 1:2], in_=lamt[:, 2 * HD:3 * HD], axis=AX.X), reads=[("lamt", 2)], writes=[("lams", 1)])
        S.op("act", lambda e: e.activation(out=lams[:, 2:4], in_=lams[:, 0:2], func=AF.Exp), reads=[("lams", 0), ("lams", 1)], writes=[("lams", 2)])
        S.op("dve", lambda e: e.scalar_tensor_tensor(out=lams[:, 4:5], in0=lams[:, 3:4], scalar=-LAMBDA_INIT, in1=lams[:, 2:3],
                                                     op0=ALU.add, op1=ALU.subtract), reads=[("lams", 2)], writes=[("lams", 4)])
        S.op("dve", lambda e: e.tensor_scalar(out=lams[:, 5:6], in0=subg[:, 0:1], scalar1=1.0 - LAMBDA_INIT, scalar2=None, op0=ALU.mult),
             reads=[cres(subg)], writes=[("lams", 5)])
        neglam = lams[:, 4:5]
        gsub = lams[:, 5:6]

        def PS(i):
            return ("ps", i)

        rt.reset()
        wblk = [rt.get([8, 512], BF16) for _ in range(2)]
        S.dma("pool", ("wld", 0), lambda e: e.dma_start(out=wblk[0], in_=w_in_d[:, O_P:O_P + 512].rearrange("(c p) n -> p c n", p=128)), writes=[("wblk", 0)])
        S.dma("pool", ("wld", 1), lambda e: e.dma_start(out=wblk[1], in_=w_in_d[:, O_Q:O_Q + 512].rearrange("(c p) n -> p c n", p=128)), writes=[("wblk", 1)])
        xw = [view(R42, n * 8192, [8, 512], BF16) for n in range(3)]
        for n, c0_ in enumerate((O_Q + 512, O_K, O_K + 512)):
            S.dma("pool", ("xwld", n), lambda e, n=n, c0_=c0_: e.dma_start(out=xw[n], in_=w_in_d[:, c0_:c0_ + 512].rearrange("(c p) n -> p c n", p=128)),
                  writes=[("xw", n)])
        g1bc = rt.get([D], F32)
        NBA = 4
        xt = [rt.get([D], F32) for _ in range(NBA)]
        junk = rt.get([D], BF16)
        xn = [rt.get([D], BF16) for _ in range(NBA)]
        S.dma("sp", "miscS", lambda e: e.dma_start(out=g1bc, in_=g1_d.to_broadcast([128, D])), writes=["g1bc"])
        S.op("pool", lambda e: e.memset(xt[0], 0.0), writes=[("xt", 0)])
        def a_s1(i):
            b = i % NBA
            if i == 0:
                S.dma("sp", ("xt", 0), lambda e: e.dma_start(out=xt[0][MOFF:128, :], in_=meta_d), writes=[("xt", 0)], reads=[])
            else:
                S.dma("sp", ("xt", b), lambda e: e.dma_start(out=xt[b], in_=x_d[(i - 1) * 128:i * 128, :]), writes=[("xt", b)])
            S.op("act", lambda e: e.activation(out=junk, in_=xt[b], func=AF.Square, accum_out=stats[:, i, 0:1]),
                 reads=[("xt", b)], writes=["junk", ("st", i)])
            S.op("act", lambda e: e.activation(out=stats[:, i, 1:2], in_=stats[:, i, 0:1], func=AF.Sqrt, scale=1.0 / D, bias=EPS),
                 reads=[("st", i)], writes=[("st1", i)])
            S.op("dve", lambda e: e.reciprocal(out=stats[:, i, 1:2], in_=stats[:, i, 1:2]), reads=[("st1", i)], writes=[("st1", i)])
            S.op("dve", lambda e: e.scalar_tensor_tensor(out=xn[b], in0=xt[b], scalar=stats[:, i, 1:2], in1=g1bc, op0=ALU.mult, op1=ALU.mult),
                 reads=[("xt", b), ("st1", i), "g1bc"], writes=[("xn", b)])

        def a_s2(i):
            b = i % NBA
            pbk = i % 4
            for c in range(8):
                S.op("pe", lambda e, c=c: e.transpose(out=pbf(pbk)[:, c * 128:(c + 1) * 128], in_=xn[b][:, c * 128:(c + 1) * 128], identity=ident[:, :]),
                     reads=[("xn", b), cres(ident)], writes=[PS(pbk)])
            S.op("act", lambda e: e.copy(out=hnT[:, :, i * 128:(i + 1) * 128], in_=pbf(pbk).rearrange("p (c n) -> p c n", c=8)),
                 reads=[PS(pbk)], writes=[("hnT", i)])

        for it_ in range(17 + 2):
            if it_ < 17:
                a_s1(it_)
            if 0 <= it_ - 2 < 17:
                a_s2(it_ - 2)
        HNT_ALL = [("hnT", i) for i in range(17)]

        if "hnT" in dbg:
            o = dbg_out("hnT", [128, 8 * TC])
            S.dma("pool", "dbgP", lambda e, o=o: e.dma_start(out=o.rearrange("p (c t) -> p c t", c=8), in_=hnT), reads=HNT_ALL, writes=["dbg_hnT"])

        def load_wblock(dst, src2d, c0, ncols, res, kch=8):
            S.dma("pool", ("wld", res[1]), lambda e: e.dma_start(out=dst, in_=src2d[:, c0:c0 + ncols].rearrange("(c p) n -> p c n", p=128)),
                  writes=[res])

        if active("B0"):
            S.barrier()
            rt.reset()
            wblk = [rt.get([8, 512], BF16) for _ in range(2)]
            u32 = [rt.get([TC], F32) for _ in range(2)]
            tmpA = rt.get([TC], F32)
            tmpB = rt.get([TC], F32)
            psrot = [0]

            def nextbank(lo, hi):
                b_ = lo + psrot[0] % (hi - lo)
                psrot[0] += 1
                return b_

            def b0_proj(g):
                ub = u32[g % 2]
                ures = ("u32", g % 2)
                for jt in range(5):
                    c0, n = (0, 128) if jt == 0 else (ROFF + (jt - 1) * 512, 512)
                    bk = nextbank(0, 4)
                    for c in range(8):
                        S.op("pe", lambda e, c=c, g=g, c0=c0, n=n, bk=bk: e.matmul(pbanks[bk][:, 0:n], lhsT=wblk[0][:, c, g * 128:(g + 1) * 128],
                                                                                 rhs=hnT[:, c, c0:c0 + n], start=(c == 0), stop=(c == 7)),
                             reads=[("wblk", 0)] + HNT_ALL, writes=[PS(bk)])
                    S.op("act", lambda e, ub=ub, c0=c0, n=n, bk=bk: e.copy(out=ub[:, c0:c0 + n], in_=pbanks[bk][:, 0:n]),
                         reads=[PS(bk)], writes=[ures])

            def b0_pool(g):
                ub = u32[g % 2]
                ures = ("u32", g % 2)
                w = POOL_W[g]
                L = TC
                cur, cur_res = ub, ures
                step = 1
                bufs = [(tmpA, "tmpA"), (tmpB, "tmpB")]
                k = 0
                while step < w:
                    dst, dres = bufs[k % 2]
                    lo = 2 * step - 1
                    S.op("dve", lambda e, dst=dst, cur=cur, lo=lo, step=step: e.tensor_tensor(out=dst[:, lo:L], in0=cur[:, lo:L], in1=cur[:, lo - step:L - step], op=ALU.add),
                         reads=[cur_res], writes=[dres])
                    cur, cur_res = dst, dres
                    step *= 2
                    k += 1
                ydst, yres = bufs[k % 2]
                ybf = ydst[:, 0:SEQ // 2].bitcast(BF16)
                S.op("dve", lambda e, ybf=ybf, cur=cur, ub=ub, w=w: e.scalar_tensor_tensor(out=ybf, in0=cur[:, ROFF:TC], scalar=1.0 / w, in1=ub[:, ROFF:TC],
                                                                                            op0=ALU.mult, op1=ALU.subtract),
                     reads=[cur_res, ures], writes=[yres])
                for jt in range(4):
                    bk = nextbank(0, 4)
                    S.op("pe", lambda e, g=g, jt=jt, bk=bk, ybf=ybf: e.matmul(pbanks[bk][:, :], lhsT=poolw[:, g, :], rhs=ybf[:, jt * 512:(jt + 1) * 512], start=True, stop=True),
                         reads=[yres, cres(poolw)], writes=[PS(bk)])
                    S.op("act", lambda e, g=g, jt=jt, bk=bk: e.activation(out=PT[:, g, jt * 512:(jt + 1) * 512], in_=pbanks[bk][:, :], func=AF.Identity, scale=pscale[:, g:g + 1]),
                         reads=[PS(bk), cres(pscale)], writes=[("PT", g)])

            for it_ in range(NPG + 1):
                if it_ < NPG:
                    b0_proj(it_)
                if it_ >= 1:
                    b0_pool(it_ - 1)
            if "PT" in dbg:
                o = dbg_out("PT", [128, NPG * SEQ])
                S.dma("pool", "dbgP", lambda e, o=o: e.dma_start(out=o.rearrange("p (c t) -> p c t", c=NPG), in_=PT), reads=[("PT", g) for g in range(NPG)], writes=["dbg_PT"])

        if active("B1"):
            S.barrier()
            rt.reset()
            wblk = [rt.get([8, 512], BF16) for _ in range(2)]
            cosT = rt.get([TC], F32)
            sinT = rt.get([TC], F32)
            zb = [rt.get([512], BF16) for _ in range(3)]
            t1 = [rt.get([512], F32) for _ in range(3)]
            t2 = [rt.get([512], F32) for _ in range(3)]
            ZB_ = (0, 1, 6)
            RB_ = (2, 3, 7)
            S.dma("sp", "miscS", lambda e: e.dma_start(out=cosT, in_=cos_d), writes=["cosT"])
            S.dma("sp", "miscS", lambda e: e.dma_start(out=sinT, in_=sin_d), writes=["sinT"])
            blocks = [("q", 0), ("q", 1), ("k", 0), ("k", 1), ("v", 0), ("v", 1)]
            col_of = {"q": O_Q, "k": O_K, "v": O_V}
            items = []

            def stage1(it):
                kind, h, c0, n, wb, nchunk, slot = it
                wbuf, wres = wb
                sl = slot % 3
                bk = ZB_[sl]
                for c in range(8):
                    S.op("pe", lambda e, c=c: e.matmul(pbanks[bk][:, 0:n], lhsT=wbuf[:, c, nchunk * 128:(nchunk + 1) * 128], rhs=hnT[:, c, c0:c0 + n],
                                                       start=(c == 0), stop=(c == 7)),
                         reads=[wres] + HNT_ALL, writes=[PS(bk)])
                S.op("act", lambda e: e.copy(out=zb[sl][:, 0:n], in_=pbanks[bk][:, 0:n]), reads=[PS(bk)], writes=[("zb", sl)])

            def stage2(it):
                kind, h, c0, n, wb, nchunk, slot = it
                sl = slot % 3
                bk = ZB_[sl]
                rb = RB_[sl]
                S.op("pe", lambda e: e.matmul(pbanks[rb][:, 0:n], lhsT=permm[:, :], rhs=zb[sl][:, 0:n], start=True, stop=True),
                     reads=[("zb", sl), cres(permm)], writes=[PS(rb)])
                S.op("dve", lambda e: e.tensor_tensor(out=t1[sl][:, 0:n], in0=pbanks[bk][:, 0:n], in1=cosT[:, c0:c0 + n], op=ALU.mult),
                     reads=[PS(bk), "cosT"], writes=[("t1", sl)])
                S.op("dve", lambda e: e.tensor_tensor(out=t2[sl][:, 0:n], in0=pbanks[rb][:, 0:n], in1=sinT[:, c0:c0 + n], op=ALU.mult),
                     reads=[PS(rb), "sinT"], writes=[("t2", sl)])
                if kind == "q":
                    dst = QT[:, h, c0 - ROFF:c0 - ROFF + n]
                    dres = ("QT", h, (c0 - ROFF) // 512)
                else:
                    dst = KT[:, h, c0:c0 + n]
                    dres = ("KT", h)
                S.op("pool", lambda e: e.tensor_tensor(out=dst, in0=t1[sl][:, 0:n], in1=t2[sl][:, 0:n], op=ALU.add),
                     reads=[("t1", sl), ("t2", sl)], writes=[dres])

            slot = 0
            prev = None
            wplan = [(wblk[1], ("wblk", 1)), (xw[0], ("xw", 0)), (xw[1], ("xw", 1)), (xw[2], ("xw", 2)), (wblk[1], ("wblk", 1)), (wblk[0], ("wblk", 0))]
            load_wblock(wblk[0], w_in_d, O_V + 512, 512, ("wblk", 0))
            for bi, (kind, half) in enumerate(blocks):
                wb = wplan[bi]
                if bi == 1:
                    load_wblock(wblk[1], w_in_d, O_V, 512, ("wblk", 1))
                if kind in ("q", "k"):
                    for nchunk in range(4):
                        h = half * 4 + nchunk
                        tiles = ([(0, 128)] if kind == "k" else []) + [(ROFF + jt * 512, 512) for jt in range(4)]
                        for (c0, n) in tiles:
                            it = (kind, h, c0, n, wb, nchunk, slot)
                            slot += 1
                            stage1(it)
                            if prev is not None:
                                stage2(prev)
                            prev = it
                else:
                    if prev is not None:
                        stage2(prev)
                        prev = None
                    for i in range(17):
                        for sub in range(1):
                            bk = 4 + (i % 2)
                            for c in range(8):
                                S.op("pe", lambda e, c=c, i=i, bk=bk, wb=wb: e.matmul(pbanks[bk][:, :], lhsT=hnT[:, c, i * 128:(i + 1) * 128], rhs=wb[0][:, c, :],
                                                                                     start=(c == 0), stop=(c == 7)),
                                     reads=[wb[1], ("hnT", i)], writes=[PS(bk)])
                            S.op("act", lambda e, i=i, bk=bk, half=half: e.copy(out=Vt[:, i, half * 512:(half + 1) * 512], in_=pbanks[bk][:, :]),
                                 reads=[PS(bk)], writes=[("V", i), ("xw", 0), ("xw", 1), ("xw", 2)])
            if prev is not None:
                stage2(prev)
                prev = None
            for nm, tns, shp, res in (("QT", QT, [128, 8 * SEQ], [("QT", h, j) for h in range(8) for j in range(4)]),
                                      ("KT", KT, [128, 8 * TC], [("KT", h) for h in range(8)]),
                                      ("V", Vt, [128, 17 * D], [("V", i) for i in range(17)])):
                if nm in dbg:
                    o = dbg_out(nm, shp)
                    S.dma("pool", "dbgP", lambda e, o=o, tns=tns: e.dma_start(out=o.rearrange("p (c t) -> p c t", c=tns.shape[1]), in_=tns), reads=res, writes=["dbg_" + nm])

        def load_e1(dc, bufs):
            b = dc % 2
            wa_, wp_, wg0_, wg1_ = bufs["wa"], bufs["wp"], bufs["wg0"], bufs["wg1"]
            S.dma("pool", ("e1w", b), lambda e: e.dma_start(out=wa_[b], in_=wattn_d[:, dc * 128:(dc + 1) * 128].rearrange("(c p) n -> p c n", p=128)), writes=[("wa", b)])
            S.dma("pool", ("e1w", b), lambda e: e.dma_start(out=wp_[b], in_=wpool_d[:, dc * 128:(dc + 1) * 128].rearrange("(c p) n -> p c n", p=128)), writes=[("wp", b)])
            S.dma("pool", ("e1w", b), lambda e: e.dma_start(out=wg0_[b], in_=w_in_d[:, O_G + dc * 128:O_G + (dc + 1) * 128].rearrange("(c p) n -> p c n", p=128)), writes=[("wg0", b)])
            S.dma("pool", ("e1w", b), lambda e: e.dma_start(out=wg1_[b], in_=w_in_d[:, O_G + D + dc * 128:O_G + D + (dc + 1) * 128].rearrange("(c p) n -> p c n", p=128)), writes=[("wg1", b)])

        if active("D"):
            S.barrier()
            rt.reset()
            wout_sb = rt.get([8, D], BF16)
            S.dma("pool", "miscP", lambda e: e.dma_start(out=wout_sb, in_=wout_d.rearrange("(c p) n -> p c n", p=128)), writes=["wout"])
            e1bufs = dict(wa=[rt.get([8, 128], BF16) for _ in range(2)], wp=[rt.get([4, 128], BF16) for _ in range(2)],
                          wg0=[rt.get([8, 128], BF16) for _ in range(2)], wg1=[rt.get([8, 128], BF16) for _ in range(2)])
            if active("E1"):
                load_e1(0, e1bufs)
                load_e1(1, e1bufs)
            pTb = [rt.get([2, 512], BF16) for _ in range(3)]
            r2 = rt.get([2, 512], F32)
            o_t = [rt.get([512], F32) for _ in range(2)]
            a_t = rt.get([512], F32)
            sq_t = rt.get([512], BF16)
            ln_t = rt.get([512], F32)
            ACC_O = (0, 2)
            ACC_S = (1, 3)
            SCALE = 1.0 / math.sqrt(HD)
            cnts = {"s": 0, "pt": 0}

            items = []
            prev_hj = None
            for h in range(NH):
                for j in range(4):
                    kts = [(0, 0, 0, False)]
                    for i in range(4 * j):
                        kts.append((1 + i, ROFF + i * 128, 0, False))
                    for r in range(4):
                        i = 4 * j + r
                        kts.append((1 + i, ROFF + i * 128, 128 * r, True))
                    for idx, kt in enumerate(kts):
                        items.append(("kt", h, j, kt, idx == 0, idx == len(kts) - 1))
                        if idx == min(5, len(kts) - 2) and prev_hj is not None:
                            items.append(("norm",) + prev_hj)
                    prev_hj = (h, j)
            final_norm = ("norm",) + prev_hj

            def stage_s(it):
                p = cnts["s"] % 2
                cnts["s"] += 1
                if it[0] == "norm":
                    _, h, j = it
                    S.op("pe", lambda e: e.matmul(pS[p][:, 0:512], lhsT=ones[:, :], rhs=sq_t, start=True, stop=True),
                         reads=["sq_t", cres(ones)], writes=[PS(4 + 2 * p)])
                    S.op("act", lambda e: e.activation(out=ln_t, in_=pS[p][:, 0:512], func=AF.Ln, scale=1.0 / VD, bias=EPS),
                         reads=[PS(4 + 2 * p)], writes=["ln_t"])
                    S.op("act", lambda e: e.activation(out=ln_t, in_=ln_t, func=AF.Exp, scale=-0.5), reads=["ln_t"], writes=["ln_t"])
                    return None
                _, h, j, (vt_i, kc0, q0, diag), first, last = it
                par = h % 2
                pb_i = cnts["pt"] % 3
                cnts["pt"] += 1
                for c in range(2):
                    S.op("pe", lambda e, c=c: e.matmul(pS[p][:, c * 512 + q0:(c + 1) * 512], lhsT=KT[c * 64:(c + 1) * 64, h, kc0:kc0 + 128],
                                                       rhs=QT[c * 64:(c + 1) * 64, h, j * 512 + q0:(j + 1) * 512], start=True, stop=True),
                         reads=[("KT", h), ("QT", h, j)], writes=[PS(4 + 2 * p + c)])
                S.op("act", lambda e: e.activation(out=pTb[pb_i][:, :, q0:512], in_=pS[p][:, :].rearrange("p (c n) -> p c n", c=2)[:, :, q0:512],
                                                   func=AF.Exp, scale=SCALE),
                     reads=[PS(4 + 2 * p), PS(4 + 2 * p + 1)], writes=[("pT", pb_i)])
                if diag:
                    S.op("dve", lambda e: e.tensor_tensor(out=pTb[pb_i][:, :, q0:q0 + 128], in0=pTb[pb_i][:, :, q0:q0 + 128], in1=tri[:, :, :], op=ALU.mult),
                         reads=[("pT", pb_i), cres(tri)], writes=[("pT", pb_i)])
                return pb_i

            def stage_av(it, pb_i):
                if it[0] == "norm":
                    _, h, j = it
                    S.op("dve", lambda e: e.scalar_tensor_tensor(out=QT[:, h, j * 512:(j + 1) * 512], in0=a_t, scalar=gsub, in1=ln_t, op0=ALU.mult, op1=ALU.mult),
                         reads=["a_t", "ln_t", ("lams", 5)], writes=[("QT", h, j)])
                    return
                _, h, j, (vt_i, kc0, q0, diag), first, last = it
                on = ones16 if vt_i == 0 else ones
                for c in range(2):
                    S.op("pe", lambda e, c=c: e.matmul(pbanks[ACC_O[c]][:, q0:512], lhsT=Vt[:, vt_i, h * 128:(h + 1) * 128], rhs=pTb[pb_i][:, c, q0:512],
                                                       start=first, stop=last),
                         reads=[("V", vt_i), ("pT", pb_i)], writes=[PS(ACC_O[c])])
                    S.op("pe", lambda e, c=c: e.matmul(pbanks[ACC_S[c]][:, q0:512], lhsT=on[:, :], rhs=pTb[pb_i][:, c, q0:512],
                                                       start=first, stop=last),
                         reads=[cres(on), ("pT", pb_i)], writes=[PS(ACC_S[c])])
                if last:
                    for c in range(2):
                        S.op("act", lambda e, c=c: e.activation(out=r2[:, c, :], in_=pbanks[ACC_S[c]], func=AF.Ln), reads=[PS(ACC_S[c])], writes=["r2"])
                        S.op("dve", lambda e, c=c: e.tensor_copy(out=o_t[c], in_=pbanks[ACC_O[c]]), reads=[PS(ACC_O[c])], writes=[("o_t", c)])
                    S.op("act", lambda e: e.activation(out=r2[:, :, :], in_=r2[:, :, :], func=AF.Exp, scale=-1.0), reads=["r2"], writes=["r2"])
                    for c in range(2):
                        S.op("dve", lambda e, c=c: e.tensor_tensor(out=o_t[c], in0=o_t[c], in1=r2[:, c, :], op=ALU.mult),
                             reads=[("o_t", c), "r2"], writes=[("o_t", c)])
                    S.op("dve", lambda e: e.scalar_tensor_tensor(out=a_t, in0=o_t[1], scalar=neglam, in1=o_t[0], op0=ALU.mult, op1=ALU.add),
                         reads=[("o_t", 0), ("o_t", 1), ("lams", 4)], writes=["a_t"])
                    S.op("pool", lambda e: e.tensor_tensor(out=sq_t, in0=a_t, in1=a_t, op=ALU.mult), reads=["a_t"], writes=["sq_t"])

            prev_it, prev_pb = None, None
            for it in items + [None]:
                pb_new = stage_s(it) if it is not None else None
                if prev_it is not None:
                    stage_av(prev_it, prev_pb)
                prev_it, prev_pb = it, pb_new
            stage_s(final_norm)
            stage_av(final_norm, None)
            if "AT" in dbg:
                o = dbg_out("AT", [128, 8 * SEQ])
                S.dma("pool", "dbgP", lambda e, o=o: e.dma_start(out=o.rearrange("p (c t) -> p c t", c=8), in_=QT), reads=[("QT", h, j) for h in range(8) for j in range(4)], writes=["dbg_AT"])

        if active("E1"):
            S.barrier()
            rt.reset()
            wout_sb = rt.get([8, D], BF16)
            wa = [rt.get([8, 128], BF16) for _ in range(2)]
            wp = [rt.get([4, 128], BF16) for _ in range(2)]
            wg0 = [rt.get([8, 128], BF16) for _ in range(2)]
            wg1 = [rt.get([8, 128], BF16) for _ in range(2)]
            gt = [[rt.get([512], F32) for _ in range(2)] for _ in range(2)]
            mt = [[rt.get([512], F32) for _ in range(2)] for _ in range(2)]
            ATR = [("QT", h, j) for h in range(8) for j in range(4)]

            def e1_tile(dc, j, b, pr):
                cs = slice(ROFF + j * 512, ROFF + (j + 1) * 512)
                qs = slice(j * 512, (j + 1) * 512)
                bk_g0, bk_a, bk_g1, bk_p = (0 + pr, 2 + pr, 4 + pr, 6 + pr)
                for c in range(8):
                    S.op("pe", lambda e, c=c: e.matmul(pbanks[bk_g0][:, :], lhsT=wg0[b][:, c, :], rhs=hnT[:, c, cs], start=(c == 0), stop=(c == 7)),
                         reads=[("wg0", b)] + HNT_ALL, writes=[PS(bk_g0)])
                S.op("act", lambda e: e.activation(out=gt[0][pr], in_=pbanks[bk_g0][:, :], func=AF.Sigmoid, bias=bgate[:, dc:dc + 1]),
                     reads=[PS(bk_g0), cres(bgate)], writes=[("gt", 0, pr)])
                for c in range(8):
                    S.op("pe", lambda e, c=c: e.matmul(pbanks[bk_a][:, :], lhsT=wa[b][:, c, :], rhs=QT[:, c, qs], start=(c == 0), stop=(c == 7)),
                         reads=[("wa", b)] + ATR, writes=[PS(bk_a)])
                S.op("dve", lambda e: e.tensor_tensor(out=mt[0][pr], in0=pbanks[bk_a][:, :], in1=gt[0][pr], op=ALU.mult),
                     reads=[PS(bk_a), ("gt", 0, pr)], writes=[("mt", 0, pr)])
                for c in range(8):
                    S.op("pe", lambda e, c=c: e.matmul(pbanks[bk_g1][:, :], lhsT=wg1[b][:, c, :], rhs=hnT[:, c, cs], start=(c == 0), stop=(c == 7)),
                         reads=[("wg1", b)] + HNT_ALL, writes=[PS(bk_g1)])
                S.op("act", lambda e: e.activation(out=gt[1][pr], in_=pbanks[bk_g1][:, :], func=AF.Sigmoid, bias=bgate[:, 8 + dc:9 + dc]),
                     reads=[PS(bk_g1), cres(bgate)], writes=[("gt", 1, pr)])
                for c in range(4):
                    S.op("pe", lambda e, c=c: e.matmul(pbanks[bk_p][:, :], lhsT=wp[b][:, c, :], rhs=PT[:, c, qs], start=(c == 0), stop=(c == 3)),
                         reads=[("wp", b)] + [("PT", g) for g in range(NPG)], writes=[PS(bk_p)])
                S.op("dve", lambda e: e.tensor_tensor(out=mt[1][pr], in0=pbanks[bk_p][:, :], in1=gt[1][pr], op=ALU.mult),
                     reads=[PS(bk_p), ("gt", 1, pr)], writes=[("mt", 1, pr)])
                S.op("pool", lambda e: e.tensor_tensor(out=mixT[:, dc, qs], in0=mt[0][pr], in1=mt[1][pr], op=ALU.add),
                     reads=[("mt", 0, pr), ("mt", 1, pr)], writes=[("mixT", j)])

            e1bufs = dict(wa=wa, wp=wp, wg0=wg0, wg1=wg1)
            if not active("D"):
                load_e1(0, e1bufs)
                load_e1(1, e1bufs)
            cnt = 0
            for dc in range(8):
                b = dc % 2
                if dc >= 1 and dc + 1 < 8:
                    load_e1(dc + 1, e1bufs)
                for j in range(4):
                    e1_tile(dc, j, b, cnt % 2)
                    cnt += 1
            if "mixT" in dbg:
                o = dbg_out("mixT", [128, 8 * SEQ])
                S.dma("pool", "dbgP", lambda e, o=o: e.dma_start(out=o.rearrange("p (c t) -> p c t", c=8), in_=mixT), reads=[("mixT", j) for j in range(4)], writes=["dbg_mixT"])

        if active("E2"):
            S.barrier()
            rt.reset()
            wout_sb = rt.get([8, D], BF16)
            x2 = [rt.get([D], F32) for _ in range(2)]
            g2bc = rt.get([D], F32)
            junk2 = rt.get([D], BF16)
            hn2 = [rt.get([D], BF16) for _ in range(2)]
            S.dma("sp", "miscS", lambda e: e.dma_start(out=g2bc, in_=g2_d.to_broadcast([128, D])), writes=["g2bc"])
            if active("F"):
                S.dma("pool", "exg0", lambda e: e.dma_start(out=wg0x, in_=weg_d[0].rearrange("(c p) n -> p c n", p=128)), writes=["wg0x"])
                S.dma("pool", ("exw", 1), lambda e: e.dma_start(out=wup[1], in_=weu_d[0].rearrange("(c p) n -> p c n", p=128)), writes=[("wup", 1)])
                S.dma("pool", ("exw", 1), lambda e: e.dma_start(out=wdn[1], in_=wed_d[0].rearrange("(c p) n -> p c n", p=128)), writes=[("wdn", 1)])
            MIXR = [("mixT", j) for j in range(4)]
            def e2_s1(i):
                b = i % 2
                S.dma("sp", ("x2", b), lambda e: e.dma_start(out=x2[b], in_=x_d[i * 128:(i + 1) * 128, :]), writes=[("x2", b)])
                for half in range(2):
                    bk = (2 * i + half) % 4
                    for c in range(8):
                        S.op("pe", lambda e, c=c, half=half, bk=bk: e.matmul(pbanks[bk][:, :], lhsT=mixT[:, c, i * 128:(i + 1) * 128],
                                                                             rhs=wout_sb[:, c, half * 512:(half + 1) * 512], start=(c == 0), stop=(c == 7)),
                             reads=["wout"] + MIXR, writes=[PS(bk)])
                    S.op("dve", lambda e, half=half, bk=bk: e.tensor_tensor(out=h1[:, i, half * 512:(half + 1) * 512], in0=pbanks[bk][:, :],
                                                                            in1=x2[b][:, half * 512:(half + 1) * 512], op=ALU.add),
                         reads=[PS(bk), ("x2", b)], writes=[("h1", i)])
                S.op("act", lambda e: e.activation(out=junk2, in_=h1[:, i, :], func=AF.Square, accum_out=stats2[:, i, 0:1]),
                     reads=[("h1", i)], writes=["junk2", ("s2", i)])
                S.op("act", lambda e: e.activation(out=stats2[:, i, 1:2], in_=stats2[:, i, 0:1], func=AF.Sqrt, scale=1.0 / D, bias=EPS),
                     reads=[("s2", i)], writes=[("s21", i)])
                S.op("dve", lambda e: e.reciprocal(out=stats2[:, i, 1:2], in_=stats2[:, i, 1:2]), reads=[("s21", i)], writes=[("s21", i)])
                S.op("dve", lambda e: e.scalar_tensor_tensor(out=hn2[b], in0=h1[:, i, :], scalar=stats2[:, i, 1:2], in1=g2bc, op0=ALU.mult, op1=ALU.mult),
                     reads=[("h1", i), ("s21", i), "g2bc"], writes=[("hn2", b)])

            def e2_s2(i):
                b = i % 2
                pbk = 4 + i % 2
                for c in range(8):
                    S.op("pe", lambda e, c=c: e.transpose(out=pbf(pbk)[:, c * 128:(c + 1) * 128], in_=hn2[b][:, c * 128:(c + 1) * 128], identity=ident[:, :]),
                         reads=[("hn2", b), cres(ident)], writes=[PS(pbk)])
                S.op("act", lambda e: e.copy(out=hn2T[:, :, i * 128:(i + 1) * 128], in_=pbf(pbk).rearrange("p (c n) -> p c n", c=8)),
                     reads=[PS(pbk)], writes=[("hn2T", i)])

            def e2_s3(i):
                rbk = 6 + i % 2
                for c in range(8):
                    S.op("pe", lambda e, c=c: e.matmul(pbanks[rbk][:, 0:20], lhsT=hn2T[:, c, i * 128:(i + 1) * 128], rhs=wr_sb[:, c, :],
                                                       start=(c == 0), stop=(c == 7)),
                         reads=[("hn2T", i), cres(wr_sb)], writes=[PS(rbk)])
                S.op("dve", lambda e: e.tensor_tensor(out=rl[:, i, :], in0=pbanks[rbk][:, 0:20], in1=br_sb[:, :], op=ALU.add),
                     reads=[PS(rbk), cres(br_sb)], writes=[("rl", i)])

            for it_ in range(16 + 2):
                if it_ < 16:
                    e2_s1(it_)
                if 0 <= it_ - 1 < 16:
                    e2_s2(it_ - 1)
                if 0 <= it_ - 2 < 16:
                    e2_s3(it_ - 2)
            if "h1" in dbg:
                o = dbg_out("h1", [128, 16 * D])
                S.dma("sp", "dbgS", lambda e, o=o: e.dma_start(out=o.rearrange("p (c t) -> p c t", c=16), in_=h1), reads=[("h1", i) for i in range(16)], writes=["dbg_h1"])
            if "rl" in dbg:
                o = dbg_out("rl", [128, 16 * 20])
                S.dma("sp", "dbgS", lambda e, o=o: e.dma_start(out=o.rearrange("p (c t) -> p c t", c=16), in_=rl), reads=[("rl", i) for i in range(16)], writes=["dbg_rl"])

        if active("F"):
            S.barrier()
            rt.reset()
            RL = [("rl", i) for i in range(16)]
            glog = rl[:, :, 0:4]
            gmx = rw[:, :, 0:1]
            gex = rw[:, :, 4:8]
            gsum = rw[:, :, 8:9]
            goh = rw[:, :, 12:16]
            esel = rw[:, :, 16:20]
            etmp = rw[:, :, 20:24]
            m1 = rw[:, :, 24:25]
            oh1 = rw[:, :, 28:32]
            e2v = rw[:, :, 32:36]
            m2 = rw[:, :, 36:37]
            oh2 = rw[:, :, 40:44]
            dd = rw[:, :, 44:45]
            w1 = rw[:, :, 45:46]
            w2_ = rw[:, :, 46:47]
            c4 = rw[:, :, 48:52]
            RW = "rw"
            V_ = S.op
            V_("dve", lambda e: e.tensor_reduce(out=gmx, in_=glog, axis=AX.X, op=ALU.max), reads=RL, writes=[RW])
            V_("dve", lambda e: e.tensor_tensor(out=gex, in0=glog, in1=gmx.to_broadcast([128, 16, 4]), op=ALU.subtract), reads=RL + [RW], writes=[RW])
            V_("dve", lambda e: e.tensor_tensor(out=goh, in0=glog, in1=gmx.to_broadcast([128, 16, 4]), op=ALU.is_ge), reads=RL + [RW], writes=[RW])
            V_("act", lambda e: e.activation(out=gex, in_=gex, func=AF.Exp), reads=[RW], writes=[RW])
            V_("dve", lambda e: e.tensor_reduce(out=gsum, in_=gex, axis=AX.X, op=ALU.add), reads=[RW], writes=[RW])
            V_("dve", lambda e: e.reciprocal(out=gsum, in_=gsum), reads=[RW], writes=[RW])
            for g in range(4):
                if g == 0:
                    V_("dve", lambda e: e.tensor_tensor(out=esel, in0=rl[:, :, 4:8], in1=goh[:, :, 0:1].to_broadcast([128, 16, 4]), op=ALU.mult), reads=RL + [RW], writes=[RW])
                else:
                    V_("dve", lambda e, g=g: e.tensor_tensor(out=etmp, in0=rl[:, :, 4 + 4 * g:8 + 4 * g], in1=goh[:, :, g:g + 1].to_broadcast([128, 16, 4]), op=ALU.mult),
                       reads=RL + [RW], writes=[RW])
                    V_("dve", lambda e: e.tensor_tensor(out=esel, in0=esel, in1=etmp, op=ALU.add), reads=[RW], writes=[RW])
            V_("dve", lambda e: e.tensor_reduce(out=m1, in_=esel, axis=AX.X, op=ALU.max), reads=[RW], writes=[RW])
            V_("dve", lambda e: e.tensor_tensor(out=oh1, in0=esel, in1=m1.to_broadcast([128, 16, 4]), op=ALU.is_ge), reads=[RW], writes=[RW])
            V_("dve", lambda e: e.scalar_tensor_tensor(out=e2v, in0=oh1, scalar=-1e30, in1=esel, op0=ALU.mult, op1=ALU.add), reads=[RW], writes=[RW])
            V_("dve", lambda e: e.tensor_reduce(out=m2, in_=e2v, axis=AX.X, op=ALU.max), reads=[RW], writes=[RW])
            V_("dve", lambda e: e.tensor_tensor(out=oh2, in0=e2v, in1=m2.to_broadcast([128, 16, 4]), op=ALU.is_ge), reads=[RW], writes=[RW])
            V_("dve", lambda e: e.tensor_tensor(out=dd, in0=m2, in1=m1, op=ALU.subtract), reads=[RW], writes=[RW])
            V_("act", lambda e: e.activation(out=dd, in_=dd, func=AF.Exp), reads=[RW], writes=[RW])
            V_("dve", lambda e: e.tensor_scalar(out=w1, in0=dd, scalar1=1.0, scalar2=None, op0=ALU.add), reads=[RW], writes=[RW])
            V_("dve", lambda e: e.reciprocal(out=w1, in_=w1), reads=[RW], writes=[RW])
            V_("dve", lambda e: e.tensor_tensor(out=w1, in0=w1, in1=gsum, op=ALU.mult), reads=[RW], writes=[RW])
            V_("dve", lambda e: e.tensor_tensor(out=w2_, in0=w1, in1=dd, op=ALU.mult), reads=[RW], writes=[RW])
            V_("dve", lambda e: e.tensor_tensor(out=c4, in0=oh1, in1=w1.to_broadcast([128, 16, 4]), op=ALU.mult), reads=[RW], writes=[RW])
            V_("dve", lambda e: e.tensor_tensor(out=etmp, in0=oh2, in1=w2_.to_broadcast([128, 16, 4]), op=ALU.mult), reads=[RW], writes=[RW])
            V_("dve", lambda e: e.tensor_tensor(out=c4, in0=c4, in1=etmp, op=ALU.add), reads=[RW], writes=[RW])
            for g in range(4):
                V_("dve", lambda e, g=g: e.tensor_tensor(out=comb[:, :, 4 * g:4 * g + 4], in0=c4, in1=goh[:, :, g:g + 1].to_broadcast([128, 16, 4]), op=ALU.mult),
                   reads=[RW], writes=["comb"])
            if "comb" in dbg:
                o = dbg_out("comb", [128, 16 * 16])
                S.dma("sp", "dbgS", lambda e, o=o: e.dma_start(out=o.rearrange("p (c t) -> p c t", c=16), in_=comb), reads=["comb"], writes=["dbg_comb"])

            sg = [rt.get([512], F32) for _ in range(2)]
            At = [rt.get([4, 512], BF16) for _ in range(2)]
            HN2R = [("hn2T", i) for i in range(16)]

            def load_expert(ex):
                b = (ex + 1) % 2
                if ex == 0:
                    return
                S.dma("pool", ("exw", b), lambda e: e.dma_start(out=wgt[b], in_=weg_d[ex].rearrange("(c p) n -> p c n", p=128)), writes=[("wgt", b)])
                S.dma("pool", ("exw", b), lambda e: e.dma_start(out=wup[b], in_=weu_d[ex].rearrange("(c p) n -> p c n", p=128)), writes=[("wup", b)])
                S.dma("pool", ("exw", b), lambda e: e.dma_start(out=wdn[b], in_=wed_d[ex].rearrange("(c p) n -> p c n", p=128)), writes=[("wdn", b)])

            do_g = active("G")
            if do_g:
                gfbc = rt.get([D], F32)
                junk3 = rt.get([D], BF16)
                ot = [rt.get([D], F32) for _ in range(2)]
                S.dma("sp", "miscS", lambda e: e.dma_start(out=gfbc, in_=gf_d.to_broadcast([128, D])), writes=["gfbc"])

            def g_tile(i):
                b = i % 2
                S.op("act", lambda e: e.activation(out=junk3, in_=h1[:, i, :], func=AF.Square, accum_out=stats3[:, i, 0:1]),
                     reads=[("h1", i)], writes=["junk3", ("s3", i)])
                S.op("act", lambda e: e.activation(out=stats3[:, i, 1:2], in_=stats3[:, i, 0:1], func=AF.Sqrt, scale=1.0 / D, bias=EPS),
                     reads=[("s3", i)], writes=[("s31", i)])
                S.op("dve", lambda e: e.reciprocal(out=stats3[:, i, 1:2], in_=stats3[:, i, 1:2]), reads=[("s31", i)], writes=[("s31", i)])
                S.op("dve", lambda e: e.scalar_tensor_tensor(out=ot[b], in0=h1[:, i, :], scalar=stats3[:, i, 1:2], in1=gfbc, op0=ALU.mult, op1=ALU.mult),
                     reads=[("h1", i), ("s31", i), "gfbc"], writes=[("ot", b)])
                S.dma("sp", ("st", b), lambda e: e.dma_start(out=out_d[i * 128:(i + 1) * 128, :], in_=ot[b]), reads=[("ot", b)], writes=[("out", i)])

            cnt_f = [0, 0]
            def f_gu(ex, j, b, ab):
                ts_ = slice(j * 512, (j + 1) * 512)
                for f in range(4):
                    pr = cnt_f[0] % 2
                    cnt_f[0] += 1
                    bg, bu = 0 + pr, 2 + pr
                    for c in range(8):
                        S.op("pe", lambda e, c=c, f=f, bg=bg: e.matmul(pbanks[bg][:, :], lhsT=(wg0x if ex == 0 else wgt[b])[:, c, f * 128:(f + 1) * 128], rhs=hn2T[:, c, ts_],
                                                                       start=(c == 0), stop=(c == 7)),
                             reads=[("wg0x" if ex == 0 else ("wgt", b))] + HN2R, writes=[PS(bg)])
                    S.op("act", lambda e, pr=pr, bg=bg: e.activation(out=sg[pr], in_=pbanks[bg][:, :], func=AF.Silu), reads=[PS(bg)], writes=[("sg", pr)])
                    for c in range(8):
                        S.op("pe", lambda e, c=c, f=f, bu=bu: e.matmul(pbanks[bu][:, :], lhsT=wup[b][:, c, f * 128:(f + 1) * 128], rhs=hn2T[:, c, ts_], start=(c == 0), stop=(c == 7)),
                             reads=[("wup", b)] + HN2R, writes=[PS(bu)])
                    S.op("dve", lambda e, pr=pr, f=f, bu=bu: e.tensor_tensor(out=At[ab][:, f, :], in0=pbanks[bu][:, :], in1=sg[pr], op=ALU.mult),
                         reads=[PS(bu), ("sg", pr)], writes=[("At", ab)])

            def f_down(ex, j, b, ab):
                for sub in range(4):
                    i = 4 * j + sub
                    for half in range(2):
                        bd = 4 + cnt_f[1] % 4
                        cnt_f[1] += 1
                        for f in range(4):
                            S.op("pe", lambda e, f=f, sub=sub, half=half, bd=bd: e.matmul(pbanks[bd][:, :], lhsT=At[ab][:, f, sub * 128:(sub + 1) * 128],
                                                                                          rhs=wdn[b][:, f, half * 512:(half + 1) * 512], start=(f == 0), stop=(f == 3)),
                                 reads=[("At", ab), ("wdn", b)], writes=[PS(bd)])
                        S.op("dve", lambda e, i=i, half=half, bd=bd, ex=ex: e.scalar_tensor_tensor(out=h1[:, i, half * 512:(half + 1) * 512], in0=pbanks[bd][:, :],
                                                                                                  scalar=comb[:, i, ex:ex + 1], in1=h1[:, i, half * 512:(half + 1) * 512],
                                                                                                  op0=ALU.mult, op1=ALU.add),
                             reads=[PS(bd), "comb", ("h1", i)], writes=[("h1", i)])
                    if do_g and ex == NE - 1:
                        g_tile(i)

            load_expert(0)
            fitems = [(ex, j) for ex in range(NE) for j in range(4)]
            for idx in range(len(fitems) + 1):
                if idx < len(fitems):
                    ex, j = fitems[idx]
                    f_gu(ex, j, (ex + 1) % 2, idx % 2)
                if idx >= 1:
                    exp_, jp_ = fitems[idx - 1]
                    f_down(exp_, jp_, (exp_ + 1) % 2, (idx - 1) % 2)
                if idx < len(fitems):
                    ex, j = fitems[idx]
                    if j == 0 and ex + 1 < NE:
                        load_expert(ex + 1)
            if "h2" in dbg:
                o = dbg_out("h2", [128, 16 * D])
                S.dma("sp", "dbgS", lambda e, o=o: e.dma_start(out=o.rearrange("p (c t) -> p c t", c=16), in_=h1), reads=[("h1", i) for i in range(16)], writes=["dbg_h2"])

        if active("G") and not active("F"):
            raise RuntimeError("phase G is fused into phase F")

        S.finish("sp")
        nsig = S.emit(nc)
    return nc, dbg_d, nsig


def _consts():
    f32 = np.float32
    inv = (1.0 / (10000.0 ** (np.arange(0, HD, 2, dtype=f32) / f32(HD)))).astype(f32)
    cos_t = np.zeros((128, TC), f32)
    sin_t = np.zeros((128, TC), f32)
    pos = np.arange(SEQ + NMETA, dtype=f32)
    ang = (pos[:, None] * inv[None, :]).astype(f32)
    ang = np.concatenate([ang, ang], axis=-1)
    c = np.cos(ang).astype(f32).T
    s = np.sin(ang).astype(f32).T
    s_signed = s.copy()
    s_signed[:HD // 2] *= -1.0
    for half in range(2):
        cos_t[half * 64:(half + 1) * 64, MOFF:] = c
        sin_t[half * 64:(half + 1) * 64, MOFF:] = s_signed
    ident = np.eye(128, dtype=f32)
    perm = np.zeros((128, 128), f32)
    for n2 in range(128):
        blk, d = divmod(n2, 64)
        perm[blk * 64 + (d + 32) % 64, n2] = 1.0
    k_ = np.arange(128)[:, None]
    q_ = np.arange(128)[None, :]
    tri = (q_ >= k_).astype(f32)
    tri = np.concatenate([tri, tri], axis=1)
    ones = np.ones((128, 128), f32)
    ones16 = np.zeros((128, 128), f32)
    ones16[MOFF:, :] = 1.0
    negm = np.where(q_ >= k_, 0.0, -30000.0).astype(f32)
    return dict(cos_t=cos_t, sin_t=sin_t, c_ident=ident, c_perm=perm, c_tri=tri, c_ones=ones, c_ones16=ones16, c_negm=negm)


def make_in_maps(inputs, n_cores=8):
    f = lambda a: np.ascontiguousarray(np.asarray(a, dtype=np.float32))
    x = f(inputs["x"])
    shared = dict(
        meta=f(inputs["meta"]),
        norm1_g=f(inputs["norm1_g"]).reshape(1, D),
        w_in=f(inputs["w_in"]).reshape(D, IN_W),
        b_gate_t=np.ascontiguousarray(f(inputs["b_gate"]).reshape(16, 128).T),
        lambda_q1=f(inputs["lambda_q1"]).reshape(1, HD),
        lambda_k1=f(inputs["lambda_k1"]).reshape(1, HD),
        lambda_q2=f(inputs["lambda_q2"]).reshape(1, HD),
        lambda_k2=f(inputs["lambda_k2"]).reshape(1, HD),
        subln_g_t=f(inputs["subln_g"]).reshape(128, 1),
        pool_w=f(inputs["pool_w"]).reshape(NPG, 128, 128),
        pool_scale_t=np.ascontiguousarray(f(inputs["pool_scale"]).reshape(NPG, 128).T),
        w_attn_br=f(inputs["w_attn_br"]).reshape(D, D),
        w_pool_br=f(inputs["w_pool_br"]).reshape(512, D),
        w_out=f(inputs["w_out"]).reshape(D, D),
        norm2_g=f(inputs["norm2_g"]).reshape(1, D),
        w_router=np.ascontiguousarray(np.concatenate([f(inputs["w_router_group"]).reshape(D, 4), f(inputs["w_router_expert"]).reshape(D, 16)], axis=1)),
        b_router=np.ascontiguousarray(np.concatenate([f(inputs["b_router_group"]).reshape(1, 4), f(inputs["b_router_expert"]).reshape(1, 16)], axis=1)),
        w_e_gate=f(inputs["w_e_gate"]).reshape(NE, D, DE),
        w_e_up=f(inputs["w_e_up"]).reshape(NE, D, DE),
        w_e_down=f(inputs["w_e_down"]).reshape(NE, DE, D),
        final_g=f(inputs["final_g"]).reshape(1, D),
    )
    shared.update(_consts())
    maps = []
    for b in range(n_cores):
        m = dict(shared)
        m["x"] = np.ascontiguousarray(x[b])
        maps.append(m)
    return maps


_CACHE = {}


def kernel(**inputs):
    if "nc" not in _CACHE:
        _CACHE["nc"] = build_program()[0]
    nc = _CACHE["nc"]
    maps = make_in_maps(inputs, 8)
    res = run_bass_kernel_spmd(nc, maps, core_ids=list(range(8)))
    out = np.stack([np.asarray(r["out"], dtype=np.float32) for r in res.results], axis=0)
    return out
```

```python
import contextlib
import math

import numpy as np

import concourse.bass as bass
import concourse.mybir as mybir
from concourse.bass_utils import run_bass_kernel_spmd

F32 = mybir.dt.float32
BF16 = mybir.dt.bfloat16
AF = mybir.ActivationFunctionType
ALU = mybir.AluOpType
AX = mybir.AxisListType

D = 1024
SEQ = 2048
NMETA = 16
NH = 8
HD = 64
VD = 128
NPG = 4
POOL_W = (2, 4, 8, 16)
IN_W = 5632
NE = 16
DE = 512
EPS = 1e-6
TC = 2176
MOFF = 112
ROFF = 128
O_Q, O_K, O_V, O_P, O_G = 0, 1024, 2048, 3072, 3584
LAMBDA_INIT = 0.8 - 0.6 * math.exp(-0.3 * 0)
EPOCH = 12000


class _Op:
    __slots__ = ("eng", "fn", "waits", "signal", "idx", "dma_stream", "sigcount")

    def __init__(self, eng, fn):
        self.eng = eng
        self.fn = fn
        self.waits = []
        self.signal = False
        self.idx = -1
        self.dma_stream = None
        self.sigcount = 0


class Sched:
    ENGS = ("pe", "act", "dve", "pool", "sp")

    def __init__(self):
        self.ops = {e: [] for e in self.ENGS}
        self.last_w = {}
        self.readers = {}
        self.known = {e: {} for e in self.ENGS}
        self.streams = {}
        self.pending = {e: [] for e in self.ENGS}

    def _deps(self, eng, reads, writes, is_dma=False):
        deps = {}

        def add(kind, src, n, raw):
            if kind == "e" and src == eng and not is_dma:
                if eng == "pe" or eng == "sp":
                    return
            if kind == "d":
                n = self.streams[src]
            key = (kind, src)
            if deps.get(key, -1) < n:
                deps[key] = n

        for r in reads:
            ev = self.last_w.get(r)
            if ev is not None:
                add(ev[0], ev[1], ev[2], True)
        for w in writes:
            ev = self.last_w.get(w)
            if ev is not None:
                add(ev[0], ev[1], ev[2], False)
            rd = self.readers.get(w)
            if rd:
                for key, n in rd.items():
                    add(key[0], key[1], n, False)
        for (kind, src, n) in self.pending[eng]:
            if kind == "e" and src == eng and not is_dma:
                continue
            key = (kind, src)
            if deps.get(key, -1) < n:
                deps[key] = n
        self.pending[eng] = []
        out = []
        kn = self.known[eng]
        for key, n in deps.items():
            if kn.get(key, -1) >= n:
                continue
            kn[key] = n
            out.append((key[0], key[1], n))
        return out

    def _commit(self, ev, reads, writes):
        for w in writes:
            self.last_w[w] = ev
            self.readers[w] = {}
        key = (ev[0], ev[1])
        for r in reads:
            d = self.readers.setdefault(r, {})
            if d.get(key, -1) < ev[2]:
                d[key] = ev[2]

    def _add(self, eng, fn, reads, writes, stream):
        ps_reads = [r for r in reads if isinstance(r, tuple) and r[0] == "ps"]
        if ps_reads:
            writes = list(writes) + ps_reads
        o = _Op(eng, fn)
        o.waits = self._deps(eng, reads, writes, is_dma=stream is not None)
        lst = self.ops[eng]
        o.idx = len(lst)
        lst.append(o)
        for (kind, src, n) in o.waits:
            if kind == "e":
                self.ops[src][n].signal = True
        if stream is None:
            ev = ("e", eng, o.idx)
        else:
            c = self.streams.get(stream, 0) + 1
            self.streams[stream] = c
            o.dma_stream = stream
            ev = ("d", stream, c)
        self._commit(ev, reads, writes)
        return o

    def op(self, eng, fn, reads=(), writes=()):
        return self._add(eng, fn, reads, writes, None)

    def dma(self, queue, stream, fn, reads=(), writes=()):
        return self._add(queue, fn, reads, writes, stream)

    def barrier(self):
        evs = []
        for e in self.ENGS:
            if self.ops[e]:
                evs.append(("e", e, len(self.ops[e]) - 1))
        for s, c in self.streams.items():
            if isinstance(s, tuple) and s[0] == "xwld":
                continue
            evs.append(("d", s, c))
        for e in self.ENGS:
            self.pending[e] = list(evs)
        fixed = []
        for (kind, src, n) in evs:
            if kind == "e":
                i = n
                while i >= 0 and self.ops[src][i].dma_stream is not None:
                    i -= 1
                if i < 0:
                    continue
                fixed.append(("e", src, i))
            else:
                fixed.append((kind, src, n))
        for e in self.ENGS:
            self.pending[e] = list(fixed)

    def finish(self, eng="sp"):
        self.barrier()
        o = _Op(eng, None)
        o.waits = self._deps(eng, (), ())
        for (kind, src, n) in o.waits:
            if kind == "e":
                self.ops[src][n].signal = True
        o.idx = len(self.ops[eng])
        self.ops[eng].append(o)

    def emit(self, nc):
        for e, lst in self.ops.items():
            c = 0
            for o in lst:
                if o.signal:
                    c += 1
                o.sigcount = c
        nsig = {e: (lst[-1].sigcount if lst else 0) for e, lst in self.ops.items()}
        with contextlib.ExitStack() as st:
            esems = {}
            for e in self.ENGS:
                n_ep = nsig[e] // EPOCH + 1
                esems[e] = [st.enter_context(nc.semaphore(f"s_{e}_{k}")) for k in range(n_ep)]
            dsems = {s: st.enter_context(nc.semaphore("d_" + "_".join(str(x) for x in (s if isinstance(s, tuple) else (s,))))) for s in self.streams}
            block = st.enter_context(nc.Block())

            def run(engname):
                def body(eng):
                    for o in self.ops[engname]:
                        for (kind, src, n) in o.waits:
                            if kind == "e":
                                sc = self.ops[src][n].sigcount
                                ep, v = (sc - 1) // EPOCH, (sc - 1) % EPOCH + 1
                                eng.wait_ge(esems[src][ep], v)
                            else:
                                eng.wait_ge(dsems[src], 16 * n)
                        if o.fn is None:
                            continue
                        ins = o.fn(eng)
                        if o.dma_stream is not None:
                            ins.then_inc(dsems[o.dma_stream], 16)
                        elif o.signal:
                            ep = (o.sigcount - 1) // EPOCH
                            ins.then_inc(esems[engname][ep], 1)
                return body

            block.tensor(run("pe"))
            block.scalar(run("act"))
            block.vector(run("dve"))
            block.gpsimd(run("pool"))
            block.sync(run("sp"))
        return nsig


def build_program(dbg=(), stop_after=None):
    nc = bass.Bass("TRN2", target_bir_lowering=False)
    dbg = set(dbg)

    def din(name, shape, dt=F32):
        return nc.dram_tensor(name, list(shape), dt, kind="ExternalInput").ap()

    x_d = din("x", [SEQ, D])
    meta_d = din("meta", [NMETA, D])
    g1_d = din("norm1_g", [1, D])
    w_in_d = din("w_in", [D, IN_W])
    bgate_d = din("b_gate_t", [128, 16])
    lq1_d = din("lambda_q1", [1, HD])
    lk1_d = din("lambda_k1", [1, HD])
    lq2_d = din("lambda_q2", [1, HD])
    lk2_d = din("lambda_k2", [1, HD])
    subg_d = din("subln_g_t", [128, 1])
    poolw_d = din("pool_w", [NPG, 128, 128])
    pscale_d = din("pool_scale_t", [128, NPG])
    wattn_d = din("w_attn_br", [D, D])
    wpool_d = din("w_pool_br", [512, D])
    wout_d = din("w_out", [D, D])
    g2_d = din("norm2_g", [1, D])
    wr_d = din("w_router", [D, 20])
    br_d = din("b_router", [1, 20])
    weg_d = din("w_e_gate", [NE, D, DE])
    weu_d = din("w_e_up", [NE, D, DE])
    wed_d = din("w_e_down", [NE, DE, D])
    gf_d = din("final_g", [1, D])
    cos_d = din("cos_t", [128, TC])
    sin_d = din("sin_t", [128, TC])
    ident_d = din("c_ident", [128, 128])
    perm_d = din("c_perm", [128, 128])
    tri_d = din("c_tri", [128, 256])
    ones_d = din("c_ones", [128, 128])
    ones16_d = din("c_ones16", [128, 128])
    negm_d = din("c_negm", [128, 128])
    out_d = nc.dram_tensor("out", [SEQ, D], F32, kind="ExternalOutput").ap()
    dbg_d = {}

    def dbg_out(name, shape):
        dbg_d[name] = nc.dram_tensor("dbg_" + name, list(shape), F32, kind="ExternalOutput").ap()
        return dbg_d[name]

    S = Sched()
    phases = ["A", "B0", "B1", "D", "E1", "E2", "F", "G"]
    last_phase = stop_after or "G"

    def active(ph):
        return phases.index(ph) <= phases.index(last_phase)

    with contextlib.ExitStack() as st:
        def sb(name, shape, dt):
            return st.enter_context(nc.sbuf_tensor(name, list(shape), dt))

        R1 = sb("R1", [128, 34816 // 4], F32)
        R42 = sb("R42", [128, 67584 // 4], F32)
        R35 = sb("R35", [128, 51200 // 4], F32)
        RT_BYTES = 50 * 1024
        RT = sb("RT", [128, RT_BYTES // 4], F32)

        def view(region, off_bytes, shape, dt):
            n = 1
            for s_ in shape:
                n *= s_
            if dt == F32:
                assert off_bytes % 4 == 0
                a = region[:, off_bytes // 4: off_bytes // 4 + n]
            else:
                assert off_bytes % 4 == 0 and (n * 2) % 4 == 0
                a = region[:, off_bytes // 4: off_bytes // 4 + (n * 2) // 4].bitcast(BF16)
            if len(shape) == 2:
                a = a.rearrange("p (a b) -> p a b", a=shape[0])
            elif len(shape) == 3:
                a = a.rearrange("p (a b c) -> p a b c", a=shape[0], b=shape[1])
            return a

        class RTAlloc:
            def __init__(self):
                self.off = 0

            def reset(self):
                self.off = 0

            def get(self, shape, dt):
                n = 1
                for s_ in shape:
                    n *= s_
                nb = n * (4 if dt == F32 else 2)
                nb = (nb + 31) // 32 * 32
                v = view(RT, self.off, shape, dt)
                self.off += nb
                assert self.off <= RT_BYTES, ("RT overflow", self.off)
                return v

        rt = RTAlloc()
        _tail = RT_BYTES - (1280 + 4096 + 1024)
        rl = view(RT, _tail, [16, 20], F32)
        rw = view(RT, _tail + 1280, [16, 64], F32)
        comb = view(RT, _tail + 1280 + 4096, [16, 16], F32)
        wg0x = view(RT, 35840, [8, DE], BF16)

        hnT = view(R1, 0, [8, TC], BF16)
        hn2T = view(R1, 0, [8, SEQ], BF16)
        Vt = view(R42, 0, [17, D], BF16)
        QT = view(R42, 34816, [8, SEQ], BF16)
        h1 = view(R42, 0, [16, D], F32)
        KT = view(R35, 0, [8, TC], BF16)
        mixT = view(R35, 0, [8, SEQ], BF16)
        PT = view(R35, 34816, [NPG, SEQ], BF16)
        WB = 24576
        wgt = [view(R35, k * WB, [8, DE], BF16) for k in range(2)]
        wup = [view(R35, k * WB + 8192, [8, DE], BF16) for k in range(2)]
        wdn = [view(R35, k * WB + 16384, [4, D], BF16) for k in range(2)]

        ident = sb("ident", [128, 128], BF16)
        permm = sb("permm", [128, 128], BF16)
        tri = sb("tri", [128, 2, 128], BF16)
        ones = sb("ones", [128, 128], BF16)
        ones16 = sb("ones16", [128, 128], BF16)
        negm = sb("negm", [128, 128], BF16)
        poolw = sb("poolw", [128, NPG, 128], BF16)
        bgate = sb("bgate", [128, 16], F32)
        pscale = sb("pscale", [128, NPG], F32)
        subg = sb("subg", [128, 1], F32)
        lamt = sb("lamt", [128, 4 * HD], F32)
        lams = sb("lams", [128, 8], F32)
        stats = sb("stats", [128, 17, 2], F32)
        stats2 = sb("stats2", [128, 16, 2], F32)
        stats3 = sb("stats3", [128, 16, 2], F32)
        wr_sb = sb("wr_sb", [128, 8, 20], BF16)
        br_sb = sb("br_sb", [128, 20], F32)

        _pb = [st.enter_context(nc.psum_tensor(f"pb{i}", [128, 512], F32)) for i in range(4)]
        pS = [st.enter_context(nc.psum_tensor(f"pS{i}", [128, 1024], F32)) for i in range(2)]
        pbanks = [t[:, :] for t in _pb] + [pS[0][:, 0:512], pS[0][:, 512:1024], pS[1][:, 0:512], pS[1][:, 512:1024]]

        def pbf(i):
            return pbanks[i].bitcast(BF16)

        cres = lambda t: ("c", t.name)
        for t, d_ in ((ident, ident_d), (permm, perm_d), (ones, ones_d), (ones16, ones16_d), (negm, negm_d)):
            S.dma("pool", "constP", lambda e, t=t, d_=d_: e.dma_start(out=t[:, :], in_=d_), writes=[cres(t)])
        S.dma("pool", "constP", lambda e: e.dma_start(out=tri[:, :, :], in_=tri_d.rearrange("p (a b) -> p a b", a=2)), writes=[cres(tri)])
        S.dma("pool", "constP", lambda e: e.dma_start(out=poolw[:, :, :], in_=poolw_d.rearrange("g c d -> c g d")), writes=[cres(poolw)])
        S.dma("pool", "constP", lambda e: e.dma_start(out=wr_sb[:, :, :], in_=wr_d.rearrange("(c p) n -> p c n", p=128)), writes=[cres(wr_sb)])
        for t, d_ in ((bgate, bgate_d), (pscale, pscale_d), (subg, subg_d)):
            S.dma("sp", "constS", lambda e, t=t, d_=d_: e.dma_start(out=t[:, :], in_=d_), writes=[cres(t)])
        S.dma("sp", "constS", lambda e: e.dma_start(out=br_sb[:, :], in_=br_d.to_broadcast([128, 20])), writes=[cres(br_sb)])
        for i, d_ in enumerate((lq1_d, lk1_d, lq2_d, lk2_d)):
            S.dma("sp", "constS", lambda e, i=i, d_=d_: e.dma_start(out=lamt[:, i * HD:(i + 1) * HD], in_=d_.to_broadcast([128, HD])), writes=[("lamt", i)])

        S.op("dve", lambda e: e.tensor_tensor(out=lamt[:, 0:HD], in0=lamt[:, 0:HD], in1=lamt[:, HD:2 * HD], op=ALU.mult),
             reads=[("lamt", 0), ("lamt", 1)], writes=[("lamt", 0)])
        S.op("dve", lambda e: e.tensor_tensor(out=lamt[:, 2 * HD:3 * HD], in0=lamt[:, 2 * HD:3 * HD], in1=lamt[:, 3 * HD:4 * HD], op=ALU.mult),
             reads=[("lamt", 2), ("lamt", 3)], writes=[("lamt", 2)])
        S.op("dve", lambda e: e.reduce_sum(out=lams[:, 0:1], in_=lamt[:, 0:HD], axis=AX.X), reads=[("lamt", 0)], writes=[("lams", 0)])
        S.op("dve", lambda e: e.reduce_sum(out=lams[:, 1:2], in_=lamt[:, 2 * HD:3 * HD], axis=AX.X), reads=[("lamt", 2)], writes=[("lams", 1)])
        S.op("act", lambda e: e.activation(out=lams[:, 2:4], in_=lams[:, 0:2], func=AF.Exp), reads=[("lams", 0), ("lams", 1)], writes=[("lams", 2)])
        S.op("dve", lambda e: e.scalar_tensor_tensor(out=lams[:, 4:5], in0=lams[:, 3:4], scalar=-LAMBDA_INIT, in1=lams[:, 2:3],
                                                     op0=ALU.add, op1=ALU.subtract), reads=[("lams", 2)], writes=[("lams", 4)])
        S.op("dve", lambda e: e.tensor_scalar(out=lams[:, 5:6], in0=subg[:, 0:1], scalar1=1.0 - LAMBDA_INIT, scalar2=None, op0=ALU.mult),
             reads=[cres(subg)], writes=[("lams", 5)])
        neglam = lams[:, 4:5]
        gsub = lams[:, 5:6]

        def PS(i):
            return ("ps", i)

        rt.reset()
        wblk = [rt.get([8, 512], BF16) for _ in range(2)]
        S.dma("pool", ("wld", 0), lambda e: e.dma_start(out=wblk[0], in_=w_in_d[:, O_P:O_P + 512].rearrange("(c p) n -> p c n", p=128)), writes=[("wblk", 0)])
        S.dma("pool", ("wld", 1), lambda e: e.dma_start(out=wblk[1], in_=w_in_d[:, O_Q:O_Q + 512].rearrange("(c p) n -> p c n", p=128)), writes=[("wblk", 1)])
        xw = [view(R42, n * 8192, [8, 512], BF16) for n in range(3)]
        for n, c0_ in enumerate((O_Q + 512, O_K, O_K + 512)):
            S.dma("pool", ("xwld", n), lambda e, n=n, c0_=c0_: e.dma_start(out=xw[n], in_=w_in_d[:, c0_:c0_ + 512].rearrange("(c p) n -> p c n", p=128)),
                  writes=[("xw", n)])
        g1bc = rt.get([D], F32)
        NBA = 4
        xt = [rt.get([D], F32) for _ in range(NBA)]
        junk = rt.get([D], BF16)
        xn = [rt.get([D], BF16) for _ in range(NBA)]
        S.dma("sp", "miscS", lambda e: e.dma_start(out=g1bc, in_=g1_d.to_broadcast([128, D])), writes=["g1bc"])
        S.op("pool", lambda e: e.memset(xt[0], 0.0), writes=[("xt", 0)])
        def a_s1(i):
            b = i % NBA
            if i == 0:
                S.dma("sp", ("xt", 0), lambda e: e.dma_start(out=xt[0][MOFF:128, :], in_=meta_d), writes=[("xt", 0)], reads=[])
            else:
                S.dma("sp", ("xt", b), lambda e: e.dma_start(out=xt[b], in_=x_d[(i - 1) * 128:i * 128, :]), writes=[("xt", b)])
            S.op("act", lambda e: e.activation(out=junk, in_=xt[b], func=AF.Square, accum_out=stats[:, i, 0:1]),
                 reads=[("xt", b)], writes=["junk", ("st", i)])
            S.op("act", lambda e: e.activation(out=stats[:, i, 1:2], in_=stats[:, i, 0:1], func=AF.Sqrt, scale=1.0 / D, bias=EPS),
                 reads=[("st", i)], writes=[("st1", i)])
            S.op("dve", lambda e: e.reciprocal(out=stats[:, i, 1:2], in_=stats[:, i, 1:2]), reads=[("st1", i)], writes=[("st1", i)])
            S.op("dve", lambda e: e.scalar_tensor_tensor(out=xn[b], in0=xt[b], scalar=stats[:, i, 1:2], in1=g1bc, op0=ALU.mult, op1=ALU.mult),
                 reads=[("xt", b), ("st1", i), "g1bc"], writes=[("xn", b)])

        def a_s2(i):
            b = i % NBA
            pbk = i % 4
            for c in range(8):
                S.op("pe", lambda e, c=c: e.transpose(out=pbf(pbk)[:, c * 128:(c + 1) * 128], in_=xn[b][:, c * 128:(c + 1) * 128], identity=ident[:, :]),
                     reads=[("xn", b), cres(ident)], writes=[PS(pbk)])
            S.op("act", lambda e: e.copy(out=hnT[:, :, i * 128:(i + 1) * 128], in_=pbf(pbk).rearrange("p (c n) -> p c n", c=8)),
                 reads=[PS(pbk)], writes=[("hnT", i)])

        for it_ in range(17 + 2):
            if it_ < 17:
                a_s1(it_)
            if 0 <= it_ - 2 < 17:
                a_s2(it_ - 2)
        HNT_ALL = [("hnT", i) for i in range(17)]

        if "hnT" in dbg:
            o = dbg_out("hnT", [128, 8 * TC])
            S.dma("pool", "dbgP", lambda e, o=o: e.dma_start(out=o.rearrange("p (c t) -> p c t", c=8), in_=hnT), reads=HNT_ALL, writes=["dbg_hnT"])

        def load_wblock(dst, src2d, c0, ncols, res, kch=8):
            S.dma("pool", ("wld", res[1]), lambda e: e.dma_start(out=dst, in_=src2d[:, c0:c0 + ncols].rearrange("(c p) n -> p c n", p=128)),
                  writes=[res])

        if active("B0"):
            S.barrier()
            rt.reset()
            wblk = [rt.get([8, 512], BF16) for _ in range(2)]
            u32 = [rt.get([TC], F32) for _ in range(2)]
            tmpA = rt.get([TC], F32)
            tmpB = rt.get([TC], F32)
            psrot = [0]

            def nextbank(lo, hi):
                b_ = lo + psrot[0] % (hi - lo)
                psrot[0] += 1
                return b_

            def b0_proj(g):
                ub = u32[g % 2]
                ures = ("u32", g % 2)
                for jt in range(5):
                    c0, n = (0, 128) if jt == 0 else (ROFF + (jt - 1) * 512, 512)
                    bk = nextbank(0, 4)
                    for c in range(8):
                        S.op("pe", lambda e, c=c, g=g, c0=c0, n=n, bk=bk: e.matmul(pbanks[bk][:, 0:n], lhsT=wblk[0][:, c, g * 128:(g + 1) * 128],
                                                                                 rhs=hnT[:, c, c0:c0 + n], start=(c == 0), stop=(c == 7)),
                             reads=[("wblk", 0)] + HNT_ALL, writes=[PS(bk)])
                    S.op("act", lambda e, ub=ub, c0=c0, n=n, bk=bk: e.copy(out=ub[:, c0:c0 + n], in_=pbanks[bk][:, 0:n]),
                         reads=[PS(bk)], writes=[ures])

            def b0_pool(g):
                ub = u32[g % 2]
                ures = ("u32", g % 2)
                w = POOL_W[g]
                L = TC
                cur, cur_res = ub, ures
                step = 1
                bufs = [(tmpA, "tmpA"), (tmpB, "tmpB")]
                k = 0
                while step < w:
                    dst, dres = bufs[k % 2]
                    lo = 2 * step - 1
                    S.op("dve", lambda e, dst=dst, cur=cur, lo=lo, step=step: e.tensor_tensor(out=dst[:, lo:L], in0=cur[:, lo:L], in1=cur[:, lo - step:L - step], op=ALU.add),
                         reads=[cur_res], writes=[dres])
                    cur, cur_res = dst, dres
                    step *= 2
                    k += 1
                ydst, yres = bufs[k % 2]
                ybf = ydst[:, 0:SEQ // 2].bitcast(BF16)
                S.op("dve", lambda e, ybf=ybf, cur=cur, ub=ub, w=w: e.scalar_tensor_tensor(out=ybf, in0=cur[:, ROFF:TC], scalar=1.0 / w, in1=ub[:, ROFF:TC],
                                                                                            op0=ALU.mult, op1=ALU.subtract),
                     reads=[cur_res, ures], writes=[yres])
                for jt in range(4):
                    bk = nextbank(0, 4)
                    S.op("pe", lambda e, g=g, jt=jt, bk=bk, ybf=ybf: e.matmul(pbanks[bk][:, :], lhsT=poolw[:, g, :], rhs=ybf[:, jt * 512:(jt + 1) * 512], start=True, stop=True),
                         reads=[yres, cres(poolw)], writes=[PS(bk)])
                    S.op("act", lambda e, g=g, jt=jt, bk=bk: e.activation(out=PT[:, g, jt * 512:(jt + 1) * 512], in_=pbanks[bk][:, :], func=AF.Identity, scale=pscale[:, g:g + 1]),
                         reads=[PS(bk), cres(pscale)], writes=[("PT", g)])

            for it_ in range(NPG + 1):
                if it_ < NPG:
                    b0_proj(it_)
                if it_ >= 1:
                    b0_pool(it_ - 1)
            if "PT" in dbg:
                o = dbg_out("PT", [128, NPG * SEQ])
                S.dma("pool", "dbgP", lambda e, o=o: e.dma_start(out=o.rearrange("p (c t) -> p c t", c=NPG), in_=PT), reads=[("PT", g) for g in range(NPG)], writes=["dbg_PT"])

        if active("B1"):
            S.barrier()
            rt.reset()
            wblk = [rt.get([8, 512], BF16) for _ in range(2)]
            cosT = rt.get([TC], F32)
            sinT = rt.get([TC], F32)
            zb = [rt.get([512], BF16) for _ in range(3)]
            t1 = [rt.get([512], F32) for _ in range(3)]
            t2 = [rt.get([512], F32) for _ in range(3)]
            ZB_ = (0, 1, 6)
            RB_ = (2, 3, 7)
            S.dma("sp", "miscS", lambda e: e.dma_start(out=cosT, in_=cos_d), writes=["cosT"])
            S.dma("sp", "miscS", lambda e: e.dma_start(out=sinT, in_=sin_d), writes=["sinT"])
            blocks = [("q", 0), ("q", 1), ("k", 0), ("k", 1), ("v", 0), ("v", 1)]
            col_of = {"q": O_Q, "k": O_K, "v": O_V}
            items = []

            def stage1(it):
                kind, h, c0, n, wb, nchunk, slot = it
                wbuf, wres = wb
                sl = slot % 3
                bk = ZB_[sl]
                for c in range(8):
                    S.op("pe", lambda e, c=c: e.matmul(pbanks[bk][:, 0:n], lhsT=wbuf[:, c, nchunk * 128:(nchunk + 1) * 128], rhs=hnT[:, c, c0:c0 + n],
                                                       start=(c == 0), stop=(c == 7)),
                         reads=[wres] + HNT_ALL, writes=[PS(bk)])
                S.op("act", lambda e: e.copy(out=zb[sl][:, 0:n], in_=pbanks[bk][:, 0:n]), reads=[PS(bk)], writes=[("zb", sl)])

            def stage2(it):
                kind, h, c0, n, wb, nchunk, slot = it
                sl = slot % 3
                bk = ZB_[sl]
                rb = RB_[sl]
                S.op("pe", lambda e: e.matmul(pbanks[rb][:, 0:n], lhsT=permm[:, :], rhs=zb[sl][:, 0:n], start=True, stop=True),
                     reads=[("zb", sl), cres(permm)], writes=[PS(rb)])
                S.op("dve", lambda e: e.tensor_tensor(out=t1[sl][:, 0:n], in0=pbanks[bk][:, 0:n], in1=cosT[:, c0:c0 + n], op=ALU.mult),
                     reads=[PS(bk), "cosT"], writes=[("t1", sl)])
                S.op("dve", lambda e: e.tensor_tensor(out=t2[sl][:, 0:n], in0=pbanks[rb][:, 0:n], in1=sinT[:, c0:c0 + n], op=ALU.mult),
                     reads=[PS(rb), "sinT"], writes=[("t2", sl)])
                if kind == "q":
                    dst = QT[:, h, c0 - ROFF:c0 - ROFF + n]
                    dres = ("QT", h, (c0 - ROFF) // 512)
                else:
                    dst = KT[:, h, c0:c0 + n]
                    dres = ("KT", h)
                S.op("pool", lambda e: e.tensor_tensor(out=dst, in0=t1[sl][:, 0:n], in1=t2[sl][:, 0:n], op=ALU.add),
                     reads=[("t1", sl), ("t2", sl)], writes=[dres])

            slot = 0
            prev = None
            wplan = [(wblk[1], ("wblk", 1)), (xw[0], ("xw", 0)), (xw[1], ("xw", 1)), (xw[2], ("xw", 2)), (wblk[1], ("wblk", 1)), (wblk[0], ("wblk", 0))]
            load_wblock(wblk[0], w_in_d, O_V + 512, 512, ("wblk", 0))
            for bi, (kind, half) in enumerate(blocks):
                wb = wplan[bi]
                if bi == 1:
                    load_wblock(wblk[1], w_in_d, O_V, 512, ("wblk", 1))
                if kind in ("q", "k"):
                    for nchunk in range(4):
                        h = half * 4 + nchunk
                        tiles = ([(0, 128)] if kind == "k" else []) + [(ROFF + jt * 512, 512) for jt in range(4)]
                        for (c0, n) in tiles:
                            it = (kind, h, c0, n, wb, nchunk, slot)
                            slot += 1
                            stage1(it)
                            if prev is not None:
                                stage2(prev)
                            prev = it
                else:
                    if prev is not None:
                        stage2(prev)
                        prev = None
                    for i in range(17):
                        for sub in range(1):
                            bk = 4 + (i % 2)
                            for c in range(8):
                                S.op("pe", lambda e, c=c, i=i, bk=bk, wb=wb: e.matmul(pbanks[bk][:, :], lhsT=hnT[:, c, i * 128:(i + 1) * 128], rhs=wb[0][:, c, :],
                                                                                     start=(c == 0), stop=(c == 7)),
                                     reads=[wb[1], ("hnT", i)], writes=[PS(bk)])
                            S.op("act", lambda e, i=i, bk=bk, half=half: e.copy(out=Vt[:, i, half * 512:(half + 1) * 512], in_=pbanks[bk][:, :]),
                                 reads=[PS(bk)], writes=[("V", i), ("xw", 0), ("xw", 1), ("xw", 2)])
            if prev is not None:
                stage2(prev)
                prev = None
            for nm, tns, shp, res in (("QT", QT, [128, 8 * SEQ], [("QT", h, j) for h in range(8) for j in range(4)]),
                                      ("KT", KT, [128, 8 * TC], [("KT", h) for h in range(8)]),
                                      ("V", Vt, [128, 17 * D], [("V", i) for i in range(17)])):
                if nm in dbg:
                    o = dbg_out(nm, shp)
                    S.dma("pool", "dbgP", lambda e, o=o, tns=tns: e.dma_start(out=o.rearrange("p (c t) -> p c t", c=tns.shape[1]), in_=tns), reads=res, writes=["dbg_" + nm])

        def load_e1(dc, bufs):
            b = dc % 2
            wa_, wp_, wg0_, wg1_ = bufs["wa"], bufs["wp"], bufs["wg0"], bufs["wg1"]
            S.dma("pool", ("e1w", b), lambda e: e.dma_start(out=wa_[b], in_=wattn_d[:, dc * 128:(dc + 1) * 128].rearrange("(c p) n -> p c n", p=128)), writes=[("wa", b)])
            S.dma("pool", ("e1w", b), lambda e: e.dma_start(out=wp_[b], in_=wpool_d[:, dc * 128:(dc + 1) * 128].rearrange("(c p) n -> p c n", p=128)), writes=[("wp", b)])
            S.dma("pool", ("e1w", b), lambda e: e.dma_start(out=wg0_[b], in_=w_in_d[:, O_G + dc * 128:O_G + (dc + 1) * 128].rearrange("(c p) n -> p c n", p=128)), writes=[("wg0", b)])
            S.dma("pool", ("e1w", b), lambda e: e.dma_start(out=wg1_[b], in_=w_in_d[:, O_G + D + dc * 128:O_G + D + (dc + 1) * 128].rearrange("(c p) n -> p c n", p=128)), writes=[("wg1", b)])

        if active("D"):
            S.barrier()
            rt.reset()
            wout_sb = rt.get([8, D], BF16)
            S.dma("pool", "miscP", lambda e: e.dma_start(out=wout_sb, in_=wout_d.rearrange("(c p) n -> p c n", p=128)), writes=["wout"])
            e1bufs = dict(wa=[rt.get([8, 128], BF16) for _ in range(2)], wp=[rt.get([4, 128], BF16) for _ in range(2)],
                          wg0=[rt.get([8, 128], BF16) for _ in range(2)], wg1=[rt.get([8, 128], BF16) for _ in range(2)])
            if active("E1"):
                load_e1(0, e1bufs)
                load_e1(1, e1bufs)
            pTb = [rt.get([2, 512], BF16) for _ in range(3)]
            r2 = rt.get([2, 512], F32)
            o_t = [rt.get([512], F32) for _ in range(2)]
            a_t = rt.get([512], F32)
            sq_t = rt.get([512], BF16)
            ln_t = rt.get([512], F32)
            ACC_O = (0, 2)
            ACC_S = (1, 3)
            SCALE = 1.0 / math.sqrt(HD)
            cnts = {"s": 0, "pt": 0}

            items = []
            prev_hj = None
            for h in range(NH):
                for j in range(4):
                    kts = [(0, 0, 0, False)]
                    for i in range(4 * j):
                        kts.append((1 + i, ROFF + i * 128, 0, False))
                    for r in range(4):
                        i = 4 * j + r
                        kts.append((1 + i, ROFF + i * 128, 128 * r, True))
                    for idx, kt in enumerate(kts):
                        items.append(("kt", h, j, kt, idx == 0, idx == len(kts) - 1))
                        if idx == len(kts) - 2 and prev_hj is not None:
                            items.append(("norm",) + prev_hj)
                    prev_hj = (h, j)
            final_norm = ("norm",) + prev_hj

            def stage_s(it):
                p = cnts["s"] % 2
                cnts["s"] += 1
                if it[0] == "norm":
                    _, h, j = it
                    S.op("pe", lambda e: e.matmul(pS[p][:, 0:512], lhsT=ones[:, :], rhs=sq_t, start=True, stop=True),
                         reads=["sq_t", cres(ones)], writes=[PS(4 + 2 * p)])
                    S.op("act", lambda e: e.activation(out=ln_t, in_=pS[p][:, 0:512], func=AF.Ln, scale=1.0 / VD, bias=EPS),
                         reads=[PS(4 + 2 * p)], writes=["ln_t"])
                    S.op("act", lambda e: e.activation(out=ln_t, in_=ln_t, func=AF.Exp, scale=-0.5), reads=["ln_t"], writes=["ln_t"])
                    return None
                _, h, j, (vt_i, kc0, q0, diag), first, last = it
                par = h % 2
                pb_i = cnts["pt"] % 3
                cnts["pt"] += 1
                for c in range(2):
                    S.op("pe", lambda e, c=c: e.matmul(pS[p][:, c * 512 + q0:(c + 1) * 512], lhsT=KT[c * 64:(c + 1) * 64, h, kc0:kc0 + 128],
                                                       rhs=QT[c * 64:(c + 1) * 64, h, j * 512 + q0:(j + 1) * 512], start=True, stop=not diag),
                         reads=[("KT", h), ("QT", h, j)], writes=[PS(4 + 2 * p + c)])
                    if diag:
                        S.op("pe", lambda e, c=c: e.matmul(pS[p][:, c * 512 + q0:c * 512 + q0 + 128], lhsT=ident[:, :], rhs=negm[:, :], start=False, stop=True),
                             reads=[cres(ident), cres(negm)], writes=[PS(4 + 2 * p + c)])
                S.op("act", lambda e: e.activation(out=pTb[pb_i][:, :, q0:512], in_=pS[p][:, :].rearrange("p (c n) -> p c n", c=2)[:, :, q0:512],
                                                   func=AF.Exp, scale=SCALE),
                     reads=[PS(4 + 2 * p), PS(4 + 2 * p + 1)], writes=[("pT", pb_i)])
                return pb_i

            def stage_av(it, pb_i):
                if it[0] == "norm":
                    _, h, j = it
                    S.op("dve", lambda e: e.scalar_tensor_tensor(out=QT[:, h, j * 512:(j + 1) * 512], in0=a_t, scalar=gsub, in1=ln_t, op0=ALU.mult, op1=ALU.mult),
                         reads=["a_t", "ln_t", ("lams", 5)], writes=[("QT", h, j)])
                    return
                _, h, j, (vt_i, kc0, q0, diag), first, last = it
                on = ones16 if vt_i == 0 else ones
                for c in range(2):
                    S.op("pe", lambda e, c=c: e.matmul(pbanks[ACC_O[c]][:, q0:512], lhsT=Vt[:, vt_i, h * 128:(h + 1) * 128], rhs=pTb[pb_i][:, c, q0:512],
                                                       start=first, stop=last),
                         reads=[("V", vt_i), ("pT", pb_i)], writes=[PS(ACC_O[c])])
                    S.op("pe", lambda e, c=c: e.matmul(pbanks[ACC_S[c]][:, q0:512], lhsT=on[:, :], rhs=pTb[pb_i][:, c, q0:512],
                                                       start=first, stop=last),
                         reads=[cres(on), ("pT", pb_i)], writes=[PS(ACC_S[c])])
                if last:
                    for c in range(2):
                        S.op("act", lambda e, c=c: e.activation(out=r2[:, c, :], in_=pbanks[ACC_S[c]], func=AF.Ln), reads=[PS(ACC_S[c])], writes=["r2"])
                        S.op("dve", lambda e, c=c: e.tensor_copy(out=o_t[c], in_=pbanks[ACC_O[c]]), reads=[PS(ACC_O[c])], writes=[("o_t", c)])
                    S.op("act", lambda e: e.activation(out=r2[:, :, :], in_=r2[:, :, :], func=AF.Exp, scale=-1.0), reads=["r2"], writes=["r2"])
                    for c in range(2):
                        S.op("dve", lambda e, c=c: e.tensor_tensor(out=o_t[c], in0=o_t[c], in1=r2[:, c, :], op=ALU.mult),
                             reads=[("o_t", c), "r2"], writes=[("o_t", c)])
                    S.op("dve", lambda e: e.scalar_tensor_tensor(out=a_t, in0=o_t[1], scalar=neglam, in1=o_t[0], op0=ALU.mult, op1=ALU.add),
                         reads=[("o_t", 0), ("o_t", 1), ("lams", 4)], writes=["a_t"])
                    S.op("pool", lambda e: e.tensor_tensor(out=sq_t, in0=a_t, in1=a_t, op=ALU.mult), reads=["a_t"], writes=["sq_t"])

            prev_it, prev_pb = None, None
            for it in items + [None]:
                pb_new = stage_s(it) if it is not None else None
                if prev_it is not None:
                    stage_av(prev_it, prev_pb)
                prev_it, prev_pb = it, pb_new
            stage_s(final_norm)
            stage_av(final_norm, None)
            if "AT" in dbg:
                o = dbg_out("AT", [128, 8 * SEQ])
                S.dma("pool", "dbgP", lambda e, o=o: e.dma_start(out=o.rearrange("p (c t) -> p c t", c=8), in_=QT), reads=[("QT", h, j) for h in range(8) for j in range(4)], writes=["dbg_AT"])

        if active("E1"):
            S.barrier()
            rt.reset()
            wout_sb = rt.get([8, D], BF16)
            wa = [rt.get([8, 128], BF16) for _ in range(2)]
            wp = [rt.get([4, 128], BF16) for _ in range(2)]
            wg0 = [rt.get([8, 128], BF16) for _ in range(2)]
            wg1 = [rt.get([8, 128], BF16) for _ in range(2)]
            gt = [[rt.get([512], F32) for _ in range(2)] for _ in range(2)]
            mt = [[rt.get([512], F32) for _ in range(2)] for _ in range(2)]
            ATR = [("QT", h, j) for h in range(8) for j in range(4)]

            def e1_tile(dc, j, b, pr):
                cs = slice(ROFF + j * 512, ROFF + (j + 1) * 512)
                qs = slice(j * 512, (j + 1) * 512)
                bk_g0, bk_a, bk_g1, bk_p = (0 + pr, 2 + pr, 4 + pr, 6 + pr)
                for c in range(8):
                    S.op("pe", lambda e, c=c: e.matmul(pbanks[bk_g0][:, :], lhsT=wg0[b][:, c, :], rhs=hnT[:, c, cs], start=(c == 0), stop=(c == 7)),
                         reads=[("wg0", b)] + HNT_ALL, writes=[PS(bk_g0)])
                S.op("act", lambda e: e.activation(out=gt[0][pr], in_=pbanks[bk_g0][:, :], func=AF.Sigmoid, bias=bgate[:, dc:dc + 1]),
                     reads=[PS(bk_g0), cres(bgate)], writes=[("gt", 0, pr)])
                for c in range(8):
                    S.op("pe", lambda e, c=c: e.matmul(pbanks[bk_a][:, :], lhsT=wa[b][:, c, :], rhs=QT[:, c, qs], start=(c == 0), stop=(c == 7)),
                         reads=[("wa", b)] + ATR, writes=[PS(bk_a)])
                S.op("dve", lambda e: e.tensor_tensor(out=mt[0][pr], in0=pbanks[bk_a][:, :], in1=gt[0][pr], op=ALU.mult),
                     reads=[PS(bk_a), ("gt", 0, pr)], writes=[("mt", 0, pr)])
                for c in range(8):
                    S.op("pe", lambda e, c=c: e.matmul(pbanks[bk_g1][:, :], lhsT=wg1[b][:, c, :], rhs=hnT[:, c, cs], start=(c == 0), stop=(c == 7)),
                         reads=[("wg1", b)] + HNT_ALL, writes=[PS(bk_g1)])
                S.op("act", lambda e: e.activation(out=gt[1][pr], in_=pbanks[bk_g1][:, :], func=AF.Sigmoid, bias=bgate[:, 8 + dc:9 + dc]),
                     reads=[PS(bk_g1), cres(bgate)], writes=[("gt", 1, pr)])
                for c in range(4):
                    S.op("pe", lambda e, c=c: e.matmul(pbanks[bk_p][:, :], lhsT=wp[b][:, c, :], rhs=PT[:, c, qs], start=(c == 0), stop=(c == 3)),
                         reads=[("wp", b)] + [("PT", g) for g in range(NPG)], writes=[PS(bk_p)])
                S.op("dve", lambda e: e.tensor_tensor(out=mt[1][pr], in0=pbanks[bk_p][:, :], in1=gt[1][pr], op=ALU.mult),
                     reads=[PS(bk_p), ("gt", 1, pr)], writes=[("mt", 1, pr)])
                S.op("pool", lambda e: e.tensor_tensor(out=mixT[:, dc, qs], in0=mt[0][pr], in1=mt[1][pr], op=ALU.add),
                     reads=[("mt", 0, pr), ("mt", 1, pr)], writes=[("mixT", j)])

            e1bufs = dict(wa=wa, wp=wp, wg0=wg0, wg1=wg1)
            if not active("D"):
                load_e1(0, e1bufs)
                load_e1(1, e1bufs)
            cnt = 0
            for dc in range(8):
                b = dc % 2
                if dc >= 1 and dc + 1 < 8:
                    load_e1(dc + 1, e1bufs)
                for j in range(4):
                    e1_tile(dc, j, b, cnt % 2)
                    cnt += 1
            if "mixT" in dbg:
                o = dbg_out("mixT", [128, 8 * SEQ])
                S.dma("pool", "dbgP", lambda e, o=o: e.dma_start(out=o.rearrange("p (c t) -> p c t", c=8), in_=mixT), reads=[("mixT", j) for j in range(4)], writes=["dbg_mixT"])

        if active("E2"):
            S.barrier()
            rt.reset()
            wout_sb = rt.get([8, D], BF16)
            x2 = [rt.get([D], F32) for _ in range(2)]
            g2bc = rt.get([D], F32)
            junk2 = rt.get([D], BF16)
            hn2 = [rt.get([D], BF16) for _ in range(2)]
            S.dma("sp", "miscS", lambda e: e.dma_start(out=g2bc, in_=g2_d.to_broadcast([128, D])), writes=["g2bc"])
            if active("F"):
                S.dma("pool", "exg0", lambda e: e.dma_start(out=wg0x, in_=weg_d[0].rearrange("(c p) n -> p c n", p=128)), writes=["wg0x"])
                S.dma("pool", ("exw", 1), lambda e: e.dma_start(out=wup[1], in_=weu_d[0].rearrange("(c p) n -> p c n", p=128)), writes=[("wup", 1)])
                S.dma("pool", ("exw", 1), lambda e: e.dma_start(out=wdn[1], in_=wed_d[0].rearrange("(c p) n -> p c n", p=128)), writes=[("wdn", 1)])
            MIXR = [("mixT", j) for j in range(4)]
            def e2_s1(i):
                b = i % 2
                S.dma("sp", ("x2", b), lambda e: e.dma_start(out=x2[b], in_=x_d[i * 128:(i + 1) * 128, :]), writes=[("x2", b)])
                for half in range(2):
                    bk = (2 * i + half) % 4
                    for c in range(8):
                        S.op("pe", lambda e, c=c, half=half, bk=bk: e.matmul(pbanks[bk][:, :], lhsT=mixT[:, c, i * 128:(i + 1) * 128],
                                                                             rhs=wout_sb[:, c, half * 512:(half + 1) * 512], start=(c == 0), stop=(c == 7)),
                             reads=["wout"] + MIXR, writes=[PS(bk)])
                    S.op("dve", lambda e, half=half, bk=bk: e.tensor_tensor(out=h1[:, i, half * 512:(half + 1) * 512], in0=pbanks[bk][:, :],
                                                                            in1=x2[b][:, half * 512:(half + 1) * 512], op=ALU.add),
                         reads=[PS(bk), ("x2", b)], writes=[("h1", i)])
                S.op("act", lambda e: e.activation(out=junk2, in_=h1[:, i, :], func=AF.Square, accum_out=stats2[:, i, 0:1]),
                     reads=[("h1", i)], writes=["junk2", ("s2", i)])
                S.op("act", lambda e: e.activation(out=stats2[:, i, 1:2], in_=stats2[:, i, 0:1], func=AF.Sqrt, scale=1.0 / D, bias=EPS),
                     reads=[("s2", i)], writes=[("s21", i)])
                S.op("dve", lambda e: e.reciprocal(out=stats2[:, i, 1:2], in_=stats2[:, i, 1:2]), reads=[("s21", i)], writes=[("s21", i)])
                S.op("dve", lambda e: e.scalar_tensor_tensor(out=hn2[b], in0=h1[:, i, :], scalar=stats2[:, i, 1:2], in1=g2bc, op0=ALU.mult, op1=ALU.mult),
                     reads=[("h1", i), ("s21", i), "g2bc"], writes=[("hn2", b)])

            def e2_s2(i):
                b = i % 2
                pbk = 4 + i % 2
                for c in range(8):
                    S.op("pe", lambda e, c=c: e.transpose(out=pbf(pbk)[:, c * 128:(c + 1) * 128], in_=hn2[b][:, c * 128:(c + 1) * 128], identity=ident[:, :]),
                         reads=[("hn2", b), cres(ident)], writes=[PS(pbk)])
                S.op("act", lambda e: e.copy(out=hn2T[:, :, i * 128:(i + 1) * 128], in_=pbf(pbk).rearrange("p (c n) -> p c n", c=8)),
                     reads=[PS(pbk)], writes=[("hn2T", i)])

            def e2_s3(i):
                rbk = 6 + i % 2
                for c in range(8):
                    S.op("pe", lambda e, c=c: e.matmul(pbanks[rbk][:, 0:20], lhsT=hn2T[:, c, i * 128:(i + 1) * 128], rhs=wr_sb[:, c, :],
                                                       start=(c == 0), stop=(c == 7)),
                         reads=[("hn2T", i), cres(wr_sb)], writes=[PS(rbk)])
                S.op("dve", lambda e: e.tensor_tensor(out=rl[:, i, :], in0=pbanks[rbk][:, 0:20], in1=br_sb[:, :], op=ALU.add),
                     reads=[PS(rbk), cres(br_sb)], writes=[("rl", i)])

            for it_ in range(16 + 2):
                if it_ < 16:
                    e2_s1(it_)
                if 0 <= it_ - 1 < 16:
                    e2_s2(it_ - 1)
                if 0 <= it_ - 2 < 16:
                    e2_s3(it_ - 2)
            if "h1" in dbg:
                o = dbg_out("h1", [128, 16 * D])
                S.dma("sp", "dbgS", lambda e, o=o: e.dma_start(out=o.rearrange("p (c t) -> p c t", c=16), in_=h1), reads=[("h1", i) for i in range(16)], writes=["dbg_h1"])
            if "rl" in dbg:
                o = dbg_out("rl", [128, 16 * 20])
                S.dma("sp", "dbgS", lambda e, o=o: e.dma_start(out=o.rearrange("p (c t) -> p c t", c=16), in_=rl), reads=[("rl", i) for i in range(16)], writes=["dbg_rl"])

        if active("F"):
            S.barrier()
            rt.reset()
            RL = [("rl", i) for i in range(16)]
            glog = rl[:, :, 0:4]
            gmx = rw[:, :, 0:1]
            gex = rw[:, :, 4:8]
            gsum = rw[:, :, 8:9]
            goh = rw[:, :, 12:16]
            esel = rw[:, :, 16:20]
            etmp = rw[:, :, 20:24]
            m1 = rw[:, :, 24:25]
            oh1 = rw[:, :, 28:32]
            e2v = rw[:, :, 32:36]
            m2 = rw[:, :, 36:37]
            oh2 = rw[:, :, 40:44]
            dd = rw[:, :, 44:45]
            w1 = rw[:, :, 45:46]
            w2_ = rw[:, :, 46:47]
            c4 = rw[:, :, 48:52]
            RW = "rw"
            V_ = S.op
            V_("dve", lambda e: e.tensor_reduce(out=gmx, in_=glog, axis=AX.X, op=ALU.max), reads=RL, writes=[RW])
            V_("dve", lambda e: e.tensor_tensor(out=gex, in0=glog, in1=gmx.to_broadcast([128, 16, 4]), op=ALU.subtract), reads=RL + [RW], writes=[RW])
            V_("dve", lambda e: e.tensor_tensor(out=goh, in0=glog, in1=gmx.to_broadcast([128, 16, 4]), op=ALU.is_ge), reads=RL + [RW], writes=[RW])
            V_("act", lambda e: e.activation(out=gex, in_=gex, func=AF.Exp), reads=[RW], writes=[RW])
            V_("dve", lambda e: e.tensor_reduce(out=gsum, in_=gex, axis=AX.X, op=ALU.add), reads=[RW], writes=[RW])
            V_("dve", lambda e: e.reciprocal(out=gsum, in_=gsum), reads=[RW], writes=[RW])
            for g in range(4):
                if g == 0:
                    V_("dve", lambda e: e.tensor_tensor(out=esel, in0=rl[:, :, 4:8], in1=goh[:, :, 0:1].to_broadcast([128, 16, 4]), op=ALU.mult), reads=RL + [RW], writes=[RW])
                else:
                    V_("dve", lambda e, g=g: e.tensor_tensor(out=etmp, in0=rl[:, :, 4 + 4 * g:8 + 4 * g], in1=goh[:, :, g:g + 1].to_broadcast([128, 16, 4]), op=ALU.mult),
                       reads=RL + [RW], writes=[RW])
                    V_("dve", lambda e: e.tensor_tensor(out=esel, in0=esel, in1=etmp, op=ALU.add), reads=[RW], writes=[RW])
            V_("dve", lambda e: e.tensor_reduce(out=m1, in_=esel, axis=AX.X, op=ALU.max), reads=[RW], writes=[RW])
            V_("dve", lambda e: e.tensor_tensor(out=oh1, in0=esel, in1=m1.to_broadcast([128, 16, 4]), op=ALU.is_ge), reads=[RW], writes=[RW])
            V_("dve", lambda e: e.scalar_tensor_tensor(out=e2v, in0=oh1, scalar=-1e30, in1=esel, op0=ALU.mult, op1=ALU.add), reads=[RW], writes=[RW])
            V_("dve", lambda e: e.tensor_reduce(out=m2, in_=e2v, axis=AX.X, op=ALU.max), reads=[RW], writes=[RW])
            V_("dve", lambda e: e.tensor_tensor(out=oh2, in0=e2v, in1=m2.to_broadcast([128, 16, 4]), op=ALU.is_ge), reads=[RW], writes=[RW])
            V_("dve", lambda e: e.tensor_tensor(out=dd, in0=m2, in1=m1, op=ALU.subtract), reads=[RW], writes=[RW])
            V_("act", lambda e: e.activation(out=dd, in_=dd, func=AF.Exp), reads=[RW], writes=[RW])
            V_("dve", lambda e: e.tensor_scalar(out=w1, in0=dd, scalar1=1.0, scalar2=None, op0=ALU.add), reads=[RW], writes=[RW])
            V_("dve", lambda e: e.reciprocal(out=w1, in_=w1), reads=[RW], writes=[RW])
            V_("dve", lambda e: e.tensor_tensor(out=w1, in0=w1, in1=gsum, op=ALU.mult), reads=[RW], writes=[RW])
            V_("dve", lambda e: e.tensor_tensor(out=w2_, in0=w1, in1=dd, op=ALU.mult), reads=[RW], writes=[RW])
            V_("dve", lambda e: e.tensor_tensor(out=c4, in0=oh1, in1=w1.to_broadcast([128, 16, 4]), op=ALU.mult), reads=[RW], writes=[RW])
            V_("dve", lambda e: e.tensor_tensor(out=etmp, in0=oh2, in1=w2_.to_broadcast([128, 16, 4]), op=ALU.mult), reads=[RW], writes=[RW])
            V_("dve", lambda e: e.tensor_tensor(out=c4, in0=c4, in1=etmp, op=ALU.add), reads=[RW], writes=[RW])
            for g in range(4):
                V_("dve", lambda e, g=g: e.tensor_tensor(out=comb[:, :, 4 * g:4 * g + 4], in0=c4, in1=goh[:, :, g:g + 1].to_broadcast([128, 16, 4]), op=ALU.mult),
                   reads=[RW], writes=["comb"])
            if "comb" in dbg:
                o = dbg_out("comb", [128, 16 * 16])
                S.dma("sp", "dbgS", lambda e, o=o: e.dma_start(out=o.rearrange("p (c t) -> p c t", c=16), in_=comb), reads=["comb"], writes=["dbg_comb"])

            sg = [rt.get([512], F32) for _ in range(2)]
            At = [rt.get([4, 512], BF16) for _ in range(2)]
            HN2R = [("hn2T", i) for i in range(16)]

            def load_expert(ex):
                b = (ex + 1) % 2
                if ex == 0:
                    return
                S.dma("pool", ("exw", b), lambda e: e.dma_start(out=wgt[b], in_=weg_d[ex].rearrange("(c p) n -> p c n", p=128)), writes=[("wgt", b)])
                S.dma("pool", ("exw", b), lambda e: e.dma_start(out=wup[b], in_=weu_d[ex].rearrange("(c p) n -> p c n", p=128)), writes=[("wup", b)])
                S.dma("pool", ("exw", b), lambda e: e.dma_start(out=wdn[b], in_=wed_d[ex].rearrange("(c p) n -> p c n", p=128)), writes=[("wdn", b)])

            do_g = active("G")
            if do_g:
                gfbc = rt.get([D], F32)
                junk3 = rt.get([D], BF16)
                ot = [rt.get([D], F32) for _ in range(2)]
                S.dma("sp", "miscS", lambda e: e.dma_start(out=gfbc, in_=gf_d.to_broadcast([128, D])), writes=["gfbc"])

            def g_tile(i):
                b = i % 2
                S.op("act", lambda e: e.activation(out=junk3, in_=h1[:, i, :], func=AF.Square, accum_out=stats3[:, i, 0:1]),
                     reads=[("h1", i)], writes=["junk3", ("s3", i)])
                S.op("act", lambda e: e.activation(out=stats3[:, i, 1:2], in_=stats3[:, i, 0:1], func=AF.Sqrt, scale=1.0 / D, bias=EPS),
                     reads=[("s3", i)], writes=[("s31", i)])
                S.op("dve", lambda e: e.reciprocal(out=stats3[:, i, 1:2], in_=stats3[:, i, 1:2]), reads=[("s31", i)], writes=[("s31", i)])
                S.op("dve", lambda e: e.scalar_tensor_tensor(out=ot[b], in0=h1[:, i, :], scalar=stats3[:, i, 1:2], in1=gfbc, op0=ALU.mult, op1=ALU.mult),
                     reads=[("h1", i), ("s31", i), "gfbc"], writes=[("ot", b)])
                S.dma("sp", ("st", b), lambda e: e.dma_start(out=out_d[i * 128:(i + 1) * 128, :], in_=ot[b]), reads=[("ot", b)], writes=[("out", i)])

            cnt_f = [0, 0]
            def f_gu(ex, j, b, ab):
                ts_ = slice(j * 512, (j + 1) * 512)
                for f in range(4):
                    pr = cnt_f[0] % 2
                    cnt_f[0] += 1
                    bg, bu = 0 + pr, 2 + pr
                    for c in range(8):
                        S.op("pe", lambda e, c=c, f=f, bg=bg: e.matmul(pbanks[bg][:, :], lhsT=(wg0x if ex == 0 else wgt[b])[:, c, f * 128:(f + 1) * 128], rhs=hn2T[:, c, ts_],
                                                                       start=(c == 0), stop=(c == 7)),
                             reads=[("wg0x" if ex == 0 else ("wgt", b))] + HN2R, writes=[PS(bg)])
                    S.op("act", lambda e, pr=pr, bg=bg: e.activation(out=sg[pr], in_=pbanks[bg][:, :], func=AF.Silu), reads=[PS(bg)], writes=[("sg", pr)])
                    for c in range(8):
                        S.op("pe", lambda e, c=c, f=f, bu=bu: e.matmul(pbanks[bu][:, :], lhsT=wup[b][:, c, f * 128:(f + 1) * 128], rhs=hn2T[:, c, ts_], start=(c == 0), stop=(c == 7)),
                             reads=[("wup", b)] + HN2R, writes=[PS(bu)])
                    S.op("dve", lambda e, pr=pr, f=f, bu=bu: e.tensor_tensor(out=At[ab][:, f, :], in0=pbanks[bu][:, :], in1=sg[pr], op=ALU.mult),
                         reads=[PS(bu), ("sg", pr)], writes=[("At", ab)])

            def f_down(ex, j, b, ab):
                for sub in range(4):
                    i = 4 * j + sub
                    for half in range(2):
                        bd = 4 + cnt_f[1] % 4
                        cnt_f[1] += 1
                        for f in range(4):
                            S.op("pe", lambda e, f=f, sub=sub, half=half, bd=bd: e.matmul(pbanks[bd][:, :], lhsT=At[ab][:, f, sub * 128:(sub + 1) * 128],
                                                                                          rhs=wdn[b][:, f, half * 512:(half + 1) * 512], start=(f == 0), stop=(f == 3)),
                                 reads=[("At", ab), ("wdn", b)], writes=[PS(bd)])
                        S.op("dve", lambda e, i=i, half=half, bd=bd, ex=ex: e.scalar_tensor_tensor(out=h1[:, i, half * 512:(half + 1) * 512], in0=pbanks[bd][:, :],
                                                                                                  scalar=comb[:, i, ex:ex + 1], in1=h1[:, i, half * 512:(half + 1) * 512],
                                                                                                  op0=ALU.mult, op1=ALU.add),
                             reads=[PS(bd), "comb", ("h1", i)], writes=[("h1", i)])
                    if do_g and ex == NE - 1:
                        g_tile(i)

            load_expert(0)
            fitems = [(ex, j) for ex in range(NE) for j in range(4)]
            for idx in range(len(fitems) + 1):
                if idx < len(fitems):
                    ex, j = fitems[idx]
                    f_gu(ex, j, (ex + 1) % 2, idx % 2)
                if idx >= 1:
                    exp_, jp_ = fitems[idx - 1]
                    f_down(exp_, jp_, (exp_ + 1) % 2, (idx - 1) % 2)
                if idx < len(fitems):
                    ex, j = fitems[idx]
                    if j == 0 and ex + 1 < NE:
                        load_expert(ex + 1)
            if "h2" in dbg:
                o = dbg_out("h2", [128, 16 * D])
                S.dma("sp", "dbgS", lambda e, o=o: e.dma_start(out=o.rearrange("p (c t) -> p c t", c=16), in_=h1), reads=[("h1", i) for i in range(16)], writes=["dbg_h2"])

        if active("G") and not active("F"):
            raise RuntimeError("phase G is fused into phase F")

        S.finish("sp")
        nsig = S.emit(nc)
    return nc, dbg_d, nsig


def _consts():
    f32 = np.float32
    inv = (1.0 / (10000.0 ** (np.arange(0, HD, 2, dtype=f32) / f32(HD)))).astype(f32)
    cos_t = np.zeros((128, TC), f32)
    sin_t = np.zeros((128, TC), f32)
    pos = np.arange(SEQ + NMETA, dtype=f32)
    ang = (pos[:, None] * inv[None, :]).astype(f32)
    ang = np.concatenate([ang, ang], axis=-1)
    c = np.cos(ang).astype(f32).T
    s = np.sin(ang).astype(f32).T
    s_signed = s.copy()
    s_signed[:HD // 2] *= -1.0
    for half in range(2):
        cos_t[half * 64:(half + 1) * 64, MOFF:] = c
        sin_t[half * 64:(half + 1) * 64, MOFF:] = s_signed
    ident = np.eye(128, dtype=f32)
    perm = np.zeros((128, 128), f32)
    for n2 in range(128):
        blk, d = divmod(n2, 64)
        perm[blk * 64 + (d + 32) % 64, n2] = 1.0
    k_ = np.arange(128)[:, None]
    q_ = np.arange(128)[None, :]
    tri = (q_ >= k_).astype(f32)
    tri = np.concatenate([tri, tri], axis=1)
    ones = np.ones((128, 128), f32)
    ones16 = np.zeros((128, 128), f32)
    ones16[MOFF:, :] = 1.0
    negm = np.where(q_ >= k_, 0.0, -30000.0).astype(f32)
    return dict(cos_t=cos_t, sin_t=sin_t, c_ident=ident, c_perm=perm, c_tri=tri, c_ones=ones, c_ones16=ones16, c_negm=negm)


def make_in_maps(inputs, n_cores=8):
    f = lambda a: np.ascontiguousarray(np.asarray(a, dtype=np.float32))
    x = f(inputs["x"])
    shared = dict(
        meta=f(inputs["meta"]),
        norm1_g=f(inputs["norm1_g"]).reshape(1, D),
        w_in=f(inputs["w_in"]).reshape(D, IN_W),
        b_gate_t=np.ascontiguousarray(f(inputs["b_gate"]).reshape(16, 128).T),
        lambda_q1=f(inputs["lambda_q1"]).reshape(1, HD),
        lambda_k1=f(inputs["lambda_k1"]).reshape(1, HD),
        lambda_q2=f(inputs["lambda_q2"]).reshape(1, HD),
        lambda_k2=f(inputs["lambda_k2"]).reshape(1, HD),
        subln_g_t=f(inputs["subln_g"]).reshape(128, 1),
        pool_w=f(inputs["pool_w"]).reshape(NPG, 128, 128),
        pool_scale_t=np.ascontiguousarray(f(inputs["pool_scale"]).reshape(NPG, 128).T),
        w_attn_br=f(inputs["w_attn_br"]).reshape(D, D),
        w_pool_br=f(inputs["w_pool_br"]).reshape(512, D),
        w_out=f(inputs["w_out"]).reshape(D, D),
        norm2_g=f(inputs["norm2_g"]).reshape(1, D),
        w_router=np.ascontiguousarray(np.concatenate([f(inputs["w_router_group"]).reshape(D, 4), f(inputs["w_router_expert"]).reshape(D, 16)], axis=1)),
        b_router=np.ascontiguousarray(np.concatenate([f(inputs["b_router_group"]).reshape(1, 4), f(inputs["b_router_expert"]).reshape(1, 16)], axis=1)),
        w_e_gate=f(inputs["w_e_gate"]).reshape(NE, D, DE),
        w_e_up=f(inputs["w_e_up"]).reshape(NE, D, DE),
        w_e_down=f(inputs["w_e_down"]).reshape(NE, DE, D),
        final_g=f(inputs["final_g"]).reshape(1, D),
    )
    shared.update(_consts())
    maps = []
    for b in range(n_cores):
        m = dict(shared)
        m["x"] = np.ascontiguousarray(x[b])
        maps.append(m)
    return maps


_CACHE = {}


def kernel(**inputs):
    if "nc" not in _CACHE:
        _CACHE["nc"] = build_program()[0]
    nc = _CACHE["nc"]
    maps = make_in_maps(inputs, 8)
    res = run_bass_kernel_spmd(nc, maps, core_ids=list(range(8)))
    out = np.stack([np.asarray(r["out"], dtype=np.float32) for r in res.results], axis=0)
    return out
```
